# Optimizing a Trainium2 kernel written in Bass

```python
import math
import jax, jax.numpy as jnp
from jax import lax
import numpy as np

D_MODEL = 4096
BATCH = 1
SEQ = 8192
DEPTH = 2

N_EVEN = (DEPTH + 1) // 2
N_ODD = DEPTH // 2
MIX_WIDTH = D_MODEL
GROUP_WIDTH = MIX_WIDTH // 2

A_HEAD_DIM = 128
A_HEADS = GROUP_WIDTH // A_HEAD_DIM
A_KV_HEADS = A_HEADS // 4
A_ROT = A_HEAD_DIM // 4
IDX_HEADS = 32
IDX_DIM = 64
IDX_ROT = IDX_DIM // 4
TOPK_MAX = 256
Q_BLOCK = 128
ROPE_THETA = 500000.0

RWKV_HEAD = 64
RWKV_DIM = GROUP_WIDTH
RWKV_HEADS = RWKV_DIM // RWKV_HEAD
DECAY_RANK = max(32, int(round(1.8 * RWKV_DIM ** 0.5 / 32)) * 32)
AAA_RANK = max(32, int(round(1.8 * RWKV_DIM ** 0.5 / 32)) * 32)
GATE_RANK = max(32, int(round(0.6 * RWKV_DIM ** 0.8 / 32)) * 32)
RWKV_GN_EPS = 1e-5 * RWKV_HEAD

S5_DIM = GROUP_WIDTH
S5_GROUP = 16
S5_GROUPS = S5_DIM // S5_GROUP
S5_STATE = 64
S5_STEP_MIN = 0.001
S5_STEP_MAX = 0.1

RET_HEAD_DIM = 256
RET_HEADS = GROUP_WIDTH // RET_HEAD_DIM
RET_CHUNK = 128
RET_ROPE_BASE = 10000.0
RET_GN_EPS = 1e-5

D_FF = ((8 * D_MODEL + 3 * 256 - 1) // (3 * 256)) * 256
NORM_EPS = 1e-6

A_SPLITS = (A_HEADS * A_HEAD_DIM, A_KV_HEADS * A_HEAD_DIM, A_KV_HEADS * A_HEAD_DIM,
            IDX_HEADS * IDX_DIM, IDX_DIM, IDX_HEADS)
RWKV_SPLITS = (RWKV_DIM, RWKV_DIM, RWKV_DIM, DECAY_RANK, AAA_RANK, GATE_RANK)
RWKV_COLS = sum(RWKV_SPLITS)
EVEN_SPLITS = A_SPLITS + (RWKV_COLS,)
EVEN_IN = sum(EVEN_SPLITS)
ODD_SPLITS = (S5_DIM, RET_HEADS * RET_HEAD_DIM, RET_HEADS * RET_HEAD_DIM,
              RET_HEADS * RET_HEAD_DIM, RET_HEADS * RET_HEAD_DIM)
ODD_IN = sum(ODD_SPLITS)

kernel_name = "hybrid_dsa_rwkv7_s5_retention_block"


def _rms_norm(x, g):
    x32 = x.astype(jnp.float32)
    y = x32 * lax.rsqrt(jnp.mean(x32 * x32, axis=-1, keepdims=True) + NORM_EPS)
    return (y * g.astype(jnp.float32)).astype(x.dtype)


def _head_norm(y, eps):
    mean = jnp.mean(y, axis=-1, keepdims=True)
    var = jnp.mean(jnp.square(y - mean), axis=-1, keepdims=True)
    return (y - mean) * lax.rsqrt(var + eps)


def _split_cols(z, sizes):
    out, off = [], 0
    for s in sizes:
        out.append(z[..., off:off + s])
        off += s
    return out


def _partial_inv_freq(rot_dim):
    return ROPE_THETA ** (-jnp.arange(0, rot_dim, 2, dtype=jnp.float32) / rot_dim)


def _rotate(x, inv_freq):
    n_half = inv_freq.shape[0]
    L = x.shape[1]
    ang = jnp.arange(L, dtype=jnp.float32)[:, None] * inv_freq[None, :]
    cos = jnp.cos(ang)[None, :, None, :]
    sin = jnp.sin(ang)[None, :, None, :]
    x32 = x.astype(jnp.float32)
    x1 = x32[..., :n_half]
    x2 = x32[..., n_half:2 * n_half]
    out = jnp.concatenate([x1 * cos - x2 * sin, x1 * sin + x2 * cos, x32[..., 2 * n_half:]], axis=-1)
    return out.astype(x.dtype)


def _token_shift(z):
    return jnp.pad(z[:, :-1], ((0, 0), (1, 0), (0, 0)))


def _swiglu(h, w_gate, w_up, w_down):
    return (jax.nn.silu(h @ w_gate) * (h @ w_up)) @ w_down


def _dsa_attention(q, k, v, qi, ki, wi):
    B, L = q.shape[:2]
    n_top = min(TOPK_MAX, L // 4)
    nb = L // Q_BLOCK
    grp = A_HEADS // A_KV_HEADS
    key_pos = jnp.arange(L)
    ki32 = ki.astype(jnp.float32)

    def to_blocks(t):
        return t.reshape((B, nb, Q_BLOCK) + t.shape[2:]).swapaxes(0, 1)

    def one_block(args):
        bi, qb, qib, wib = args
        q_pos = bi * Q_BLOCK + jnp.arange(Q_BLOCK)
        logits = jnp.einsum('bqhd,bsd->bqhs', qib.astype(jnp.float32), ki32) * (IDX_DIM ** -0.5)
        score = jnp.einsum('bqhs,bqh->bqs', jax.nn.relu(logits),
                           wib.astype(jnp.float32) * (IDX_HEADS ** -0.5))
        causal = key_pos[None, :] <= q_pos[:, None]
        score = jnp.where(causal[None], score, -jnp.inf)
        _, idx = lax.top_k(score, n_top)
        valid = idx <= q_pos[None, :, None]
        k_sel = jax.vmap(lambda kk, ii: kk[ii])(k, idx)
        v_sel = jax.vmap(lambda vv, ii: vv[ii])(v, idx)
        qg = qb.reshape(B, Q_BLOCK, A_KV_HEADS, grp, A_HEAD_DIM).astype(jnp.float32)
        s = jnp.einsum('bqhgd,bqkhd->bqhgk', qg, k_sel.astype(jnp.float32)) * (A_HEAD_DIM ** -0.5)
        s = jnp.where(valid[:, :, None, None, :], s, -jnp.inf)
        p = jax.nn.softmax(s, axis=-1)
        o = jnp.einsum('bqhgk,bqkhd->bqhgd', p, v_sel.astype(jnp.float32))
        return o.reshape(B, Q_BLOCK, A_HEADS * A_HEAD_DIM)

    out = lax.map(one_block, (jnp.arange(nb), to_blocks(q), to_blocks(qi), to_blocks(wi)))
    return out.swapaxes(0, 1).reshape(B, L, A_HEADS * A_HEAD_DIM)


def _rwkv7_step(state, inp):
    r, w, k, v, kk, a = inp
    sa = jnp.einsum('bhij,bhj->bhi', state, -kk)
    state = (state * w[:, :, None, :] + sa[..., None] * (kk * a)[:, :, None, :]
             + v[..., None] * k[:, :, None, :])
    return state, jnp.einsum('bhij,bhj->bhi', state, r)


def _rwkv7_mix(z, mu, w0, w_up, a0, a_up, g_up, k_k, k_a, r_k, ln_w, ln_b):
    B, L, _ = z.shape
    z = z.astype(jnp.float32)
    z = z + (_token_shift(z) - z) * mu
    r, k, v, wd, ad, gd = _split_cols(z, RWKV_SPLITS)
    w_log = -jax.nn.softplus(-(w0 + jnp.tanh(wd) @ w_up)) - 0.5
    decay = jnp.exp(-jnp.exp(w_log))
    a = jax.nn.sigmoid(a0 + ad @ a_up)
    g = jax.nn.sigmoid(gd) @ g_up

    def heads(t):
        return t.reshape(B, L, RWKV_HEADS, RWKV_HEAD)

    kk = heads(k * k_k)
    kk = kk * lax.rsqrt(jnp.maximum(jnp.sum(kk * kk, axis=-1, keepdims=True), 1e-24))
    k = k * (1.0 + (a - 1.0) * k_a)
    r_h, k_h, v_h, w_h, a_h = heads(r), heads(k), heads(v), heads(decay), heads(a)
    seq = tuple(t.swapaxes(0, 1) for t in (r_h, w_h, k_h, v_h, kk, a_h))
    s0 = jnp.zeros((B, RWKV_HEADS, RWKV_HEAD, RWKV_HEAD), jnp.float32)
    _, y = lax.scan(_rwkv7_step, s0, seq)
    y = y.swapaxes(0, 1)
    y = (_head_norm(y, RWKV_GN_EPS) * ln_w.reshape(RWKV_HEADS, RWKV_HEAD)
         + ln_b.reshape(RWKV_HEADS, RWKV_HEAD))
    y = y + jnp.sum(r_h * k_h * r_k, axis=-1, keepdims=True) * v_h
    return y.reshape(B, L, RWKV_DIM) * g


def _complex_combine(e1, e2):
    a1r, a1i, b1r, b1i = e1
    a2r, a2i, b2r, b2i = e2
    return (a2r * a1r - a2i * a1i,
            a2r * a1i + a2i * a1r,
            a2r * b1r - a2i * b1i + b2r,
            a2r * b1i + a2i * b1r + b2i)


def _s5_mix(u, lam_re, lam_im, log_step, b_re, b_im, c_re, c_im, d_skip, w_glu, b_glu):
    B, L, _ = u.shape
    u32 = u.astype(jnp.float32)
    ug = u32.reshape(B, L, S5_GROUPS, S5_GROUP)
    lr = jnp.minimum(lam_re.astype(jnp.float32), -1e-4)
    li = lam_im.astype(jnp.float32)
    step = jnp.exp(log_step.astype(jnp.float32))[:, None]
    mag = jnp.exp(lr * step)
    abar_r = mag * jnp.cos(li * step)
    abar_i = mag * jnp.sin(li * step)
    den = lr * lr + li * li
    cr = (lr * (abar_r - 1.0) + li * abar_i) / den
    ci = (lr * abar_i - li * (abar_r - 1.0)) / den
    bbar_r = cr[..., None] * b_re - ci[..., None] * b_im
    bbar_i = cr[..., None] * b_im + ci[..., None] * b_re
    bu_r = jnp.einsum('blgi,gpi->blgp', ug, bbar_r)
    bu_i = jnp.einsum('blgi,gpi->blgp', ug, bbar_i)
    ar = jnp.broadcast_to(abar_r, bu_r.shape)
    ai = jnp.broadcast_to(abar_i, bu_i.shape)
    _, _, xr, xi = lax.associative_scan(_complex_combine, (ar, ai, bu_r, bu_i), axis=1)
    y = jnp.einsum('blgp,gop->blgo', xr, c_re) - jnp.einsum('blgp,gop->blgo', xi, c_im)
    y = y.reshape(B, L, S5_DIM) + d_skip * u32
    zg = jax.nn.gelu(y)
    return zg * jax.nn.sigmoid(zg @ w_glu + b_glu)


def _retention_mix(q, k, v, gate):
    B, L, _ = q.shape
    nc = L // RET_CHUNK

    def heads(t):
        return t.astype(jnp.float32).reshape(B, L, RET_HEADS, RET_HEAD_DIM)

    inv = 1.0 / (RET_ROPE_BASE ** jnp.linspace(0.0, 1.0, RET_HEAD_DIM // 2, dtype=jnp.float32))
    q = _rotate(heads(q), inv)
    k = _rotate(heads(k), inv) * (RET_HEAD_DIM ** -0.5)
    v = heads(v)
    log_g = jnp.log(1.0 - 2.0 ** (-5.0 - jnp.arange(RET_HEADS, dtype=jnp.float32)))
    pos = jnp.arange(RET_CHUNK, dtype=jnp.float32)
    diff = pos[:, None] - pos[None, :]
    intra = jnp.where(diff >= 0, jnp.exp(jnp.maximum(diff, 0.0)[None] * log_g[:, None, None]), 0.0)
    xi = jnp.exp((pos + 1.0)[None, :] * log_g[:, None])
    zeta = jnp.exp((RET_CHUNK - 1.0 - pos)[None, :] * log_g[:, None])
    g_chunk = jnp.exp(RET_CHUNK * log_g)

    def chunks(t):
        return t.reshape(B, nc, RET_CHUNK, RET_HEADS, RET_HEAD_DIM).transpose(1, 0, 3, 2, 4)

    def step(state, inp):
        qc, kc, vc = inp
        att = jnp.einsum('bhqd,bhkd->bhqk', qc, kc) * intra
        o = (jnp.einsum('bhqk,bhkv->bhqv', att, vc)
             + jnp.einsum('bhqd,bhdv->bhqv', qc, state) * xi[None, :, :, None])
        state = (state * g_chunk[None, :, None, None]
                 + jnp.einsum('bhkd,bhkv->bhdv', kc * zeta[None, :, :, None], vc))
        return state, o

    s0 = jnp.zeros((B, RET_HEADS, RET_HEAD_DIM, RET_HEAD_DIM), jnp.float32)
    _, o = lax.scan(step, s0, (chunks(q), chunks(k), chunks(v)))
    o = o.transpose(1, 0, 3, 2, 4).reshape(B, L, RET_HEADS, RET_HEAD_DIM)
    o = _head_norm(o, RET_GN_EPS).reshape(B, L, RET_HEADS * RET_HEAD_DIM)
    return jax.nn.silu(gate.astype(jnp.float32)) * o


def _even_mixer(h, w_in, w_out, mu, w0, w_up, a0, a_up, g_up, k_k, k_a, r_k, ln_w, ln_b):
    B, L, _ = h.shape
    z = h @ w_in
    q, k, v, qi, ki, wi, zr = _split_cols(z, EVEN_SPLITS)
    inv_a = _partial_inv_freq(A_ROT)
    inv_i = _partial_inv_freq(IDX_ROT)
    q = _rotate(q.reshape(B, L, A_HEADS, A_HEAD_DIM), inv_a)
    k = _rotate(k.reshape(B, L, A_KV_HEADS, A_HEAD_DIM), inv_a)
    v = v.reshape(B, L, A_KV_HEADS, A_HEAD_DIM)
    qi = _rotate(qi.reshape(B, L, IDX_HEADS, IDX_DIM), inv_i)
    ki = _rotate(ki.reshape(B, L, 1, IDX_DIM), inv_i)[:, :, 0]
    o_a = _dsa_attention(q, k, v, qi, ki, wi)
    o_b = _rwkv7_mix(zr, mu, w0, w_up, a0, a_up, g_up, k_k, k_a, r_k, ln_w, ln_b)
    return jnp.concatenate([o_a, o_b], axis=-1).astype(h.dtype) @ w_out


def _odd_mixer(h, w_in, w_out, lam_re, lam_im, log_step, b_re, b_im, c_re, c_im,
               d_skip, w_glu, b_glu):
    z = h @ w_in
    u, rq, rk, rv, rg = _split_cols(z, ODD_SPLITS)
    o_c = _s5_mix(u, lam_re, lam_im, log_step, b_re, b_im, c_re, c_im, d_skip, w_glu, b_glu)
    o_d = _retention_mix(rq, rk, rv, rg)
    return jnp.concatenate([o_c, o_d], axis=-1).astype(h.dtype) @ w_out


def setup_inputs(seed: int = 0) -> dict:
    key = jax.random.key(seed)
    ks = iter(jax.random.split(key, 40))
    f32 = jnp.float32

    def nrm(shape, scale):
        return jax.random.normal(next(ks), shape, f32) * scale

    def uni(shape, lo, hi):
        return jax.random.uniform(next(ks), shape, f32, lo, hi)

    return {
        "x": nrm((BATCH, SEQ, D_MODEL), 1.0),
        "norm_mix": 1.0 + nrm((DEPTH, D_MODEL), 0.01),
        "norm_ffn": 1.0 + nrm((DEPTH, D_MODEL), 0.01),
        "ffn_gate": nrm((DEPTH, D_MODEL, D_FF), D_MODEL ** -0.5),
        "ffn_up": nrm((DEPTH, D_MODEL, D_FF), D_MODEL ** -0.5),
        "ffn_down": nrm((DEPTH, D_FF, D_MODEL), D_FF ** -0.5),
        "e_w_in": nrm((N_EVEN, D_MODEL, EVEN_IN), D_MODEL ** -0.5),
        "e_w_out": nrm((N_EVEN, MIX_WIDTH, D_MODEL), MIX_WIDTH ** -0.5),
        "e_mu": uni((N_EVEN, RWKV_COLS), 0.0, 1.0),
        "e_w0": uni((N_EVEN, RWKV_DIM), -6.5, -1.5),
        "e_w_up": nrm((N_EVEN, DECAY_RANK, RWKV_DIM), 0.5 * DECAY_RANK ** -0.5),
        "e_a0": nrm((N_EVEN, RWKV_DIM), 0.1),
        "e_a_up": nrm((N_EVEN, AAA_RANK, RWKV_DIM), 0.5 * AAA_RANK ** -0.5),
        "e_g_up": nrm((N_EVEN, GATE_RANK, RWKV_DIM), GATE_RANK ** -0.5),
        "e_k_k": 0.85 + nrm((N_EVEN, RWKV_DIM), 0.05),
        "e_k_a": 1.0 + nrm((N_EVEN, RWKV_DIM), 0.05),
        "e_r_k": nrm((N_EVEN, RWKV_HEADS, RWKV_HEAD), 0.1),
        "e_ln_w": 1.0 + nrm((N_EVEN, RWKV_DIM), 0.01),
        "e_ln_b": nrm((N_EVEN, RWKV_DIM), 0.01),
        "o_w_in": nrm((N_ODD, D_MODEL, ODD_IN), D_MODEL ** -0.5),
        "o_w_out": nrm((N_ODD, MIX_WIDTH, D_MODEL), MIX_WIDTH ** -0.5),
        "o_lam_re": -0.5 + nrm((N_ODD, S5_GROUPS, S5_STATE), 0.01),
        "o_lam_im": math.pi * jnp.arange(S5_STATE, dtype=f32) + nrm((N_ODD, S5_GROUPS, S5_STATE), 0.01),
        "o_log_step": uni((N_ODD, S5_GROUPS), math.log(S5_STEP_MIN), math.log(S5_STEP_MAX)),
        "o_b_re": nrm((N_ODD, S5_GROUPS, S5_STATE, S5_GROUP), S5_GROUP ** -0.5),
        "o_b_im": nrm((N_ODD, S5_GROUPS, S5_STATE, S5_GROUP), S5_GROUP ** -0.5),
        "o_c_re": nrm((N_ODD, S5_GROUPS, S5_GROUP, S5_STATE), 2.0 * S5_STATE ** -0.5),
        "o_c_im": nrm((N_ODD, S5_GROUPS, S5_GROUP, S5_STATE), 2.0 * S5_STATE ** -0.5),
        "o_d_skip": nrm((N_ODD, S5_DIM), 0.5),
        "o_w_glu": nrm((N_ODD, S5_DIM, S5_DIM), S5_DIM ** -0.5),
        "o_b_glu": nrm((N_ODD, S5_DIM), 0.01),
        "final_norm": 1.0 + nrm((D_MODEL,), 0.01),
    }


def reference(x, norm_mix, norm_ffn, ffn_gate, ffn_up, ffn_down,
              e_w_in, e_w_out, e_mu, e_w0, e_w_up, e_a0, e_a_up, e_g_up, e_k_k, e_k_a,
              e_r_k, e_ln_w, e_ln_b,
              o_w_in, o_w_out, o_lam_re, o_lam_im, o_log_step, o_b_re, o_b_im, o_c_re,
              o_c_im, o_d_skip, o_w_glu, o_b_glu,
              final_norm):
    h = x
    for layer in range(DEPTH):
        i = layer // 2
        hn = _rms_norm(h, norm_mix[layer])
        if layer % 2 == 0:
            mix = _even_mixer(hn, e_w_in[i], e_w_out[i], e_mu[i], e_w0[i], e_w_up[i], e_a0[i],
                              e_a_up[i], e_g_up[i], e_k_k[i], e_k_a[i], e_r_k[i], e_ln_w[i],
                              e_ln_b[i])
        else:
            mix = _odd_mixer(hn, o_w_in[i], o_w_out[i], o_lam_re[i], o_lam_im[i], o_log_step[i],
                             o_b_re[i], o_b_im[i], o_c_re[i], o_c_im[i], o_d_skip[i],
                             o_w_glu[i], o_b_glu[i])
        h = h + mix
        h = h + _swiglu(_rms_norm(h, norm_ffn[layer]), ffn_gate[layer], ffn_up[layer], ffn_down[layer])
    return _rms_norm(h, final_norm)
```

```python
import numpy as np
import concourse.bass as bass
import concourse.mybir as mybir
from concourse.bass_utils import run_bass_kernel_spmd

F32 = mybir.dt.float32
BF16 = mybir.dt.bfloat16
I32 = mybir.dt.int32
U32 = mybir.dt.uint32
AF = mybir.ActivationFunctionType
ALU = mybir.AluOpType
AX = mybir.AxisListType


class Prog:
    ENG = ("sync", "scalar", "vector", "gpsimd", "tensor")
    NDMA = 6

    def __init__(self, name="k"):
        self.nc = bass.Bass("TRN2", target_bir_lowering=False)
        self.ops = {e: [] for e in self.ENG}
        self.cnt = {}
        self.lastw = {}
        self.readers = {}
        self.waited = {e: {} for e in self.ENG}
        self.dma_rr = {e: 0 for e in self.ENG}
        self.dma_out = {}
        self.n_inst = 0
        self.tail_waits = []

    def dram(self, name, shape, dt=F32, kind="Internal"):
        return self.nc.dram_tensor(name, list(shape), dt, kind=kind).ap()

    def sb(self, name, shape, dt=F32):
        return self.nc.alloc_sbuf_tensor("sb_" + name, list(shape), dt).ap()

    def ps(self, name, shape, dt=F32):
        return self.nc.alloc_psum_tensor("pp_" + name, list(shape), dt).ap()

    def _need(self, eng, dep, waits):
        if dep is None:
            return
        sk, val = dep
        if self.waited[eng].get(sk, 0) >= val:
            return
        waits[sk] = max(waits.get(sk, 0), val)

    def op(self, eng, fn, reads=(), writes=(), dma=False, pe_acc=False, final=False):
        waits = {}
        reads = [k for k in reads if k is not None]
        writes = list(writes)
        if eng != "tensor":
            for k in reads:
                if isinstance(k, str) and "ps" in k and k not in writes:
                    writes.append(k)
        for k in reads:
            self._need(eng, self.lastw.get(k), waits)
        for k in writes:
            lw = self.lastw.get(k)
            if not (pe_acc and lw is not None and lw[0] == "tensor" and eng == "tensor"):
                self._need(eng, lw, waits)
            for rd in self.readers.get(k, ()):
                self._need(eng, rd, waits)
        if dma:
            slot = self.dma_rr[eng]
            self.dma_rr[eng] = (slot + 1) % self.NDMA
            sk = ("dma", eng, slot)
            prev = self.dma_out.get(sk)
            self._need(eng, prev, waits)
            inc = 16
        else:
            sk = eng
            inc = 1
        val = self.cnt.get(sk, 0) + inc
        self.cnt[sk] = val
        if dma:
            self.dma_out[sk] = (sk, val)
        for s, v in waits.items():
            self.waited[eng][s] = max(self.waited[eng].get(s, 0), v)
        self.ops[eng].append((fn, sorted(waits.items(), key=str), sk, inc))
        for k in writes:
            self.lastw[k] = (sk, val)
            self.readers[k] = []
        for k in reads:
            if k not in writes:
                self.readers.setdefault(k, []).append((sk, val))
        if final:
            self.tail_waits.append((sk, val))
        self.n_inst += 1
        return (sk, val)

    def dma(self, eng, out, in_, reads=(), writes=(), final=False, **kw):
        return self.op(eng, lambda e: e.dma_start(out=out, in_=in_, **kw), reads, writes, dma=True, final=final)

    def mm(self, out, lhsT, rhs, start, stop, reads=(), writes=()):
        return self.op("tensor", lambda e: e.matmul(out, lhsT, rhs, start=start, stop=stop),
                       reads, writes, pe_acc=True)

    def tr(self, out, in_, ident, reads=(), writes=()):
        return self.op("tensor", lambda e: e.transpose(out, in_, ident), reads, writes, pe_acc=True)

    def act(self, out, in_, func, reads=(), writes=(), eng="scalar", **kw):
        return self.op(eng, lambda e: e.activation(out, in_, func, **kw), reads, writes)

    def V(self, eng, meth, *args, reads=(), writes=(), **kw):
        return self.op(eng, lambda e: getattr(e, meth)(*args, **kw), reads, writes)

    def build(self):
        nc = self.nc
        semkeys = list(self.cnt.keys())
        sems = {}
        import contextlib
        with contextlib.ExitStack() as st:
            for i, sk in enumerate(semkeys):
                sems[sk] = st.enter_context(nc.semaphore("s%d" % i))
            block = st.enter_context(nc.Block())

            def emit(engname):
                def body(e):
                    for fn, waits, sk, inc in self.ops[engname]:
                        for s, v in waits:
                            e.wait_ge(sems[s], v)
                        fn(e).then_inc(sems[sk], inc)
                    if engname == "sync":
                        for s, v in self.tail_waits:
                            e.wait_ge(sems[s], v)
                return body
            block.sync(emit("sync"))
            block.scalar(emit("scalar"))
            block.vector(emit("vector"))
            block.gpsimd(emit("gpsimd"))
            block.tensor(emit("tensor"))
        return nc


def run(prog, in_maps, trace=False):
    nc = prog.build()
    res = run_bass_kernel_spmd(nc, in_maps, core_ids=list(range(len(in_maps))), trace=trace)
    return res


def dense(P, tag, XT, KT, T, W, N, epi, kchunk=32, cast_engs=("gpsimd", "vector"), nstage=2, npsum=2, xkey=None, ps_tiles=None, n_off=0):
    Wv = W.rearrange("(kt p) n -> p kt n", p=128)
    nk = (KT + kchunk - 1) // kchunk
    stg = [P.sb(f"{tag}_stg{i}", [128, kchunk, 128], F32) for i in range(nstage)]
    wbf = [P.sb(f"{tag}_wbf{i}", [128, kchunk, 128], BF16) for i in range(nstage)]
    if ps_tiles is None:
        ps_tiles = [P.ps(f"{tag}_ps{i}", [128, 512], F32) for i in range(npsum)]
    NT = (N + 127) // 128
    it = 0
    dq = ("sync", "scalar")
    for nt in range(NT):
        n0 = nt * 128
        nsz = min(128, N - n0)
        pi = nt % len(ps_tiles)
        pst = ps_tiles[pi]
        pskey = f"{tag}_ps{pi}"
        for kc in range(nk):
            k0 = kc * kchunk
            ksz = min(kchunk, KT - k0)
            si = it % nstage
            P.dma(dq[it % 2], stg[si][:, :ksz, :nsz], Wv[:, k0:k0 + ksz, n_off + n0:n_off + n0 + nsz],
                  writes=[f"{tag}_stg{si}"])
            ce = cast_engs[it % len(cast_engs)]
            P.V(ce, "tensor_copy", wbf[si][:, :ksz, :nsz], stg[si][:, :ksz, :nsz],
                reads=[f"{tag}_stg{si}"], writes=[f"{tag}_wbf{si}"])
            for kk in range(ksz):
                P.mm(pst[:nsz, :T], wbf[si][:, kk, :nsz], XT[:, k0 + kk, :T],
                     start=(kc == 0 and kk == 0), stop=(kc == nk - 1 and kk == ksz - 1),
                     reads=[f"{tag}_wbf{si}"] + ([xkey] if xkey else []), writes=[pskey])
            it += 1
        epi(nt, nsz, pst[:nsz, :T], pskey)


EI, EO = "ExternalInput", "ExternalOutput"

class Streamer:
    def __init__(self, P, kchunk, T):
        self.P = P; self.kc = kchunk; self.T = T
        self.stg = [P.sb(f"w_stg{i}", [128, kchunk, 128], F32) for i in range(2)]
        self.wbf = [P.sb(f"w_bf{i}", [128, kchunk, 128], BF16) for i in range(2)]
        self.it = 0

    def tile(self, W, KT, n0, nsz, XT, xkey, pst, pskey, T=None):
        P = self.P; T = T or self.T
        Wv = W.rearrange("(kt p) n -> p kt n", p=128)
        nk = (KT + self.kc - 1) // self.kc
        for kc in range(nk):
            k0 = kc * self.kc; ksz = min(self.kc, KT - k0)
            si = self.it % 2
            P.dma(("sync", "scalar")[self.it % 2], self.stg[si][:, :ksz, :nsz], Wv[:, k0:k0 + ksz, n0:n0 + nsz], writes=[f"w_stg{si}"])
            ce = ("gpsimd", "vector")[self.it % 2]
            P.V(ce, "tensor_copy", self.wbf[si][:, :ksz, :nsz], self.stg[si][:, :ksz, :nsz], reads=[f"w_stg{si}"], writes=[f"w_bf{si}"])
            for kk in range(ksz):
                P.mm(pst[:nsz, :T], self.wbf[si][:, kk, :nsz], XT[:, k0 + kk, :T], kc == 0 and kk == 0, kc == nk - 1 and kk == ksz - 1,
                     reads=[f"w_bf{si}", xkey], writes=[pskey])
            self.it += 1

def build_dense(cfg):
    P = Prog()
    D, FF, TC, TB = cfg["D"], cfg["FF"], cfg["TC"], cfg.get("TB", 512)
    mode = cfg["mode"]; NIN = cfg.get("NIN", 0); G = cfg.get("G", 0)
    DT, FT = D // 128, (FF + 127) // 128
    assert FF % 128 == 0
    NBLK = TC // TB
    T = P.sb
    hT_d = P.dram("hT", [D, TC], F32, kind=EI)
    ones = T("ones", [128, 128]); P.V("vector", "memset", ones, 1.0, writes=["ones"])
    S = Streamer(P, cfg.get("kchunk", 16), TB)
    PSA = [P.ps(f"psa{i}", [128, 512], F32) for i in range(2)]
    PSB = [P.ps(f"psb{i}", [128, 512], F32) for i in range(2)]
    PSS = P.ps("pss", [128, 512], F32)
    hn = T("hn", [128, DT, TB], BF16)
    rstd = T("rstd", [128, TB]); xt = [T(f"xt{i}", [128, TB]) for i in range(2)]; xsq = [T(f"xsq{i}", [128, TB]) for i in range(2)]
    ev = [T(f"ev{i}", [128, TB]) for i in range(2)]; ev2 = [T(f"evb{i}", [128, TB]) for i in range(2)]
    cnt = [0]

    def norm(src_d, skey, gam, gkey, tb, out_dram=None):
        cs = slice(tb * TB, (tb + 1) * TB)
        for kt in range(DT):
            i = cnt[0] % 2; cnt[0] += 1
            P.dma(("sync", "scalar")[i], xt[i], src_d[kt * 128:(kt + 1) * 128, cs], reads=[skey], writes=[f"xt{i}"])
            P.V("gpsimd", "tensor_tensor", xsq[i], xt[i], xt[i], ALU.mult, reads=[f"xt{i}"], writes=[f"xsq{i}"])
            P.mm(PSS[:, :TB], ones, xsq[i], kt == 0, kt == DT - 1, reads=["ones", f"xsq{i}"], writes=["pss"])
        P.act(rstd, PSS[:, :TB], AF.Sqrt, scale=1.0 / D, bias=epsb, reads=["pss", "epsb"], writes=["rstd"])
        P.V("vector", "reciprocal", rstd, rstd, reads=["rstd"], writes=["rstd"])
        for kt in range(DT):
            i = cnt[0] % 2; cnt[0] += 1
            P.dma(("sync", "scalar")[i], xt[i], src_d[kt * 128:(kt + 1) * 128, cs], reads=[skey], writes=[f"xt{i}"])
            if out_dram is None:
                P.V("vector", "scalar_tensor_tensor", hn[:, kt, :], xt[i], gam[:, kt:kt + 1], rstd, ALU.mult, ALU.mult,
                    reads=[f"xt{i}", gkey, "rstd"], writes=["hn"])
            else:
                P.V("vector", "scalar_tensor_tensor", ev[i], xt[i], gam[:, kt:kt + 1], rstd, ALU.mult, ALU.mult,
                    reads=[f"xt{i}", gkey, "rstd"], writes=[f"ev{i}"])
                P.dma("gpsimd", out_dram[kt * 128:(kt + 1) * 128, cs], ev[i], reads=[f"ev{i}"], final=True)

    epsb = T("epsb", [128, 1]); P.V("vector", "memset", epsb, 1e-6, writes=["epsb"])
    def loadvec(name, n):
        d = P.dram(name, [128, n], F32, kind=EI); t = T("s_" + name, [128, n]); P.dma("sync", t, d, writes=["s_" + name]); return t, "s_" + name

    def inproj(Wd, N, zT_d, tb):
        cs = slice(tb * TB, (tb + 1) * TB)
        for nt in range((N + 127) // 128):
            n0 = nt * 128; nsz = min(128, N - n0); pi = nt % 2
            S.tile(Wd, DT, n0, nsz, hn, "hn", PSA[pi], f"psa{pi}")
            P.act(ev[pi][:nsz], PSA[pi][:nsz, :TB], AF.Copy, reads=[f"psa{pi}"], writes=[f"ev{pi}"])
            P.dma("gpsimd", zT_d[n0:n0 + nsz, cs], ev[pi][:nsz], reads=[f"ev{pi}"], final=True)

    if mode == "in0":
        g_mix, gk = loadvec("g_mix", DT)
        Win = P.dram("w_in", [D, NIN], F32, kind=EI)
        zT_d = P.dram("zT", [NIN, TC], F32, kind=EO)
        for tb in range(NBLK):
            norm(hT_d, None, g_mix, gk, tb)
            inproj(Win, NIN, zT_d, tb)
        return P

    g_ffn, gfk = loadvec("g_ffn", DT)
    Wout = P.dram("w_out", [D, D], F32, kind=EI)
    Wg = P.dram("w_gate", [D, FF], F32, kind=EI); Wu = P.dram("w_up", [D, FF], F32, kind=EI); Wd = P.dram("w_down", [FF, D], F32, kind=EI)
    oT_d = P.dram("oT", [D, TC], F32, kind=EI)
    h1_d = P.dram("h1s", [D, TC], F32)
    oT = T("oT", [128, DT, TB], BF16)
    aT = T("aT", [128, FT, TB], BF16)
    if mode == "mid":
        g_mix, gk = loadvec("g_mix", DT)
        Win = P.dram("w_in", [D, NIN], F32, kind=EI)
        zT_d = P.dram("zT", [NIN, TC], F32, kind=EO)
        h2_d = P.dram("h2T", [D, TC], F32, kind=EO)
    else:
        g_fin, gfin = loadvec("g_fin", DT)
        Wglu = P.dram("w_glu", [G, G], F32, kind=EI)
        bglu, bgk = loadvec("b_glu", G // 128)
        h2_d = P.dram("h2s", [D, TC], F32)
        y_d = P.dram("yT", [D, TC], F32, kind=EO)
        zgb = aT[:, 0:G // 128, :]

    for tb in range(NBLK):
        cs = slice(tb * TB, (tb + 1) * TB)
        for kt in range(DT):
            i = cnt[0] % 2; cnt[0] += 1
            P.dma(("sync", "scalar")[i], xt[i], oT_d[kt * 128:(kt + 1) * 128, cs], writes=[f"xt{i}"])
            if mode == "last" and kt < G // 128:
                P.V("vector", "tensor_copy", zgb[:, kt, :], xt[i], reads=[f"xt{i}"], writes=["aT"])
            else:
                P.V("vector", "tensor_copy", oT[:, kt, :], xt[i], reads=[f"xt{i}"], writes=["oT"])
        if mode == "last":
            for nt in range(G // 128):
                pi = nt % 2
                S.tile(Wglu, G // 128, nt * 128, 128, zgb, "aT", PSA[pi], f"psa{pi}")
                P.act(ev[pi], PSA[pi][:, :TB], AF.Sigmoid, bias=bglu[:, nt:nt + 1], reads=[f"psa{pi}", bgk], writes=[f"ev{pi}"])
                i = cnt[0] % 2; cnt[0] += 1
                P.dma(("sync", "scalar")[i], xt[i], oT_d[nt * 128:(nt + 1) * 128, cs], writes=[f"xt{i}"])
                P.V("vector", "tensor_tensor", oT[:, nt, :], ev[pi], xt[i], ALU.mult, reads=[f"ev{pi}", f"xt{i}"], writes=["oT"])
        for nt in range(DT):
            pi = nt % 2
            S.tile(Wout, DT, nt * 128, 128, oT, "oT", PSA[pi], f"psa{pi}")
            i = cnt[0] % 2; cnt[0] += 1
            P.dma(("sync", "scalar")[i], xt[i], hT_d[nt * 128:(nt + 1) * 128, cs], writes=[f"xt{i}"])
            P.V("vector", "tensor_tensor", ev[pi], PSA[pi][:, :TB], xt[i], ALU.add, reads=[f"psa{pi}", f"xt{i}"], writes=[f"ev{pi}"])
            P.dma("gpsimd", h1_d[nt * 128:(nt + 1) * 128, cs], ev[pi], reads=[f"ev{pi}"], writes=["h1s"])
        norm(h1_d, "h1s", g_ffn, gfk, tb)
        for ft in range(FT):
            pi = ft % 2
            S.tile(Wg, DT, ft * 128, 128, hn, "hn", PSA[pi], f"psa{pi}")
            S.tile(Wu, DT, ft * 128, 128, hn, "hn", PSB[pi], f"psb{pi}")
            P.act(ev[pi], PSA[pi][:, :TB], AF.Silu, reads=[f"psa{pi}"], writes=[f"ev{pi}"])
            P.V("vector", "tensor_tensor", aT[:, ft, :], ev[pi], PSB[pi][:, :TB], ALU.mult, reads=[f"ev{pi}", f"psb{pi}"], writes=["aT"])
        dst = h2_d
        for nt in range(DT):
            pi = nt % 2
            S.tile(Wd, FT, nt * 128, 128, aT, "aT", PSA[pi], f"psa{pi}")
            i = cnt[0] % 2; cnt[0] += 1
            P.dma(("sync", "scalar")[i], xt[i], h1_d[nt * 128:(nt + 1) * 128, cs], reads=["h1s"], writes=[f"xt{i}"])
            P.V("vector", "tensor_tensor", ev2[pi], PSA[pi][:, :TB], xt[i], ALU.add, reads=[f"psa{pi}", f"xt{i}"], writes=[f"evb{pi}"])
            P.dma("gpsimd", dst[nt * 128:(nt + 1) * 128, cs], ev2[pi], reads=[f"evb{pi}"], writes=["h2"], final=(mode == "mid"))
        if mode == "mid":
            norm(h2_d, "h2", g_mix, gk, tb)
            inproj(Win, NIN, zT_d, tb)
        else:
            norm(h2_d, "h2", g_fin, gfin, tb, out_dram=y_d)
    return P


EI, EO = "ExternalInput", "ExternalOutput"

def rwkv_consts():
    ident = np.eye(128, dtype=np.float32)
    blk = np.zeros((128, 128), np.float32); blk[:64, :64] = 1; blk[64:, 64:] = 1
    su = np.triu(np.ones((64, 64), np.float32), 1)
    ui = np.triu(np.ones((64, 64), np.float32), 0)
    sl = su.T.copy()
    m = np.zeros((2, 64, 4, 2, 64), np.float32)
    for h2 in range(2):
        for g in range(2):
            m[h2, :, 0, g] = su; m[h2, :, 1, g] = ui; m[h2, :, 2, g] = sl; m[h2, :, 3, g] = np.eye(64)
    ind = np.zeros((128, 2), np.float32); ind[:64, 0] = 1; ind[64:, 1] = 1
    return {"c_ident": ident, "c_blk": blk, "c_masks": m.reshape(128, 8 * 64), "c_ind": ind}

def build_rwkv(L, TS=1024, stage=99):
    P = Prog()
    CH = 64
    NSEG = L // TS; NCH = TS // CH
    zin = {n: P.dram("z" + n, [2, 128, L + 1], F32, kind=EI) for n in "rkvg"}
    zw = P.dram("zw", [96, L + 1], F32, kind=EI)
    za = P.dram("za", [96, L + 1], F32, kind=EI)
    par_d = P.dram("par", [2, 128, 9], F32, kind=EI)
    parw_d = P.dram("parw", [96, 2], F32, kind=EI)
    wup_d = P.dram("wup", [96, 256], F32, kind=EI)
    aup_d = P.dram("aup", [96, 256], F32, kind=EI)
    gup_d = P.dram("gup", [2, 128, 256], F32, kind=EI)
    lnwb_d = P.dram("lnwb", [2, 256], F32, kind=EI)
    c_ident = P.dram("c_ident", [128, 128], F32, kind=EI)
    c_blk = P.dram("c_blk", [128, 128], F32, kind=EI)
    c_masks = P.dram("c_masks", [128, 8 * 64], F32, kind=EI)
    c_ind = P.dram("c_ind", [128, 2], F32, kind=EI)
    ob = P.dram("ob", [L, 256], F32, kind=EO)

    def T(name, shape, dt=F32):
        return P.sb(name, shape, dt)
    ident = T("ident", [128, 128]); blk = T("blk", [128, 128]); masks = T("masks", [128, 4, 2, 64]); ind = T("ind", [128, 2])
    par = [T(f"par{g}", [128, 9]) for g in range(2)]
    parw = T("parw", [96, 2]); wup = T("wup", [96, 256]); aup = T("aup", [96, 256])
    gup = [T(f"gup{g}", [128, 256]) for g in range(2)]
    lnw = T("lnw", [128, 2, 64]); lnb = T("lnb", [128, 2, 64])
    ones = T("ones", [128, 64])
    P.dma("sync", ident, c_ident, writes=["ident"]); P.dma("sync", blk, c_blk, writes=["blk"])
    P.dma("sync", masks.rearrange("p a g s -> p (a g s)"), c_masks, writes=["masks"]); P.dma("sync", ind, c_ind, writes=["ind"])
    for g in range(2):
        P.dma("scalar", par[g], par_d[g], writes=[f"par{g}"])
        P.dma("scalar", gup[g], gup_d[g], writes=[f"gup{g}"])
    P.dma("scalar", parw, parw_d, writes=["parw"]); P.dma("scalar", wup, wup_d, writes=["wup"]); P.dma("scalar", aup, aup_d, writes=["aup"])
    lv = lnwb_d.rearrange("a (g h v) -> a h g v", g=2, h=2)
    for h2 in range(2):
        P.dma("gpsimd", lnw[64 * h2:64 * h2 + 64], lv[0:1, h2].partition_broadcast(64), writes=["lnw"])
        P.dma("gpsimd", lnb[64 * h2:64 * h2 + 64], lv[1:2, h2].partition_broadcast(64), writes=["lnb"])
    P.V("vector", "memset", ones, 1.0, writes=["ones"])
    msu = masks[:, 0]; mui = masks[:, 1]; msl = masks[:, 2]; mid = masks[:, 3]

    inp = {n: [T(f"in_{n}{g}", [128, TS + 1]) for g in range(2)] for n in "rkvg"}
    in_w = T("in_w", [96, TS + 1]); in_a = T("in_a", [96, TS + 1])
    xr = [T(f"xr{g}", [128, TS]) for g in range(2)]
    xk = [T(f"xk{g}", [128, TS]) for g in range(2)]
    xv = [T(f"xv{g}", [128, TS]) for g in range(2)]
    sg = [T(f"sg{g}", [128, TS]) for g in range(2)]
    lw = [T(f"lw{g}", [128, TS]) for g in range(2)]
    aa = [T(f"aa{g}", [128, TS]) for g in range(2)]
    tkk = [T(f"tkk{g}", [128, TS]) for g in range(2)]
    ttm = [T(f"ttm{g}", [128, TS]) for g in range(2)]
    tpr = [T(f"tpr{g}", [128, TS]) for g in range(2)]
    tcs = [T(f"tcs{g}", [128, TS]) for g in range(2)]
    te1 = [T(f"te1{g}", [128, TS]) for g in range(2)]
    te2 = [T(f"te2{g}", [128, TS]) for g in range(2)]
    gC = [T(f"gC{g}", [128, NCH]) for g in range(2)]
    tw = T("tw", [96, TS]); xa = T("xa", [96, TS])
    ST = [T(f"ST{g}", [128, 64]) for g in range(2)]
    for g in range(2):
        P.V("vector", "memset", ST[g], 0.0, writes=[f"ST{g}"])
    def CT(name):
        return T(name, [128, 2, 64])
    cN = CT("cN"); cNT = CT("cNT"); cAak = CT("cAak"); cBbr = CT("cBbr"); cBkr = CT("cBkr")
    cP = [CT("cP0"), CT("cP1")]; cPT = [CT("cPT0"), CT("cPT1")]; cR = [CT("cR0"), CT("cR1")]
    cV = CT("cV"); cBt = CT("cBt"); cKt = CT("cKt"); cWT = CT("cWT"); cUT = CT("cUT"); cY = CT("cY")
    cYc = CT("cYc"); cSq = CT("cSq"); cYB = T("cYB", [64, 2, 64]); cOut = [T("cOut0", [64, 2, 2, 64]), T("cOut1", [64, 2, 2, 64])]
    st1 = T("st1", [128, 2]); st2 = T("st2", [128, 2]); st3 = T("st3", [128, 2]); bo = T("bo", [128, 2])
    PS = [P.ps(f"ps{i}", [128, 512], F32) for i in range(8)]
    def slot(b, i):
        return PS[b][:, i * 128:(i + 1) * 128], f"ps{b}"
    (pAab, kAab), (pAabT, kAabT), (pAak, kAak), (pBbr, kBbr) = [slot(0, i) for i in range(4)]
    (pBkr, kBkr), (ptV, ktV), (ptB, ktB), (ptK, ktK) = [slot(1, i) for i in range(4)]
    (pP, kP), (pPT, kPT) = slot(2, 0), slot(2, 1)
    (pR, kR) = slot(3, 0)
    (pWT, kWT) = slot(4, 0)
    (pUT, kUT) = slot(5, 0)
    (pYT, kYT) = slot(6, 0)
    pM, kM = PS[6][:, 256:512], "ps6"
    pBo, kBo = PS[6][:, 128:130], "ps6"
    (pS, kS) = slot(7, 0)

    MU_R, MU_K, MU_V, MU_G, W0, A0, KK, KA, RK = range(9)
    dq = ["sync", "scalar", "gpsimd"]
    for seg in range(NSEG):
        t0 = seg * TS
        qi = 0
        for n in "rkvg":
            for g in range(2):
                P.dma(dq[qi % 3], inp[n][g], zin[n][g, :, t0:t0 + TS + 1], writes=[f"in_{n}{g}"]); qi += 1
        P.dma("sync", in_w, zw[:, t0:t0 + TS + 1], writes=["in_w"])
        P.dma("scalar", in_a, za[:, t0:t0 + TS + 1], writes=["in_a"])

        def lerp(out, okey, src, skey, mu_ap, tmp, tkey, np_=128, eng="vector", mkey=None):
            P.V(eng, "tensor_tensor", tmp[:np_], src[:np_, 0:TS], src[:np_, 1:TS + 1], ALU.subtract, reads=[skey], writes=[tkey])
            P.V("vector", "scalar_tensor_tensor", out[:np_], tmp[:np_], mu_ap, src[:np_, 1:TS + 1], ALU.mult, ALU.add,
                reads=[tkey, skey, mkey], writes=[okey])
        for g in range(2):
            lerp(xr[g], f"xr{g}", inp["r"][g], f"in_r{g}", par[g][:, MU_R:MU_R + 1], ttm[g], f"ttm{g}", mkey=f"par{g}")
            lerp(xk[g], f"xk{g}", inp["k"][g], f"in_k{g}", par[g][:, MU_K:MU_K + 1], ttm[g], f"ttm{g}", mkey=f"par{g}")
            lerp(xv[g], f"xv{g}", inp["v"][g], f"in_v{g}", par[g][:, MU_V:MU_V + 1], ttm[g], f"ttm{g}", mkey=f"par{g}")
            lerp(sg[g], f"sg{g}", inp["g"][g], f"in_g{g}", par[g][:, MU_G:MU_G + 1], ttm[g], f"ttm{g}", mkey=f"par{g}")
            P.act(sg[g], sg[g], AF.Sigmoid, reads=[f"sg{g}"], writes=[f"sg{g}"])
        if stage == 0: return P
        lerp(tw, "tw", in_w, "in_w", parw[:, 0:1], ttm[0], "ttm0", np_=96, mkey="parw")
        P.act(tw, tw, AF.Tanh, reads=["tw"], writes=["tw"])
        lerp(xa, "xa", in_a, "in_a", parw[:, 1:2], ttm[1], "ttm1", np_=96, mkey="parw")
        if stage == 1: return P
        for g in range(2):
            for hf in range(TS // 512):
                cs_ = slice(hf * 512, hf * 512 + 512)
                P.mm(PS[0][:, :], wup[:, g * 128:(g + 1) * 128], tw[:, cs_], True, True, reads=["wup", "tw"], writes=["ps0"])
                P.act(lw[g][:, cs_], PS[0][:, :], AF.Sigmoid, bias=par[g][:, W0:W0 + 1], reads=["ps0", f"par{g}"], writes=[f"lw{g}"])
                P.mm(PS[1][:, :], aup[:, g * 128:(g + 1) * 128], xa[:, cs_], True, True, reads=["aup", "xa"], writes=["ps1"])
                P.act(aa[g][:, cs_], PS[1][:, :], AF.Sigmoid, bias=par[g][:, A0:A0 + 1], reads=["ps1", f"par{g}"], writes=[f"aa{g}"])
            P.V("gpsimd", "tensor_scalar", lw[g], lw[g], -float(np.exp(-0.5)), None, ALU.mult, reads=[f"lw{g}"], writes=[f"lw{g}"])
            P.V("vector", "tensor_scalar", tkk[g], xk[g], par[g][:, KK:KK + 1], None, ALU.mult, reads=[f"xk{g}", f"par{g}"], writes=[f"tkk{g}"])
            P.V("gpsimd", "tensor_tensor", ttm[g], tkk[g], tkk[g], ALU.mult, reads=[f"tkk{g}"], writes=[f"ttm{g}"])
            for hf in range(TS // 512):
                cs_ = slice(hf * 512, hf * 512 + 512)
                P.mm(PS[2][:, :], blk, ttm[g][:, cs_], True, True, reads=["blk", f"ttm{g}"], writes=["ps2"])
                P.V("vector", "tensor_scalar", tpr[g][:, cs_], PS[2][:, :], 1e-24, None, ALU.max, reads=["ps2"], writes=[f"tpr{g}"])
            P.act(tpr[g], tpr[g], AF.Sqrt, reads=[f"tpr{g}"], writes=[f"tpr{g}"])
            P.V("vector", "reciprocal", tpr[g], tpr[g], reads=[f"tpr{g}"], writes=[f"tpr{g}"])
            P.V("vector", "tensor_tensor", tkk[g], tkk[g], tpr[g], ALU.mult, reads=[f"tkk{g}", f"tpr{g}"], writes=[f"tkk{g}"])
            P.V("vector", "tensor_scalar", ttm[g], aa[g], 1.0, par[g][:, KA:KA + 1], ALU.subtract, ALU.mult, reads=[f"aa{g}", f"par{g}"], writes=[f"ttm{g}"])
            P.V("vector", "scalar_tensor_tensor", xk[g], ttm[g], 1.0, xk[g], ALU.add, ALU.mult, reads=[f"ttm{g}", f"xk{g}"], writes=[f"xk{g}"])
            P.V("vector", "scalar_tensor_tensor", tpr[g], xr[g], par[g][:, RK:RK + 1], xk[g], ALU.mult, ALU.mult, reads=[f"xr{g}", f"xk{g}", f"par{g}"], writes=[f"tpr{g}"])
            P.V("gpsimd", "tensor_tensor", aa[g], tkk[g], aa[g], ALU.mult, reads=[f"tkk{g}", f"aa{g}"], writes=[f"aa{g}"])
            for c in range(NCH):
                cc = slice(c * CH, (c + 1) * CH)
                P.V("vector", "tensor_tensor_scan", tcs[g][:, cc], ones[:, :], lw[g][:, cc], 0.0, ALU.mult, ALU.add, reads=[f"lw{g}", "ones"], writes=[f"tcs{g}"])
            P.act(te1[g], tcs[g], AF.Exp, reads=[f"tcs{g}"], writes=[f"te1{g}"])
            P.act(te2[g], tcs[g], AF.Exp, scale=-1.0, reads=[f"tcs{g}"], writes=[f"te2{g}"])
            P.V("gpsimd", "tensor_tensor", lw[g], tcs[g], lw[g], ALU.subtract, reads=[f"tcs{g}", f"lw{g}"], writes=[f"lw{g}"])
            P.act(lw[g], lw[g], AF.Exp, reads=[f"lw{g}"], writes=[f"lw{g}"])
            e1v = te1[g].rearrange("p (c s) -> p c s", s=CH)
            P.V("vector", "tensor_copy", gC[g], e1v[:, :, CH - 1], reads=[f"te1{g}"], writes=[f"gC{g}"])
            P.V("vector", "scalar_tensor_tensor", lw[g], tkk[g], -1.0, lw[g], ALU.mult, ALU.mult, reads=[f"tkk{g}", f"lw{g}"], writes=[f"lw{g}"])
            P.V("gpsimd", "tensor_tensor", xr[g], xr[g], te1[g], ALU.mult, reads=[f"xr{g}", f"te1{g}"], writes=[f"xr{g}"])
            P.V("vector", "tensor_tensor", tcs[g].rearrange("p (c s) -> p c s", s=CH), te2[g].rearrange("p (c s) -> p c s", s=CH),
                gC[g].unsqueeze(2).to_broadcast([128, NCH, CH]), ALU.mult, reads=[f"te2{g}", f"gC{g}"], writes=[f"tcs{g}"])
            P.V("gpsimd", "tensor_tensor", tkk[g], aa[g], te2[g], ALU.mult, reads=[f"aa{g}", f"te2{g}"], writes=[f"tkk{g}"])
            P.V("vector", "tensor_tensor", te2[g], xk[g], te2[g], ALU.mult, reads=[f"xk{g}", f"te2{g}"], writes=[f"te2{g}"])
            P.V("gpsimd", "tensor_tensor", aa[g], aa[g], tcs[g], ALU.mult, reads=[f"aa{g}", f"tcs{g}"], writes=[f"aa{g}"])
            P.V("vector", "tensor_tensor", tcs[g], xk[g], tcs[g], ALU.mult, reads=[f"xk{g}", f"tcs{g}"], writes=[f"tcs{g}"])
        if stage == 2: return P
        alb, rb, beb, kb, bet, kt = lw, xr, tkk, te2, aa, tcs
        kalb, krb, kbeb, kkb, kbet, kkt = "lw", "xr", "tkk", "te2", "aa", "tcs"

        for c in range(NCH):
            cg = seg * NCH + c
            cols = slice(c * CH, (c + 1) * CH)
            def F(x, h):
                g, h2 = h // 2, h % 2
                return x[g][64 * h2:64 * h2 + 64, cols]
            def RW(h):
                return slice(64 * (h % 2), 64 * (h % 2) + 64)
            def O(ps, h):
                return ps[RW(h), (h // 2) * 64:(h // 2) * 64 + 64]
            def C(t, h):
                return t[RW(h), h // 2, :]
            for h in range(4):
                g, h2 = h // 2, h % 2
                rows = RW(h)
                idb = ident[rows, 64 * h2:64 * h2 + 64]
                P.mm(O(pAab, h), F(beb, h), F(alb, h), True, True, reads=[f"{kbeb}{g}", f"{kalb}{g}"], writes=[kAab])
                P.mm(O(pAabT, h), F(alb, h), F(beb, h), True, True, reads=[f"{kbeb}{g}", f"{kalb}{g}"], writes=[kAabT])
                P.mm(O(pAak, h), F(kb, h), F(alb, h), True, True, reads=[f"{kkb}{g}", f"{kalb}{g}"], writes=[kAak])
                P.mm(O(pBbr, h), F(beb, h), F(rb, h), True, True, reads=[f"{kbeb}{g}", f"{krb}{g}"], writes=[kBbr])
            for h in range(4):
                g, h2 = h // 2, h % 2
                rows = RW(h)
                idb = ident[rows, 64 * h2:64 * h2 + 64]
                P.mm(O(pBkr, h), F(kb, h), F(rb, h), True, True, reads=[f"{kkb}{g}", f"{krb}{g}"], writes=[kBkr])
                P.mm(O(ptV, h), F(xv, h), idb, True, True, reads=[f"xv{g}", "ident"], writes=[ktV])
                P.mm(O(ptB, h), F(bet, h), idb, True, True, reads=[f"{kbet}{g}", "ident"], writes=[ktB])
                P.mm(O(ptK, h), F(kt, h), idb, True, True, reads=[f"{kkt}{g}", "ident"], writes=[ktK])
            if stage == 3: return P
            f2 = lambda t: t.rearrange("p g s -> p (g s)")
            P.V("vector", "tensor_tensor", f2(cN), pAab, f2(msu), ALU.mult, reads=[kAab, "masks"], writes=["cN"])
            P.V("vector", "tensor_tensor", f2(cNT), pAabT, f2(msl), ALU.mult, reads=[kAabT, "masks"], writes=["cNT"])
            P.V("vector", "tensor_tensor", f2(cAak), pAak, f2(msu), ALU.mult, reads=[kAak, "masks"], writes=["cAak"])
            P.V("vector", "tensor_tensor", f2(cBbr), pBbr, f2(mui), ALU.mult, reads=[kBbr, "masks"], writes=["cBbr"])
            P.V("vector", "tensor_tensor", f2(cBkr), pBkr, f2(mui), ALU.mult, reads=[kBkr, "masks"], writes=["cBkr"])
            P.act(f2(cV), ptV, AF.Copy, reads=[ktV], writes=["cV"])
            P.act(f2(cBt), ptB, AF.Copy, reads=[ktB], writes=["cBt"])
            P.act(f2(cKt), ptK, AF.Copy, reads=[ktK], writes=["cKt"])
            P.V("gpsimd", "tensor_tensor", f2(cR[0]), f2(cN), f2(mid), ALU.add, reads=["cN", "masks"], writes=["cR0"])
            if stage == 4: return P
            Pc, PTc, Rc = (cN, "cN"), (cNT, "cNT"), (cR[0], "cR0")
            for i in range(1, 6):
                nP = (cP[i % 2], f"cP{i%2}"); nPT = (cPT[i % 2], f"cPT{i%2}"); nR = (cR[i % 2], f"cR{i%2}")
                for h in range(4):
                    if i < 5:
                        P.mm(O(pP, h), C(PTc[0], h), C(Pc[0], h), True, True, reads=[PTc[1], Pc[1]], writes=[kP])
                    P.mm(O(pPT, h), C(Pc[0], h), C(PTc[0], h), True, True, reads=[PTc[1], Pc[1]], writes=[kPT])
                if i < 5:
                    P.act(f2(nP[0]), pP, AF.Copy, reads=[kP], writes=[nP[1]])
                P.act(f2(nPT[0]), pPT, AF.Copy, reads=[kPT], writes=[nPT[1]])
                for h in range(4):
                    P.mm(O(pR, h), C(nPT[0], h), C(Rc[0], h), True, True, reads=[nPT[1], Rc[1]], writes=[kR])
                P.V("vector", "tensor_tensor", f2(nR[0]), pR, f2(Rc[0]), ALU.add, reads=[kR, Rc[1]], writes=[nR[1]])
                Pc, PTc, Rc = nP, nPT, nR
            Tm = Rc
            if stage == 5: return P
            for h in range(4):
                g = h // 2
                P.mm(O(pWT, h), F(alb, h), ST[g][RW(h), :], True, False, reads=[f"{kalb}{g}", f"ST{g}"], writes=[kWT])
                P.mm(O(pWT, h), C(cAak, h), C(cV, h), False, True, reads=["cAak", "cV"], writes=[kWT])
            P.act(f2(cWT), pWT, AF.Copy, reads=[kWT], writes=["cWT"])
            for h in range(4):
                P.mm(O(pUT, h), C(Tm[0], h), C(cWT, h), True, True, reads=[Tm[1], "cWT"], writes=[kUT])
            P.V("vector", "tensor_scalar", f2(cUT), pUT, 1.0, None, ALU.mult, reads=[kUT], writes=["cUT"])
            for h in range(4):
                g = h // 2
                P.mm(O(pYT, h), F(rb, h), ST[g][RW(h), :], True, False, reads=[f"{krb}{g}", f"ST{g}"], writes=[kYT])
                P.mm(O(pYT, h), C(cBbr, h), C(cUT, h), False, False, reads=["cBbr", "cUT"], writes=[kYT])
                P.mm(O(pYT, h), C(cBkr, h), C(cV, h), False, True, reads=["cBkr", "cV"], writes=[kYT])
            P.act(f2(cY), pYT, AF.Copy, reads=[kYT], writes=["cY"])
            for h in range(4):
                P.mm(O(pS, h), C(cBt, h), C(cUT, h), True, False, reads=["cBt", "cUT"], writes=[kS])
                P.mm(O(pS, h), C(cKt, h), C(cV, h), False, True, reads=["cKt", "cV"], writes=[kS])
            for g in range(2):
                P.V("vector", "scalar_tensor_tensor", ST[g], ST[g], gC[g][:, c:c + 1], pS[:, g * 64:(g + 1) * 64], ALU.mult, ALU.add,
                    reads=[f"ST{g}", f"gC{g}", kS], writes=[f"ST{g}"])
            if stage == 6: return P
            B3 = lambda t: t.unsqueeze(2).to_broadcast([128, 2, 64])
            P.V("vector", "tensor_reduce", st1, cY, AX.X, ALU.add, reads=["cY"], writes=["st1"])
            P.V("vector", "tensor_scalar", st1, st1, 1.0 / 64, None, ALU.mult, reads=["st1"], writes=["st1"])
            P.V("vector", "tensor_tensor", cYc, cY, B3(st1), ALU.subtract, reads=["cY", "st1"], writes=["cYc"])
            P.V("gpsimd", "tensor_tensor", cSq, cYc, cYc, ALU.mult, reads=["cYc"], writes=["cSq"])
            P.V("vector", "tensor_reduce", st2, cSq, AX.X, ALU.add, reads=["cSq"], writes=["st2"])
            P.V("vector", "tensor_scalar", st2, st2, 1.0 / 64, 64e-5, ALU.mult, ALU.add, reads=["st2"], writes=["st2"])
            P.act(st2, st2, AF.Sqrt, reads=["st2"], writes=["st2"])
            P.V("vector", "reciprocal", st3, st2, reads=["st2"], writes=["st3"])
            P.V("vector", "tensor_tensor", cYc, cYc, B3(st3), ALU.mult, reads=["cYc", "st3"], writes=["cYc"])
            P.V("gpsimd", "tensor_tensor", cYc, cYc, lnw, ALU.mult, reads=["cYc", "lnw"], writes=["cYc"])
            P.V("gpsimd", "tensor_tensor", cYc, cYc, lnb, ALU.add, reads=["cYc", "lnb"], writes=["cYc"])
            for h in range(4):
                g = h // 2
                P.mm(pBo[RW(h), g:g + 1], F(tpr, h), ones[RW(h), 0:1], True, True, reads=[f"tpr{g}", "ones"], writes=[kBo])
            P.act(bo, pBo, AF.Copy, reads=[kBo], writes=["bo"])
            P.V("vector", "tensor_tensor", cSq, cV, B3(bo), ALU.mult, reads=["cV", "bo"], writes=["cSq"])
            P.V("gpsimd", "tensor_tensor", cYc, cYc, cSq, ALU.add, reads=["cYc", "cSq"], writes=["cYc"])
            P.dma("gpsimd", cYB, cYc[64:128], reads=["cYc"], writes=["cYB"])
            for g in range(2):
                P.mm(pM[:64, :], sg[g][:, cols], gup[g], g == 0, g == 1, reads=[f"sg{g}", f"gup{g}"], writes=[kM])
            co = cOut[cg % 2]; cok = f"cOut{cg%2}"
            pMv = pM[:64, :].rearrange("p (g h v) -> p g h v", g=2, h=2)
            P.V("vector", "tensor_tensor", co[:, :, 0, :], cYc[0:64], pMv[:, :, 0, :], ALU.mult, reads=["cYc", kM], writes=[cok])
            P.V("vector", "tensor_tensor", co[:, :, 1, :], cYB, pMv[:, :, 1, :], ALU.mult, reads=["cYB", kM], writes=[cok])
            P.dma("sync" if cg % 2 == 0 else "scalar", ob[cg * CH:(cg + 1) * CH, :], co.rearrange("p g h v -> p (g h v)"), reads=[cok], final=True)
    return P


EI, EO = "ExternalInput", "ExternalOutput"
NEG = -1.0e30

def dsa_consts():
    RA = np.zeros((128, 128), np.float32)
    for d in range(16):
        RA[d, d + 16] = -1.0; RA[d + 16, d] = 1.0
    RI = np.zeros((128, 128), np.float32)
    for b in (0, 64):
        for d in range(8):
            RI[b + d, b + d + 8] = -1.0; RI[b + d + 8, b + d] = 1.0
    return {"c_RAT": np.ascontiguousarray(RA.T), "c_RIT": np.ascontiguousarray(RI.T), "c_ident": np.eye(128, dtype=np.float32),
            "c_iota": np.tile(np.arange(512, dtype=np.float32)[None, :], (128, 1))}

def build_dsa(L, stage=99, idx_dt=None):
    IDT = F32
    P = Prog()
    J = L // 1024
    qT_d = P.dram("qT", [J, 16, 128, 128], F32, kind=EI)
    cosq_d = P.dram("cosq", [J, 32, 128], F32, kind=EI); sinq_d = P.dram("sinq", [J, 32, 128], F32, kind=EI)
    qiT_d = P.dram("qiT", [J, 16, 128, 128], F32, kind=EI)
    cosqi_d = P.dram("cosqi", [J, 16, 128], F32, kind=EI); sinqi_d = P.dram("sinqi", [J, 16, 128], F32, kind=EI)
    kT_d = P.dram("kT", [4, 128, L], F32, kind=EI)
    cosk_d = P.dram("cosk", [32, L], F32, kind=EI); sink_d = P.dram("sink", [32, L], F32, kind=EI)
    kiT_d = P.dram("kiT", [64, L], F32, kind=EI)
    coski_d = P.dram("coski", [16, L], F32, kind=EI); sinki_d = P.dram("sinki", [16, L], F32, kind=EI)
    v_d = P.dram("v", [L, 512], F32, kind=EI)
    wi_d = P.dram("wi", [J, 128, 32], F32, kind=EI)
    qpos_d = P.dram("qpos", [J, 128, 1], F32, kind=EI)
    RAT_d = P.dram("c_RAT", [128, 128], F32, kind=EI); RIT_d = P.dram("c_RIT", [128, 128], F32, kind=EI)
    ident_d = P.dram("c_ident", [128, 128], F32, kind=EI); iota_d = P.dram("c_iota", [128, 512], F32, kind=EI)
    oa_d = P.dram("oa", [J, 128, 2048], F32, kind=EO)
    kscr = P.dram("kscr", [4, 128, L], BF16)
    vscr = P.dram("vscr", [L, 512], BF16)

    T = P.sb
    RAT = T("RAT", [128, 128]); RIT = T("RIT", [128, 128]); identf = T("identf", [128, 128]); identb = T("identb", [128, 128], BF16)
    iota = T("iota", [128, 512])
    P.dma("sync", RAT, RAT_d, writes=["RAT"]); P.dma("sync", RIT, RIT_d, writes=["RIT"])
    P.dma("scalar", identf, ident_d, writes=["identf"]); P.dma("scalar", iota, iota_d, writes=["iota"])
    P.V("vector", "tensor_copy", identb, identf, reads=["identf"], writes=["identb"])
    PS = [P.ps(f"ps{i}", [128, 512], F32) for i in range(6)]
    PSB = [P.ps(f"psb{i}", [128, 1024], BF16) for i in range(2)]

    xin = [T(f"xin{i}", [128, 512]) for i in range(2)]
    tc_ = [T(f"tcos{i}", [128, 512]) for i in range(2)]
    tsn = [T(f"tsin{i}", [128, 512]) for i in range(2)]
    rt = T("rt", [128, 512]); ru = T("ru", [128, 512])
    xo = [T(f"xo{i}", [128, 512], BF16) for i in range(2)]
    cnt = [0]

    def rotary(src_ap, Tn, RT, rtkey, ranges, cos_ap, sin_ap, out_ap, okey, out_reads=(), tab_rows=None):
        i = cnt[0] % 2; cnt[0] += 1
        if isinstance(src_ap, list):
            for (r0, nr, ap_) in src_ap:
                P.dma("sync", xin[i][r0:r0 + nr, :Tn], ap_, writes=[f"xin{i}"])
        else:
            P.dma("sync", xin[i][:, :Tn], src_ap, writes=[f"xin{i}"])
        for (r0, nr) in ranges:
            P.dma("scalar", tc_[i][r0:r0 + nr, :Tn], cos_ap, writes=[f"tcos{i}"])
            P.dma("gpsimd", tsn[i][r0:r0 + nr, :Tn], sin_ap, writes=[f"tsin{i}"])
        pk = f"ps{i}"
        P.mm(PS[i][:, :Tn], RT, xin[i][:, :Tn], True, True, reads=[rtkey, f"xin{i}"], writes=[pk])
        P.act(out_ap, xin[i][:, :Tn], AF.Copy, reads=[f"xin{i}"] + list(out_reads), writes=[okey])
        for (r0, nr) in ranges:
            rs = slice(r0, r0 + nr)
            P.V("vector", "tensor_tensor", rt[rs, :Tn], PS[i][rs, :Tn], tsn[i][rs, :Tn], ALU.mult, reads=[pk, f"tsin{i}"], writes=["rt"])
            P.V("gpsimd", "tensor_tensor", ru[rs, :Tn], xin[i][rs, :Tn], tc_[i][rs, :Tn], ALU.mult, reads=[f"xin{i}", f"tcos{i}"], writes=["ru"])
            P.V("vector", "tensor_tensor", out_ap[rs], rt[rs, :Tn], ru[rs, :Tn], ALU.add, reads=["rt", "ru"], writes=[okey])

    kiT = T("kiT", [128, L], IDT)
    xoi = [T(f"xoi{i}", [128, 512], IDT) for i in range(2)]
    for kv in range(4):
        for ch in range(L // 512):
            cs = slice(ch * 512, ch * 512 + 512)
            i = cnt[0] % 2
            rotary(kT_d[kv, :, cs], 512, RAT, "RAT", [(0, 32)], cosk_d[:, cs], sink_d[:, cs], xo[i], f"xo{i}")
            P.dma("sync", kscr[kv, :, cs], xo[i], reads=[f"xo{i}"], writes=["kscr"])
    for ch in range(L // 512):
        cs = slice(ch * 512, ch * 512 + 512)
        i = cnt[0] % 2
        rotary([(0, 64, kiT_d[:, cs]), (64, 64, kiT_d[:, cs])], 512, RIT, "RIT", [(0, 16), (64, 16)], coski_d[:, cs], sinki_d[:, cs], xoi[i], f"xoi{i}")
        P.V("vector", "tensor_copy", kiT[:, cs], xoi[i], reads=[f"xoi{i}"], writes=["kiT"])
    NKMAX = L
    acc = T("acc", [128, NKMAX]); work = T("work", [128, NKMAX]); mb = T("mb", [128, NKMAX], BF16)
    pbf = T("pbf", [128, NKMAX], BF16)
    Kb = [T(f"Kb{i}", [128, NKMAX], BF16) for i in range(1)]
    Vb = [T(f"Vb{i}", [128, NKMAX // 128, 128], BF16) for i in range(1)]
    nvt = min(4, NKMAX // 512)
    vst = work[:, 0:nvt * 512].rearrange("p (t c) -> p t c", c=512)
    vsb = pbf[:, 0:nvt * 512].rearrange("p (t c) -> p t c", c=512)
    vr = nvt * 128
    for it in range(L // vr):
        P.dma("scalar", vst, v_d[it * vr:(it + 1) * vr, :].rearrange("(t p) c -> p t c", p=128), writes=["work"])
        P.V("gpsimd", "tensor_copy", vsb, vst, reads=["work"], writes=["pbf"])
        P.dma("scalar", vscr[it * vr:(it + 1) * vr, :].rearrange("(t p) c -> p t c", p=128), vsb, reads=["pbf"], writes=["vscr"])
    if stage == 0: return P

    QT = T("QT", [128, 16, 128], BF16); QI = T("QI", [128, 16, 128], IDT)
    wi = T("wi", [128, 32]); qpos = T("qpos", [128, 1]); qoff = T("qoff", [128, 2])
    rl = [T(f"rl{i}", [128, 512]) for i in range(2)]
    m8 = T("m8", [128, 8]); thr = T("thr", [128, 1]); mx = T("mx", [128, 1]); nmx = T("nmx", [128, 1]); rsum = T("rsum", [128, 1]); rinv = T("rinv", [128, 1])
    pT = [T(f"pT{i}", [128, 4, 128], BF16) for i in range(2)]
    obh = [T(f"obh{i}", [128, 128]) for i in range(2)]
    cbias = rt
    SC = float(32 ** -0.5 * 64 ** -0.5)

    for j in range(J):
        nk = 1024 * (j + 1)
        NCHK = nk // 512
        P.dma("sync", wi, wi_d[j], writes=["wi"]); P.dma("sync", qpos, qpos_d[j], writes=["qpos"])
        P.V("vector", "tensor_scalar", wi, wi, SC, None, ALU.mult, reads=["wi"], writes=["wi"])
        for h in range(16):
            rotary(qT_d[j, h], 128, RAT, "RAT", [(0, 32)], cosq_d[j], sinq_d[j], QT[:, h, :], "QT")
        for pr in range(16):
            rotary(qiT_d[j, pr], 128, RIT, "RIT", [(0, 16), (64, 16)], cosqi_d[j], sinqi_d[j], QI[:, pr, :], "QI")
        for hh in range(32):
            pr, h2 = hh // 2, hh % 2
            rows = slice(64 * h2, 64 * h2 + 64)
            for ck in range(NCHK):
                cs = slice(ck * 512, ck * 512 + 512)
                pi = 2 + (hh * NCHK + ck) % 2
                pk = f"ps{pi}"
                P.mm(PS[pi], QI[rows, pr, :], kiT[rows, cs], True, True, reads=["QI", "kiT"], writes=[pk])
                ri = (hh * NCHK + ck) % 2
                P.act(rl[ri], PS[pi], AF.Relu, reads=[pk], writes=[f"rl{ri}"])
                if hh == 0:
                    P.V("vector", "tensor_scalar", acc[:, cs], rl[ri], wi[:, 0:1], None, ALU.mult, reads=[f"rl{ri}", "wi"], writes=["acc"])
                else:
                    P.V("vector", "scalar_tensor_tensor", acc[:, cs], rl[ri], wi[:, hh:hh + 1], acc[:, cs], ALU.mult, ALU.add,
                        reads=[f"rl{ri}", "wi", "acc"], writes=["acc"])
        for t in range(2):
            off = nk - 1024 + t * 512
            P.V("vector", "tensor_scalar", qoff[:, t:t + 1], qpos, float(-off), None, ALU.add, reads=["qpos"], writes=["qoff"])
            P.V("vector", "tensor_scalar", cbias, iota, qoff[:, t:t + 1], NEG, ALU.is_gt, ALU.mult, reads=["iota", "qoff"], writes=["rt"])
            P.V("vector", "tensor_tensor", acc[:, off:off + 512], acc[:, off:off + 512], cbias, ALU.add, reads=["acc", "rt"], writes=["acc"])
        for r in range(32):
            src = acc if r == 0 else work
            sk = "acc" if r == 0 else "work"
            P.V("vector", "max", m8, src[:, :nk], reads=[sk], writes=["m8"])
            if r < 31:
                P.V("vector", "match_replace", work[:, :nk], m8, src[:, :nk], NEG, reads=["m8", sk], writes=["work"])
        P.V("vector", "tensor_scalar", thr, m8[:, 7:8], -1.0e29, None, ALU.max, reads=["m8"], writes=["thr"])
        P.V("vector", "tensor_scalar", mb[:, :nk], acc[:, :nk], thr, NEG, ALU.is_lt, ALU.mult, reads=["acc", "thr"], writes=["mb"])
        if stage == 1:
            P.dma("sync", oa_d[j, :, 0:1024], acc[:, nk - 1024:nk], reads=["acc"], final=True)
            return P
        for kv in range(4):
            bi = 0
            P.dma("sync", Kb[bi][:, :nk], kscr[kv, :, :nk], reads=["kscr"], writes=[f"Kb{bi}"])
            P.dma("scalar", Vb[bi][:, :nk // 128, :], vscr[0:nk, kv * 128:(kv + 1) * 128].rearrange("(t p) c -> p t c", p=128),
                  reads=["vscr"], writes=[f"Vb{bi}"])
            for hq in range(4):
                h = kv * 4 + hq
                for ck in range(NCHK):
                    cs = slice(ck * 512, ck * 512 + 512)
                    pi = 2 + ck % 2
                    pk = f"ps{pi}"
                    P.mm(PS[pi], QT[:, h, :], Kb[bi][:, cs], True, True, reads=["QT", f"Kb{bi}"], writes=[pk])
                    P.V("vector", "scalar_tensor_tensor", work[:, cs], PS[pi], float(128 ** -0.5), mb[:, cs], ALU.mult, ALU.add,
                        reads=[pk, "mb"], writes=["work"])
                P.V("vector", "reduce_max", mx, work[:, :nk], AX.X, reads=["work"], writes=["mx"])
                P.V("vector", "tensor_scalar", nmx, mx, -1.0, None, ALU.mult, reads=["mx"], writes=["nmx"])
                P.act(pbf[:, :nk], work[:, :nk], AF.Exp, bias=nmx, accum_out=rsum, reads=["work", "nmx"], writes=["pbf", "rsum"])
                P.V("vector", "reciprocal", rinv, rsum, reads=["rsum"], writes=["rinv"])
                NT4 = nk // 512
                for t4 in range(NT4):
                    bq = t4 % 2
                    for u in range(4):
                        kt = t4 * 4 + u
                        P.tr(PSB[bq][:, u * 128:(u + 1) * 128], pbf[:, kt * 128:(kt + 1) * 128], identb, reads=["pbf", "identb"], writes=[f"psb{bq}"])
                    if t4 % 2 == 0:
                        P.act(pT[bq].rearrange("p u q -> p (u q)"), PSB[bq][:, 0:512], AF.Copy, reads=[f"psb{bq}"], writes=[f"pT{bq}"])
                    else:
                        P.V("vector", "tensor_scalar", pT[bq].rearrange("p u q -> p (u q)"), PSB[bq][:, 0:512], 1.0, None, ALU.mult, reads=[f"psb{bq}"], writes=[f"pT{bq}"])
                    for u in range(4):
                        kt = t4 * 4 + u
                        P.mm(PS[4 + h % 2][:, 0:128], pT[bq][:, u, :], Vb[bi][:, kt, :], kt == 0, kt == nk // 128 - 1,
                             reads=[f"pT{bq}", f"Vb{bi}"], writes=[f"ps{4 + h % 2}"])
                P.V("vector", "tensor_scalar", obh[h % 2], PS[4 + h % 2][:, 0:128], rinv, None, ALU.mult, reads=[f"ps{4 + h % 2}", "rinv"], writes=[f"obh{h % 2}"])
                P.dma("gpsimd", oa_d[j, :, h * 128:(h + 1) * 128], obh[h % 2], reads=[f"obh{h % 2}"], final=True)
    return P


EI, EO = "ExternalInput", "ExternalOutput"
PI = float(np.pi)

def build_s5(L, C=256):
    P = Prog()
    NCK = L // C
    uT_d = P.dram("uT", [2, 128, L], F32, kind=EI)
    Bre_d = P.dram("Bre", [8, 128, 128], F32, kind=EI); Bim_d = P.dram("Bim", [8, 128, 128], F32, kind=EI)
    Cre_d = P.dram("Cre", [8, 128, 128], F32, kind=EI); Cim_d = P.dram("Cim", [8, 128, 128], F32, kind=EI)
    lam_d = P.dram("lam", [128, 8, 3], F32, kind=EI)
    dsk_d = P.dram("dsk", [128, 2], F32, kind=EI)
    iota_d = P.dram("c_iota", [128, C + 1], F32, kind=EI)
    zg_d = P.dram("zgT", [2, 128, L], F32, kind=EO)
    T = P.sb
    Bre = T("Bre", [128, 8, 128]); Bim = T("Bim", [128, 8, 128]); Cre = T("Cre", [128, 8, 128]); Cim = T("Cim", [128, 8, 128])
    lam = T("lam", [128, 8, 3]); dsk = T("dsk", [128, 2]); iota = T("iota", [128, C + 1])
    for r in range(8):
        P.dma("sync", Bre[:, r, :], Bre_d[r], writes=["Bre"]); P.dma("scalar", Bim[:, r, :], Bim_d[r], writes=["Bim"])
        P.dma("sync", Cre[:, r, :], Cre_d[r], writes=["Cre"]); P.dma("scalar", Cim[:, r, :], Cim_d[r], writes=["Cim"])
    P.dma("sync", lam, lam_d, writes=["lam"]); P.dma("sync", dsk, dsk_d, writes=["dsk"]); P.dma("sync", iota, iota_d, writes=["iota"])
    P.V("vector", "tensor_scalar", Cim.rearrange("p r c -> p (r c)"), Cim.rearrange("p r c -> p (r c)"), -1.0, None, ALU.mult, reads=["Cim"], writes=["Cim"])

    W = C + 1
    ki = T("ki", [128, W], I32); t1 = T("t1", [128, W]); t2 = T("t2", [128, W]); t3 = T("t3", [128, W])

    def sin_of(out, okey, ang, akey, w, shift):
        P.V("vector", "tensor_scalar", t1[:, :w], ang, shift, 1.0 / (2 * PI), ALU.add, ALU.mult, reads=[akey], writes=["t1"])
        P.V("vector", "tensor_copy", ki[:, :w], t1[:, :w], reads=["t1"], writes=["ki"])
        P.V("vector", "tensor_copy", t2[:, :w], ki[:, :w], reads=["ki"], writes=["t2"])
        P.V("vector", "tensor_scalar", t1[:, :w], ang, shift, None, ALU.add, reads=[akey], writes=["t1"])
        P.V("vector", "scalar_tensor_tensor", t1[:, :w], t2[:, :w], -2 * PI, t1[:, :w], ALU.mult, ALU.add, reads=["t2", "t1"], writes=["t1"])
        P.V("vector", "tensor_scalar", t2[:, :w], t1[:, :w], PI, -2 * PI, ALU.is_gt, ALU.mult, reads=["t1"], writes=["t2"])
        P.V("vector", "tensor_scalar", t3[:, :w], t1[:, :w], -PI, 2 * PI, ALU.is_lt, ALU.mult, reads=["t1"], writes=["t3"])
        P.V("vector", "tensor_tensor", t1[:, :w], t1[:, :w], t2[:, :w], ALU.add, reads=["t1", "t2"], writes=["t1"])
        P.V("vector", "tensor_tensor", t1[:, :w], t1[:, :w], t3[:, :w], ALU.add, reads=["t1", "t3"], writes=["t1"])
        P.act(out, t1[:, :w], AF.Sin, reads=["t1"], writes=[okey])

    NP = 16
    pp = T("pp", [128, 8, NP])
    LR, LI, ST, TH, MAG, CT, SN, AR, AI, DEN, CR, CI, COSC, SINC, TMP, TMP2 = range(16)
    cosT = T("cosT", [128, 8, W]); sinT = T("sinT", [128, 8, W]); Er = T("Er", [128, 8, C]); Ei = T("Ei", [128, 8, C])
    rmag = T("rmag", [128, 8, C]); ang = T("ang", [128, W])
    def pc(r, i):
        return pp[:, r, i:i + 1]
    K = ["pp"]
    for r in range(8):
        P.V("vector", "tensor_scalar", pc(r, LR), lam[:, r, 0:1], -1e-4, None, ALU.min, reads=["lam"], writes=K)
        P.V("vector", "tensor_copy", pc(r, LI), lam[:, r, 1:2], reads=["lam"], writes=K)
        P.act(pc(r, ST), lam[:, r, 2:3], AF.Exp, reads=["lam"], writes=K)
        P.V("vector", "tensor_tensor", pc(r, TH), pc(r, LI), pc(r, ST), ALU.mult, reads=K, writes=K)
        P.V("vector", "tensor_tensor", pc(r, TMP), pc(r, LR), pc(r, ST), ALU.mult, reads=K, writes=K)
        P.act(pc(r, MAG), pc(r, TMP), AF.Exp, reads=K, writes=K)
        sin_of(pc(r, SN), "pp", pc(r, TH), "pp", 1, 0.0)
        sin_of(pc(r, CT), "pp", pc(r, TH), "pp", 1, PI / 2)
        P.V("vector", "tensor_tensor", pc(r, AR), pc(r, MAG), pc(r, CT), ALU.mult, reads=K, writes=K)
        P.V("vector", "tensor_tensor", pc(r, AI), pc(r, MAG), pc(r, SN), ALU.mult, reads=K, writes=K)
        P.V("vector", "tensor_tensor", pc(r, DEN), pc(r, LR), pc(r, LR), ALU.mult, reads=K, writes=K)
        P.V("vector", "scalar_tensor_tensor", pc(r, DEN), pc(r, LI), pc(r, LI), pc(r, DEN), ALU.mult, ALU.add, reads=K, writes=K)
        P.V("vector", "reciprocal", pc(r, DEN), pc(r, DEN), reads=K, writes=K)
        P.V("vector", "tensor_scalar", pc(r, TMP), pc(r, AR), -1.0, None, ALU.add, reads=K, writes=K)
        P.V("vector", "tensor_tensor", pc(r, TMP2), pc(r, LI), pc(r, AI), ALU.mult, reads=K, writes=K)
        P.V("vector", "scalar_tensor_tensor", pc(r, CR), pc(r, TMP), pc(r, LR), pc(r, TMP2), ALU.mult, ALU.add, reads=K, writes=K)
        P.V("vector", "tensor_tensor", pc(r, CR), pc(r, CR), pc(r, DEN), ALU.mult, reads=K, writes=K)
        P.V("vector", "tensor_tensor", pc(r, TMP2), pc(r, LI), pc(r, TMP), ALU.mult, reads=K, writes=K)
        P.V("vector", "scalar_tensor_tensor", pc(r, CI), pc(r, AI), pc(r, LR), pc(r, TMP2), ALU.mult, ALU.subtract, reads=K, writes=K)
        P.V("vector", "tensor_tensor", pc(r, CI), pc(r, CI), pc(r, DEN), ALU.mult, reads=K, writes=K)
        P.V("vector", "tensor_scalar", ang, iota, pc(r, TH), None, ALU.mult, reads=["iota"] + K, writes=["ang"])
        sin_of(sinT[:, r, :], "sinT", ang, "ang", W, 0.0)
        sin_of(cosT[:, r, :], "cosT", ang, "ang", W, PI / 2)
        P.V("vector", "tensor_scalar", t1[:, :C], sinT[:, r, :C], pc(r, CI), None, ALU.mult, reads=["sinT"] + K, writes=["t1"])
        P.V("vector", "scalar_tensor_tensor", Er[:, r, :], cosT[:, r, :C], pc(r, CR), t1[:, :C], ALU.mult, ALU.add, reads=["cosT", "t1"] + K, writes=["Er"])
        P.V("vector", "tensor_scalar", t1[:, :C], sinT[:, r, :C], pc(r, CR), None, ALU.mult, reads=["sinT"] + K, writes=["t1"])
        P.V("vector", "scalar_tensor_tensor", Ei[:, r, :], cosT[:, r, :C], pc(r, CI), t1[:, :C], ALU.mult, ALU.subtract, reads=["cosT", "t1"] + K, writes=["Ei"])
        P.V("vector", "tensor_scalar", rmag[:, r, :], iota[:, :C], 0.0, pc(r, MAG), ALU.mult, ALU.add, reads=["iota"] + K, writes=["rmag"])
    wst = T("wst", [128, 8, 2]);
    P.V("vector", "memset", wst, 0.0, writes=["wst"])
    PSb = [P.ps(f"psb{i}", [128, 512], F32) for i in range(4)]
    PSy = [P.ps(f"psy{i}", [128, 512], F32) for i in range(2)]
    ub = [T(f"ub{i}", [128, C]) for i in range(2)]
    vr = T("vr", [128, C]); vi = T("vi", [128, C]); m1 = T("m1", [128, C]); m2 = T("m2", [128, C])
    wr = T("wr", [128, C]); wi_ = T("wi_", [128, C])
    xr = [T(f"xr{i}", [128, C]) for i in range(2)]; xi = [T(f"xi{i}", [128, C]) for i in range(2)]
    d1 = T("d1", [128, C]); d2 = T("d2", [128, C]); cw = T("cw", [128, 4])
    yb = T("yb", [128, C]); g1 = T("g1", [128, C]); g2 = T("g2", [128, C]); zo = [T(f"zo{i}", [128, C]) for i in range(2)]
    it = 0
    for cb in range(2):
        for ck in range(NCK):
            cs = slice(ck * C, (ck + 1) * C)
            ui = (cb * NCK + ck) % 2
            P.dma("sync", ub[ui], uT_d[cb, :, cs], writes=[f"ub{ui}"])
            yi = (cb * NCK + ck) % 2
            for rr in range(4):
                r = cb * 4 + rr
                pb = PSb[it % 4]; pbk = f"psb{it % 4}"
                P.mm(pb[:, 0:C], Bre[:, r, :], ub[ui], True, True, reads=["Bre", f"ub{ui}"], writes=[pbk])
                P.mm(pb[:, 256:256 + C], Bim[:, r, :], ub[ui], True, True, reads=["Bim", f"ub{ui}"], writes=[pbk])
                bur = pb[:, 0:C]; bui = pb[:, 256:256 + C]
                P.V("vector", "tensor_tensor", m1, bur, Er[:, r, :], ALU.mult, reads=[pbk, "Er"], writes=["m1"])
                P.V("vector", "tensor_tensor", m2, bui, Ei[:, r, :], ALU.mult, reads=[pbk, "Ei"], writes=["m2"])
                P.V("gpsimd", "tensor_tensor", vr, m1, m2, ALU.subtract, reads=["m1", "m2"], writes=["vr"])
                P.V("vector", "tensor_tensor", m1, bui, Er[:, r, :], ALU.mult, reads=[pbk, "Er"], writes=["m1"])
                P.V("vector", "tensor_tensor", m2, bur, Ei[:, r, :], ALU.mult, reads=[pbk, "Ei"], writes=["m2"])
                P.V("gpsimd", "tensor_tensor", vi, m1, m2, ALU.add, reads=["m1", "m2"], writes=["vi"])
                P.V("vector", "tensor_tensor_scan", wr, rmag[:, r, :], vr, wst[:, r, 0:1], ALU.mult, ALU.add, reads=["rmag", "vr", "wst"], writes=["wr"])
                P.V("vector", "tensor_tensor_scan", wi_, rmag[:, r, :], vi, wst[:, r, 1:2], ALU.mult, ALU.add, reads=["rmag", "vi", "wst"], writes=["wi_"])
                P.V("vector", "tensor_tensor", cw[:, 0:1], wr[:, C - 1:C], cosT[:, r, C:C + 1], ALU.mult, reads=["wr", "cosT"], writes=["cw"])
                P.V("vector", "tensor_tensor", cw[:, 1:2], wi_[:, C - 1:C], sinT[:, r, C:C + 1], ALU.mult, reads=["wi_", "sinT"], writes=["cw"])
                P.V("vector", "tensor_tensor", cw[:, 2:3], wr[:, C - 1:C], sinT[:, r, C:C + 1], ALU.mult, reads=["wr", "sinT"], writes=["cw"])
                P.V("vector", "tensor_tensor", cw[:, 3:4], wi_[:, C - 1:C], cosT[:, r, C:C + 1], ALU.mult, reads=["wi_", "cosT"], writes=["cw"])
                P.V("vector", "tensor_tensor", wst[:, r, 0:1], cw[:, 0:1], cw[:, 1:2], ALU.subtract, reads=["cw"], writes=["wst"])
                P.V("vector", "tensor_tensor", wst[:, r, 1:2], cw[:, 2:3], cw[:, 3:4], ALU.add, reads=["cw"], writes=["wst"])
                xi_ = it % 2
                P.V("gpsimd", "tensor_tensor", d1, wr, cosT[:, r, :C], ALU.mult, reads=["wr", "cosT"], writes=["d1"])
                P.V("gpsimd", "tensor_tensor", d2, wi_, sinT[:, r, :C], ALU.mult, reads=["wi_", "sinT"], writes=["d2"])
                P.V("gpsimd", "tensor_tensor", xr[xi_], d1, d2, ALU.subtract, reads=["d1", "d2"], writes=[f"xr{xi_}"])
                P.V("gpsimd", "tensor_tensor", d1, wr, sinT[:, r, :C], ALU.mult, reads=["wr", "sinT"], writes=["d1"])
                P.V("vector", "tensor_tensor", d2, wi_, cosT[:, r, :C], ALU.mult, reads=["wi_", "cosT"], writes=["d2"])
                P.V("gpsimd", "tensor_tensor", xi[xi_], d1, d2, ALU.add, reads=["d1", "d2"], writes=[f"xi{xi_}"])
                P.mm(PSy[yi][:, 0:C], Cre[:, r, :], xr[xi_], rr == 0, False, reads=["Cre", f"xr{xi_}"], writes=[f"psy{yi}"])
                P.mm(PSy[yi][:, 0:C], Cim[:, r, :], xi[xi_], False, rr == 3, reads=["Cim", f"xi{xi_}"], writes=[f"psy{yi}"])
                it += 1
            P.V("vector", "scalar_tensor_tensor", yb, ub[ui], dsk[:, cb:cb + 1], PSy[yi][:, 0:C], ALU.mult, ALU.add, reads=[f"ub{ui}", "dsk", f"psy{yi}"], writes=["yb"])
            P.V("gpsimd", "tensor_tensor", g1, yb, yb, ALU.mult, reads=["yb"], writes=["g1"])
            P.V("vector", "tensor_scalar", g1, g1, 0.044715, 1.0, ALU.mult, ALU.add, reads=["g1"], writes=["g1"])
            P.V("gpsimd", "tensor_tensor", g1, g1, yb, ALU.mult, reads=["g1", "yb"], writes=["g1"])
            P.act(g2, g1, AF.Tanh, scale=float(np.sqrt(2.0 / np.pi)), reads=["g1"], writes=["g2"])
            P.V("vector", "tensor_scalar", g2, g2, 1.0, 0.5, ALU.add, ALU.mult, reads=["g2"], writes=["g2"])
            zi = (cb * NCK + ck) % 2
            P.V("gpsimd", "tensor_tensor", zo[zi], g2, yb, ALU.mult, reads=["g2", "yb"], writes=[f"zo{zi}"])
            P.dma("scalar", zg_d[cb, :, cs], zo[zi], reads=[f"zo{zi}"], final=True)
    return P


EI, EO = "ExternalInput", "ExternalOutput"

def ret_consts(head):
    log_g = np.log(1.0 - 2.0 ** (-5.0 - np.float32(head))).astype(np.float32)
    pos = np.arange(128, dtype=np.float32)
    diff = pos[None, :] - pos[:, None]
    intraT = np.where(diff >= 0, np.exp(np.maximum(diff, 0.0) * log_g), 0.0).astype(np.float32)
    xi = np.exp((pos + 1.0) * log_g).astype(np.float32)
    zeta = np.exp((127.0 - pos) * log_g).astype(np.float32)
    gch = np.exp(128.0 * log_g).astype(np.float32)
    return {"c_intraT": intraT, "c_xi": np.tile(xi[None, :], (128, 1)), "c_zg": np.stack([zeta, np.full(128, gch, np.float32)], 1).astype(np.float32),
            "c_ident": np.eye(128, dtype=np.float32)}

def build_ret(L):
    P = Prog()
    NC = L // 128
    qT_d = P.dram("qT", [2, 128, L], F32, kind=EI); kT_d = P.dram("kT", [2, 128, L], F32, kind=EI)
    cos_d = P.dram("cosr", [128, L], F32, kind=EI); sin_d = P.dram("sinr", [128, L], F32, kind=EI)
    v_d = P.dram("v", [L, 256], F32, kind=EI); g_d = P.dram("gate", [L, 256], F32, kind=EI)
    intraT_d = P.dram("c_intraT", [128, 128], F32, kind=EI); xi_d = P.dram("c_xi", [128, 128], F32, kind=EI)
    zg_d = P.dram("c_zg", [128, 2], F32, kind=EI); ident_d = P.dram("c_ident", [128, 128], F32, kind=EI)
    od_d = P.dram("od", [L, 256], F32, kind=EO)
    T = P.sb
    intraT = T("intraT", [128, 128]); xib = T("xib", [128, 128]); zg = T("zg", [128, 2]); identf = T("identf", [128, 128]); identb = T("identb", [128, 128], BF16)
    P.dma("sync", intraT, intraT_d, writes=["intraT"]); P.dma("sync", xib, xi_d, writes=["xib"]); P.dma("sync", zg, zg_d, writes=["zg"])
    P.dma("sync", identf, ident_d, writes=["identf"])
    P.V("vector", "tensor_copy", identb, identf, reads=["identf"], writes=["identb"])
    Sf = T("Sf", [128, 2, 256]); Sb = T("Sb", [128, 2, 256], BF16)
    P.V("vector", "memset", Sf, 0.0, writes=["Sf"]); P.V("vector", "memset", Sb, 0.0, writes=["Sb"])
    NB = 2
    qin = [T(f"qin{i}", [128, 2, 128]) for i in range(NB)]; kin = [T(f"kin{i}", [128, 2, 128]) for i in range(NB)]
    cs_ = [T(f"cs{i}", [128, 128]) for i in range(NB)]; sn_ = [T(f"sn{i}", [128, 128]) for i in range(NB)]
    vin = [T(f"vin{i}", [128, 256]) for i in range(NB)]; gin = [T(f"gin{i}", [128, 256]) for i in range(NB)]
    a1 = T("a1", [128, 128]); a2 = T("a2", [128, 128]); a3 = T("a3", [128, 128]); a4 = T("a4", [128, 128])
    QT = T("QT", [128, 2, 128], BF16); KT = T("KT", [128, 2, 128], BF16); QX = T("QX", [128, 2, 128], BF16); qf = T("qf", [128, 2, 128])
    Vb = T("Vb", [128, 256], BF16); attT = T("attT", [128, 128], BF16); Kz = T("Kz", [128, 256], BF16)
    osb = T("osb", [128, 256]); oc = T("oc", [128, 256]); sq = T("sq", [128, 256]); sgt = T("sgt", [128, 256])
    outb = [T(f"outb{i}", [128, 256]) for i in range(2)]
    s1 = T("s1", [128, 1]); s2 = T("s2", [128, 1]); nm = T("nm", [128, 1]); rstd = T("rstd", [128, 1])
    pA = P.ps("psA", [128, 512], F32); pO = P.ps("psO", [128, 512], F32); pS = [P.ps(f"psS{i}", [128, 512], F32) for i in range(2)]
    pT = P.ps("psT", [128, 1024], BF16)
    for c in range(NC):
        i = c % NB
        cs = slice(c * 128, (c + 1) * 128)
        P.dma("sync", qin[i], qT_d[:, :, cs].rearrange("a p t -> p a t"), writes=[f"qin{i}"])
        P.dma("scalar", kin[i], kT_d[:, :, cs].rearrange("a p t -> p a t"), writes=[f"kin{i}"])
        P.dma("gpsimd", cs_[i], cos_d[:, cs], writes=[f"cs{i}"]); P.dma("gpsimd", sn_[i], sin_d[:, cs], writes=[f"sn{i}"])
        P.dma("sync", vin[i], v_d[cs, :], writes=[f"vin{i}"]); P.dma("scalar", gin[i], g_d[cs, :], writes=[f"gin{i}"])
        def rot(xin, xkey, out_f32, okey, scale):
            P.V("vector", "tensor_tensor", a1, xin[:, 0, :], cs_[i], ALU.mult, reads=[xkey, f"cs{i}"], writes=["a1"])
            P.V("gpsimd", "tensor_tensor", a2, xin[:, 1, :], sn_[i], ALU.mult, reads=[xkey, f"sn{i}"], writes=["a2"])
            P.V("vector", "tensor_tensor", a3, xin[:, 0, :], sn_[i], ALU.mult, reads=[xkey, f"sn{i}"], writes=["a3"])
            P.V("gpsimd", "tensor_tensor", a4, xin[:, 1, :], cs_[i], ALU.mult, reads=[xkey, f"cs{i}"], writes=["a4"])
            if scale == 1.0:
                P.V("vector", "tensor_tensor", out_f32[:, 0, :], a1, a2, ALU.subtract, reads=["a1", "a2"], writes=[okey])
                P.V("gpsimd", "tensor_tensor", out_f32[:, 1, :], a3, a4, ALU.add, reads=["a3", "a4"], writes=[okey])
            else:
                P.V("vector", "scalar_tensor_tensor", out_f32[:, 0, :], a1, scale, a2, ALU.mult, ALU.subtract, reads=["a1", "a2"], writes=[okey])
        rot(qin[i], f"qin{i}", qf, "qf", 1.0)
        P.act(QT.rearrange("p a t -> p (a t)"), qf.rearrange("p a t -> p (a t)"), AF.Copy, reads=["qf"], writes=["QT"])
        P.V("vector", "tensor_tensor", QX, qf, xib.unsqueeze(1).to_broadcast([128, 2, 128]), ALU.mult, reads=["qf", "xib"], writes=["QX"])
        P.V("vector", "tensor_tensor", a1, kin[i][:, 0, :], cs_[i], ALU.mult, reads=[f"kin{i}", f"cs{i}"], writes=["a1"])
        P.V("gpsimd", "tensor_tensor", a2, kin[i][:, 1, :], sn_[i], ALU.mult, reads=[f"kin{i}", f"sn{i}"], writes=["a2"])
        P.V("vector", "tensor_tensor", a3, kin[i][:, 0, :], sn_[i], ALU.mult, reads=[f"kin{i}", f"sn{i}"], writes=["a3"])
        P.V("gpsimd", "tensor_tensor", a4, kin[i][:, 1, :], cs_[i], ALU.mult, reads=[f"kin{i}", f"cs{i}"], writes=["a4"])
        P.V("vector", "tensor_tensor", a1, a1, a2, ALU.subtract, reads=["a1", "a2"], writes=["a1"])
        P.V("gpsimd", "tensor_tensor", a3, a3, a4, ALU.add, reads=["a3", "a4"], writes=["a3"])
        P.act(KT[:, 0, :], a1, AF.Copy, scale=1.0 / 16, reads=["a1"], writes=["KT"])
        P.act(KT[:, 1, :], a3, AF.Copy, scale=1.0 / 16, reads=["a3"], writes=["KT"])
        P.V("gpsimd", "tensor_copy", Vb, vin[i], reads=[f"vin{i}"], writes=["Vb"])
        for dt in range(2):
            P.mm(pA[:, 0:128], KT[:, dt, :], QT[:, dt, :], dt == 0, dt == 1, reads=["KT", "QT"], writes=["psA"])
        P.V("vector", "tensor_tensor", attT, pA[:, 0:128], intraT, ALU.mult, reads=["psA", "intraT"], writes=["attT"])
        P.mm(pO[:, 0:256], attT, Vb, True, False, reads=["attT", "Vb"], writes=["psO"])
        for dt in range(2):
            P.mm(pO[:, 0:256], QX[:, dt, :], Sb[:, dt, :], False, dt == 1, reads=["QX", "Sb"], writes=["psO"])
        for dt in range(2):
            P.tr(pT[:, dt * 128:(dt + 1) * 128], KT[:, dt, :], identb, reads=["KT", "identb"], writes=["psT"])
        P.V("vector", "tensor_scalar", Kz, pT[:, 0:256], zg[:, 0:1], None, ALU.mult, reads=["psT", "zg"], writes=["Kz"])
        for dt in range(2):
            P.mm(pS[dt][:, 0:256], Kz[:, dt * 128:(dt + 1) * 128], Vb, True, True, reads=["Kz", "Vb"], writes=[f"psS{dt}"])
            P.V("vector", "scalar_tensor_tensor", Sf[:, dt, :], Sf[:, dt, :], zg[:, 1:2], pS[dt][:, 0:256], ALU.mult, ALU.add,
                reads=["Sf", "zg", f"psS{dt}", "psO"], writes=["Sf"])
        P.act(Sb.rearrange("p a t -> p (a t)"), Sf.rearrange("p a t -> p (a t)"), AF.Copy, reads=["Sf"], writes=["Sb"])
        P.act(osb, pO[:, 0:256], AF.Copy, accum_out=s1, reads=["psO"], writes=["osb", "s1"])
        P.V("vector", "tensor_scalar", nm, s1, -1.0 / 256, None, ALU.mult, reads=["s1"], writes=["nm"])
        P.V("vector", "tensor_scalar", oc, osb, nm, None, ALU.add, reads=["osb", "nm"], writes=["oc"])
        P.act(sq, oc, AF.Square, accum_out=s2, reads=["oc"], writes=["sq", "s2"])
        P.V("vector", "tensor_scalar", s2, s2, 1.0 / 256, 1e-5, ALU.mult, ALU.add, reads=["s2"], writes=["s2"])
        P.act(s2, s2, AF.Sqrt, reads=["s2"], writes=["s2"])
        P.V("vector", "reciprocal", rstd, s2, reads=["s2"], writes=["rstd"])
        P.act(sgt, gin[i], AF.Silu, reads=[f"gin{i}"], writes=["sgt"])
        ob = outb[c % 2]
        P.V("vector", "scalar_tensor_tensor", ob, oc, rstd, sgt, ALU.mult, ALU.mult, reads=["oc", "rstd", "sgt"], writes=[f"outb{c%2}"])
        P.dma("sync", od_d[cs, :], ob, reads=[f"outb{c%2}"], final=True)
    return P


D_MODEL = 4096; SEQ = 8192; D_FF = 11008
ROPE_THETA = 500000.0

def _vec(v):
    return np.ascontiguousarray(np.asarray(v, np.float32).reshape(-1, 128).T)

def _launch(P, ins):
    nc = P.build()
    res = run_bass_kernel_spmd(nc, ins, core_ids=list(range(len(ins))))
    return res.results

def _rot_tables(L, inv):
    ang = np.arange(L, dtype=np.float32)[:, None] * inv[None, :].astype(np.float32)
    c = np.cos(ang).astype(np.float32); s = np.sin(ang).astype(np.float32)
    return np.ascontiguousarray(np.concatenate([c, c], 1).T), np.ascontiguousarray(np.concatenate([s, s], 1).T)

def _inv_freq(rot):
    return (np.float32(ROPE_THETA) ** (-np.arange(0, rot, 2, dtype=np.float32) / np.float32(rot))).astype(np.float32)

def _dsa_blocks(c, L):
    return [8 * j + (c if j % 2 == 0 else 7 - c) for j in range(L // 1024)]

def _dsa_inputs(q0, k0, v0, qi0, ki0, wi0, c, L, tabs):
    (ca, sa), (ci, si) = tabs
    d = {}
    tok = [np.arange(b * 128, b * 128 + 128) for b in _dsa_blocks(c, L)]
    d["qT"] = np.ascontiguousarray(np.stack([q0[t].transpose(1, 2, 0) for t in tok]))
    d["cosq"] = np.ascontiguousarray(np.stack([ca[:, t] for t in tok])); d["sinq"] = np.ascontiguousarray(np.stack([sa[:, t] for t in tok]))
    d["qiT"] = np.ascontiguousarray(np.stack([qi0[t].transpose(1, 2, 0).reshape(16, 128, 128) for t in tok]))
    d["cosqi"] = np.ascontiguousarray(np.stack([ci[:, t] for t in tok])); d["sinqi"] = np.ascontiguousarray(np.stack([si[:, t] for t in tok]))
    d["wi"] = np.ascontiguousarray(np.stack([wi0[t] for t in tok]))
    d["qpos"] = np.ascontiguousarray(np.stack([t.astype(np.float32)[:, None] for t in tok]))
    return d

def _rwkv_inputs(zrT, prm, core, L):
    Dh = 2048
    def sec(off, n):
        a = zrT[off:off + n]
        return np.concatenate([np.zeros((n, 1), np.float32), a], axis=1)
    ch = slice(core * 256, core * 256 + 256)
    d = {}
    for i, nme in enumerate("rkv"):
        d["z" + nme] = np.ascontiguousarray(sec(i * Dh + core * 256, 256).reshape(2, 128, L + 1))
    d["zg"] = None
    mu = prm["e_mu"]
    cols = [mu[0:Dh][ch], mu[Dh:2 * Dh][ch], mu[2 * Dh:3 * Dh][ch], mu[3 * Dh + 192:3 * Dh + 448],
            prm["e_w0"][ch], prm["e_a0"][ch], prm["e_k_k"][ch], prm["e_k_a"][ch], prm["e_r_k"].reshape(-1)[ch]]
    d["par"] = np.ascontiguousarray(np.stack(cols, axis=1).reshape(2, 128, 9).astype(np.float32))
    d["wup"] = np.ascontiguousarray(prm["e_w_up"][:, ch]); d["aup"] = np.ascontiguousarray(prm["e_a_up"][:, ch])
    d["gup"] = np.ascontiguousarray(prm["e_g_up"][:, ch].reshape(2, 128, 256))
    d["lnwb"] = np.ascontiguousarray(np.stack([prm["e_ln_w"][ch], prm["e_ln_b"][ch]], axis=0))
    return d

def _s5_inputs(prm, c, C=256):
    d = {}
    Bre = np.zeros((8, 128, 128), np.float32); Bim = np.zeros_like(Bre); Cre = np.zeros_like(Bre); Cim = np.zeros_like(Bre)
    lam = np.zeros((128, 8, 3), np.float32)
    for r in range(8):
        for s in range(2):
            g = c * 16 + r * 2 + s
            gl = (r % 4) * 2 + s
            Bre[r, 16 * gl:16 * gl + 16, 64 * s:64 * s + 64] = prm["o_b_re"][g].T
            Bim[r, 16 * gl:16 * gl + 16, 64 * s:64 * s + 64] = prm["o_b_im"][g].T
            Cre[r, 64 * s:64 * s + 64, 16 * gl:16 * gl + 16] = prm["o_c_re"][g].T
            Cim[r, 64 * s:64 * s + 64, 16 * gl:16 * gl + 16] = prm["o_c_im"][g].T
            lam[64 * s:64 * s + 64, r, 0] = prm["o_lam_re"][g]; lam[64 * s:64 * s + 64, r, 1] = prm["o_lam_im"][g]
            lam[64 * s:64 * s + 64, r, 2] = prm["o_log_step"][g]
    d["Bre"] = Bre; d["Bim"] = Bim; d["Cre"] = Cre; d["Cim"] = Cim; d["lam"] = lam
    d["dsk"] = np.ascontiguousarray(prm["o_d_skip"][c * 256:(c + 1) * 256].reshape(2, 128).T)
    d["c_iota"] = np.tile(np.arange(C + 1, dtype=np.float32)[None, :], (128, 1))
    return d

def kernel(**inp):
    A = {k: np.asarray(v, np.float32) for k, v in inp.items()}
    L = SEQ; NCORE = 8; TC = L // NCORE
    x = A["x"][0]
    xT = np.ascontiguousarray(x.T)
    tsl = [slice(c * TC, (c + 1) * TC) for c in range(NCORE)]
    P = build_dense(dict(D=D_MODEL, FF=D_FF, TC=TC, TB=512, mode="in0", NIN=11808, kchunk=16))
    w_in0 = np.ascontiguousarray(A["e_w_in"][0]); gm0 = _vec(A["norm_mix"][0])
    r = _launch(P, [{"hT": np.ascontiguousarray(xT[:, tsl[c]]), "g_mix": gm0, "w_in": w_in0} for c in range(NCORE)])
    z0T = np.concatenate([r[c]["zT"] for c in range(NCORE)], axis=1)
    del r
    q0 = np.ascontiguousarray(z0T[0:2048].T).reshape(L, 16, 128)
    qi0 = np.ascontiguousarray(z0T[3072:5120].T).reshape(L, 32, 64)
    wi0 = np.ascontiguousarray(z0T[5184:5216].T)
    tabs = (_rot_tables(L, _inv_freq(32)), _rot_tables(L, _inv_freq(16)))
    common = {"kT": np.ascontiguousarray(z0T[2048:2560].reshape(4, 128, L)), "cosk": tabs[0][0], "sink": tabs[0][1],
              "kiT": np.ascontiguousarray(z0T[5120:5184]), "coski": tabs[1][0], "sinki": tabs[1][1],
              "v": np.ascontiguousarray(z0T[2560:3072].T)}
    common.update(dsa_consts())
    P = build_dsa(L)
    ins = []
    for c in range(NCORE):
        d = _dsa_inputs(q0, None, None, qi0, None, wi0, c, L, tabs); d.update(common); ins.append(d)
    r = _launch(P, ins)
    o_a = np.zeros((L, 2048), np.float32)
    for c in range(NCORE):
        for j, b in enumerate(_dsa_blocks(c, L)):
            o_a[b * 128:(b + 1) * 128] = r[c]["oa"][j]
    del r, ins, q0, qi0
    prm = {k: A[k][0] for k in ["e_mu", "e_w0", "e_w_up", "e_a0", "e_a_up", "e_g_up", "e_k_k", "e_k_a", "e_r_k", "e_ln_w", "e_ln_b"]}
    zrT = z0T[5216:11808]
    def pad(a):
        return np.concatenate([np.zeros((a.shape[0], 1), np.float32), a], axis=1)
    zw = np.ascontiguousarray(pad(zrT[6144:6240])); za = np.ascontiguousarray(pad(zrT[6240:6336]))
    zg = np.ascontiguousarray(pad(zrT[6336:6592]).reshape(2, 128, L + 1))
    mu = prm["e_mu"]
    parw = np.ascontiguousarray(np.stack([mu[6144:6240], mu[6240:6336]], axis=1))
    cst = rwkv_consts()
    ins = []
    for c in range(NCORE):
        d = _rwkv_inputs(zrT, prm, c, L); d["zg"] = zg; d["zw"] = zw; d["za"] = za; d["parw"] = parw; d.update(cst); ins.append(d)
    P = build_rwkv(L)
    r = _launch(P, ins)
    o_b = np.concatenate([r[c]["ob"] for c in range(NCORE)], axis=1)
    del r, ins, z0T
    oT = np.ascontiguousarray(np.concatenate([o_a, o_b], axis=1).T)
    P = build_dense(dict(D=D_MODEL, FF=D_FF, TC=TC, TB=512, mode="mid", NIN=10240, kchunk=16))
    wts = {"g_ffn": _vec(A["norm_ffn"][0]), "w_out": np.ascontiguousarray(A["e_w_out"][0]), "w_gate": np.ascontiguousarray(A["ffn_gate"][0]),
           "w_up": np.ascontiguousarray(A["ffn_up"][0]), "w_down": np.ascontiguousarray(A["ffn_down"][0]),
           "g_mix": _vec(A["norm_mix"][1]), "w_in": np.ascontiguousarray(A["o_w_in"][0])}
    ins = []
    for c in range(NCORE):
        d = {"hT": np.ascontiguousarray(xT[:, tsl[c]]), "oT": np.ascontiguousarray(oT[:, tsl[c]])}; d.update(wts); ins.append(d)
    r = _launch(P, ins)
    h2T = np.concatenate([r[c]["h2T"] for c in range(NCORE)], axis=1)
    z1T = np.concatenate([r[c]["zT"] for c in range(NCORE)], axis=1)
    del r, ins, wts, oT
    prm = {k: A[k][0] for k in ["o_lam_re", "o_lam_im", "o_log_step", "o_b_re", "o_b_im", "o_c_re", "o_c_im", "o_d_skip"]}
    ins = []
    for c in range(NCORE):
        d = _s5_inputs(prm, c); d["uT"] = np.ascontiguousarray(z1T[c * 256:(c + 1) * 256].reshape(2, 128, L)); ins.append(d)
    P = build_s5(L)
    r = _launch(P, ins)
    zgT = np.concatenate([r[c]["zgT"].reshape(256, L) for c in range(NCORE)], axis=0)
    del r, ins
    inv = (1.0 / (np.float32(10000.0) ** np.linspace(0.0, 1.0, 128, dtype=np.float32))).astype(np.float32)
    ang = np.arange(L, dtype=np.float32)[:, None] * inv[None, :]
    cosr = np.ascontiguousarray(np.cos(ang).astype(np.float32).T); sinr = np.ascontiguousarray(np.sin(ang).astype(np.float32).T)
    ins = []
    for c in range(NCORE):
        ch = slice(c * 256, (c + 1) * 256)
        d = {"qT": np.ascontiguousarray(z1T[2048:4096][ch].reshape(2, 128, L)), "kT": np.ascontiguousarray(z1T[4096:6144][ch].reshape(2, 128, L)),
             "v": np.ascontiguousarray(z1T[6144:8192][ch].T), "gate": np.ascontiguousarray(z1T[8192:10240][ch].T), "cosr": cosr, "sinr": sinr}
        d.update(ret_consts(c)); ins.append(d)
    P = build_ret(L)
    r = _launch(P, ins)
    odT = np.concatenate([np.ascontiguousarray(r[c]["od"].T) for c in range(NCORE)], axis=0)
    del r, ins, z1T
    oT = np.ascontiguousarray(np.concatenate([zgT, odT], axis=0))
    P = build_dense(dict(D=D_MODEL, FF=D_FF, TC=TC, TB=512, mode="last", G=2048, kchunk=16))
    wts = {"g_ffn": _vec(A["norm_ffn"][1]), "w_out": np.ascontiguousarray(A["o_w_out"][0]), "w_gate": np.ascontiguousarray(A["ffn_gate"][1]),
           "w_up": np.ascontiguousarray(A["ffn_up"][1]), "w_down": np.ascontiguousarray(A["ffn_down"][1]),
           "g_fin": _vec(A["final_norm"]), "w_glu": np.ascontiguousarray(A["o_w_glu"][0]), "b_glu": _vec(A["o_b_glu"][0])}
    ins = []
    for c in range(NCORE):
        d = {"hT": np.ascontiguousarray(h2T[:, tsl[c]]), "oT": np.ascontiguousarray(oT[:, tsl[c]])}; d.update(wts); ins.append(d)
    r = _launch(P, ins)
    yT = np.concatenate([r[c]["yT"] for c in range(NCORE)], axis=1)
    return np.ascontiguousarray(yT.T).reshape(1, L, D_MODEL).astype(np.float32)
```

```python
import numpy as np
import concourse.bass as bass
import concourse.mybir as mybir
from concourse.bass_utils import run_bass_kernel_spmd

F32 = mybir.dt.float32
BF16 = mybir.dt.bfloat16
I32 = mybir.dt.int32
U32 = mybir.dt.uint32
AF = mybir.ActivationFunctionType
ALU = mybir.AluOpType
AX = mybir.AxisListType


class Prog:
    ENG = ("sync", "scalar", "vector", "gpsimd", "tensor")
    NDMA = 6

    def __init__(self, name="k"):
        self.nc = bass.Bass("TRN2", target_bir_lowering=False)
        self.ops = {e: [] for e in self.ENG}
        self.cnt = {}
        self.lastw = {}
        self.readers = {}
        self.waited = {e: {} for e in self.ENG}
        self.dma_rr = {e: 0 for e in self.ENG}
        self.dma_out = {}
        self.n_inst = 0
        self.tail_waits = []

    def dram(self, name, shape, dt=F32, kind="Internal"):
        return self.nc.dram_tensor(name, list(shape), dt, kind=kind).ap()

    def sb(self, name, shape, dt=F32):
        return self.nc.alloc_sbuf_tensor("sb_" + name, list(shape), dt).ap()

    def ps(self, name, shape, dt=F32):
        return self.nc.alloc_psum_tensor("pp_" + name, list(shape), dt).ap()

    def _need(self, eng, dep, waits):
        if dep is None:
            return
        sk, val = dep
        if self.waited[eng].get(sk, 0) >= val:
            return
        waits[sk] = max(waits.get(sk, 0), val)

    def op(self, eng, fn, reads=(), writes=(), dma=False, pe_acc=False, final=False):
        waits = {}
        reads = [k for k in reads if k is not None]
        writes = list(writes)
        if eng != "tensor":
            for k in reads:
                if isinstance(k, str) and "ps" in k and k not in writes:
                    writes.append(k)
        for k in reads:
            self._need(eng, self.lastw.get(k), waits)
        for k in writes:
            lw = self.lastw.get(k)
            if not (pe_acc and lw is not None and lw[0] == "tensor" and eng == "tensor"):
                self._need(eng, lw, waits)
            for rd in self.readers.get(k, ()):
                self._need(eng, rd, waits)
        if dma:
            slot = self.dma_rr[eng]
            self.dma_rr[eng] = (slot + 1) % self.NDMA
            sk = ("dma", eng, slot)
            prev = self.dma_out.get(sk)
            self._need(eng, prev, waits)
            inc = 16
        else:
            sk = eng
            inc = 1
        val = self.cnt.get(sk, 0) + inc
        self.cnt[sk] = val
        if dma:
            self.dma_out[sk] = (sk, val)
        for s, v in waits.items():
            self.waited[eng][s] = max(self.waited[eng].get(s, 0), v)
        self.ops[eng].append((fn, sorted(waits.items(), key=str), sk, inc))
        for k in writes:
            self.lastw[k] = (sk, val)
            self.readers[k] = []
        for k in reads:
            if k not in writes:
                self.readers.setdefault(k, []).append((sk, val))
        if final:
            self.tail_waits.append((sk, val))
        self.n_inst += 1
        return (sk, val)

    def dma(self, eng, out, in_, reads=(), writes=(), final=False, **kw):
        return self.op(eng, lambda e: e.dma_start(out=out, in_=in_, **kw), reads, writes, dma=True, final=final)

    def mm(self, out, lhsT, rhs, start, stop, reads=(), writes=()):
        return self.op("tensor", lambda e: e.matmul(out, lhsT, rhs, start=start, stop=stop),
                       reads, writes, pe_acc=True)

    def tr(self, out, in_, ident, reads=(), writes=()):
        return self.op("tensor", lambda e: e.transpose(out, in_, ident), reads, writes, pe_acc=True)

    def act(self, out, in_, func, reads=(), writes=(), eng="scalar", **kw):
        return self.op(eng, lambda e: e.activation(out, in_, func, **kw), reads, writes)

    def V(self, eng, meth, *args, reads=(), writes=(), **kw):
        return self.op(eng, lambda e: getattr(e, meth)(*args, **kw), reads, writes)

    def build(self):
        nc = self.nc
        semkeys = list(self.cnt.keys())
        sems = {}
        import contextlib
        with contextlib.ExitStack() as st:
            for i, sk in enumerate(semkeys):
                sems[sk] = st.enter_context(nc.semaphore("s%d" % i))
            block = st.enter_context(nc.Block())

            def emit(engname):
                def body(e):
                    for fn, waits, sk, inc in self.ops[engname]:
                        for s, v in waits:
                            e.wait_ge(sems[s], v)
                        fn(e).then_inc(sems[sk], inc)
                    if engname == "sync":
                        for s, v in self.tail_waits:
                            e.wait_ge(sems[s], v)
                return body
            block.sync(emit("sync"))
            block.scalar(emit("scalar"))
            block.vector(emit("vector"))
            block.gpsimd(emit("gpsimd"))
            block.tensor(emit("tensor"))
        return nc


def run(prog, in_maps, trace=False):
    nc = prog.build()
    res = run_bass_kernel_spmd(nc, in_maps, core_ids=list(range(len(in_maps))), trace=trace)
    return res


def dense(P, tag, XT, KT, T, W, N, epi, kchunk=32, cast_engs=("gpsimd", "vector"), nstage=2, npsum=2, xkey=None, ps_tiles=None, n_off=0):
    Wv = W.rearrange("(kt p) n -> p kt n", p=128)
    nk = (KT + kchunk - 1) // kchunk
    stg = [P.sb(f"{tag}_stg{i}", [128, kchunk, 128], F32) for i in range(nstage)]
    wbf = [P.sb(f"{tag}_wbf{i}", [128, kchunk, 128], BF16) for i in range(nstage)]
    if ps_tiles is None:
        ps_tiles = [P.ps(f"{tag}_ps{i}", [128, 512], F32) for i in range(npsum)]
    NT = (N + 127) // 128
    it = 0
    dq = ("sync", "scalar")
    for nt in range(NT):
        n0 = nt * 128
        nsz = min(128, N - n0)
        pi = nt % len(ps_tiles)
        pst = ps_tiles[pi]
        pskey = f"{tag}_ps{pi}"
        for kc in range(nk):
            k0 = kc * kchunk
            ksz = min(kchunk, KT - k0)
            si = it % nstage
            P.dma(dq[it % 2], stg[si][:, :ksz, :nsz], Wv[:, k0:k0 + ksz, n_off + n0:n_off + n0 + nsz],
                  writes=[f"{tag}_stg{si}"])
            ce = cast_engs[it % len(cast_engs)]
            P.V(ce, "tensor_copy", wbf[si][:, :ksz, :nsz], stg[si][:, :ksz, :nsz],
                reads=[f"{tag}_stg{si}"], writes=[f"{tag}_wbf{si}"])
            for kk in range(ksz):
                P.mm(pst[:nsz, :T], wbf[si][:, kk, :nsz], XT[:, k0 + kk, :T],
                     start=(kc == 0 and kk == 0), stop=(kc == nk - 1 and kk == ksz - 1),
                     reads=[f"{tag}_wbf{si}"] + ([xkey] if xkey else []), writes=[pskey])
            it += 1
        epi(nt, nsz, pst[:nsz, :T], pskey)


EI, EO = "ExternalInput", "ExternalOutput"

class Streamer:
    NBUF = 4
    AHEAD = 3
    def __init__(self, P, kchunk, T):
        self.P = P; self.kc = kchunk; self.T = T
        self.stg = [P.sb(f"w_stg{i}", [128, kchunk, 128], F32) for i in range(self.NBUF)]
        self.wbf = [P.sb(f"w_bf{i}", [128, kchunk, 128], BF16) for i in range(self.NBUF)]
        self.it = 0

    def run(self, jobs):
        P = self.P; T = self.T
        Q = []
        for jb in jobs:
            nk = (jb["KT"] + self.kc - 1) // self.kc
            for kc in range(nk):
                Q.append((jb, kc, nk))
        base = self.it
        def load(q):
            jb, kc, nk = Q[q]
            Wv = jb["W"].rearrange("(kt p) n -> p kt n", p=128)
            k0 = kc * self.kc; ksz = min(self.kc, jb["KT"] - k0); nsz = jb["nsz"]; n0 = jb["n0"]
            si = (base + q) % self.NBUF
            if kc == 0 and jb.get("pre") is not None:
                jb["pre"]()
            P.dma("sync", self.stg[si][:, :ksz, :nsz], Wv[:, k0:k0 + ksz, n0:n0 + nsz], writes=[f"w_stg{si}"])
            ce = ("gpsimd", "vector")[(base + q) % 2]
            P.V(ce, "tensor_copy", self.wbf[si][:, :ksz, :nsz], self.stg[si][:, :ksz, :nsz], reads=[f"w_stg{si}"], writes=[f"w_bf{si}"])
        def mm(q):
            jb, kc, nk = Q[q]
            k0 = kc * self.kc; ksz = min(self.kc, jb["KT"] - k0); nsz = jb["nsz"]
            si = (base + q) % self.NBUF
            for kk in range(ksz):
                P.mm(jb["pst"][:nsz, :T], self.wbf[si][:, kk, :nsz], jb["XT"][:, k0 + kk, :T], kc == 0 and kk == 0, kc == nk - 1 and kk == ksz - 1,
                     reads=[f"w_bf{si}", jb["xkey"]], writes=[jb["pskey"]])
            if kc == nk - 1 and jb.get("epi") is not None:
                jb["epi"]()
        n = len(Q)
        for q in range(n + self.AHEAD):
            p = q - self.AHEAD
            if p >= 0:
                mm(p)
            if q < n:
                load(q)
        self.it += n

def build_dense(cfg):
    P = Prog()
    D, FF, TC, TB = cfg["D"], cfg["FF"], cfg["TC"], cfg.get("TB", 512)
    mode = cfg["mode"]; NIN = cfg.get("NIN", 0); G = cfg.get("G", 0)
    DT, FT = D // 128, (FF + 127) // 128
    assert FF % 128 == 0
    NBLK = TC // TB
    T = P.sb
    hT_d = P.dram("hT", [D, TC], F32, kind=EI)
    ones = T("ones", [128, 128]); P.V("vector", "memset", ones, 1.0, writes=["ones"])
    S = Streamer(P, cfg.get("kchunk", 8), TB)
    PSA = [P.ps(f"psa{i}", [128, 512], F32) for i in range(2)]
    PSB = [P.ps(f"psb{i}", [128, 512], F32) for i in range(2)]
    PSS = P.ps("pss", [128, 512], F32)
    hn = T("hn", [128, DT, TB], BF16)
    rstd = T("rstd", [128, TB]); xt = [T(f"xt{i}", [128, TB]) for i in range(2)]; xsq = [T(f"xsq{i}", [128, TB]) for i in range(2)]
    ev = [T(f"ev{i}", [128, TB]) for i in range(2)]; ev2 = [T(f"evb{i}", [128, TB]) for i in range(2)]
    rs = [T(f"rs{i}", [128, TB]) for i in range(2)]
    cnt = [0]

    def norm(src_d, skey, gam, gkey, tb, out_dram=None):
        cs = slice(tb * TB, (tb + 1) * TB)
        for kt in range(DT):
            i = cnt[0] % 2; cnt[0] += 1
            P.dma("sync", xt[i], src_d[kt * 128:(kt + 1) * 128, cs], reads=[skey], writes=[f"xt{i}"])
            P.V("gpsimd", "tensor_tensor", xsq[i], xt[i], xt[i], ALU.mult, reads=[f"xt{i}"], writes=[f"xsq{i}"])
            P.mm(PSS[:, :TB], ones, xsq[i], kt == 0, kt == DT - 1, reads=["ones", f"xsq{i}"], writes=["pss"])
        P.act(rstd, PSS[:, :TB], AF.Sqrt, scale=1.0 / D, bias=epsb, reads=["pss", "epsb"], writes=["rstd"])
        P.V("vector", "reciprocal", rstd, rstd, reads=["rstd"], writes=["rstd"])
        for kt in range(DT):
            i = cnt[0] % 2; cnt[0] += 1
            P.dma("sync", xt[i], src_d[kt * 128:(kt + 1) * 128, cs], reads=[skey], writes=[f"xt{i}"])
            if out_dram is None:
                P.V("vector", "scalar_tensor_tensor", hn[:, kt, :], xt[i], gam[:, kt:kt + 1], rstd, ALU.mult, ALU.mult,
                    reads=[f"xt{i}", gkey, "rstd"], writes=["hn"])
            else:
                P.V("vector", "scalar_tensor_tensor", ev[i], xt[i], gam[:, kt:kt + 1], rstd, ALU.mult, ALU.mult,
                    reads=[f"xt{i}", gkey, "rstd"], writes=[f"ev{i}"])
                P.dma("scalar", out_dram[kt * 128:(kt + 1) * 128, cs], ev[i], reads=[f"ev{i}"], final=True)

    epsb = T("epsb", [128, 1]); P.V("vector", "memset", epsb, 1e-6, writes=["epsb"])
    def loadvec(name, n):
        d = P.dram(name, [128, n], F32, kind=EI); t = T("s_" + name, [128, n]); P.dma("sync", t, d, writes=["s_" + name]); return t, "s_" + name

    def inproj(Wd, N, zT_d, tb):
        cs = slice(tb * TB, (tb + 1) * TB)
        jobs = []
        for nt in range((N + 127) // 128):
            n0 = nt * 128; nsz = min(128, N - n0); pi = nt % 2
            def epi(n0=n0, nsz=nsz, pi=pi):
                P.act(ev[pi][:nsz], PSA[pi][:nsz, :TB], AF.Copy, reads=[f"psa{pi}"], writes=[f"ev{pi}"])
                P.dma("scalar", zT_d[n0:n0 + nsz, cs], ev[pi][:nsz], reads=[f"ev{pi}"], final=True)
            jobs.append(dict(W=Wd, KT=DT, n0=n0, nsz=nsz, XT=hn, xkey="hn", pst=PSA[pi], pskey=f"psa{pi}", epi=epi))
        S.run(jobs)

    if mode == "in0":
        g_mix, gk = loadvec("g_mix", DT)
        Win = P.dram("w_in", [D, NIN], F32, kind=EI)
        zT_d = P.dram("zT", [NIN, TC], F32, kind=EO)
        for tb in range(NBLK):
            norm(hT_d, None, g_mix, gk, tb)
            inproj(Win, NIN, zT_d, tb)
        return P

    g_ffn, gfk = loadvec("g_ffn", DT)
    Wout = P.dram("w_out", [D, D], F32, kind=EI)
    Wg = P.dram("w_gate", [D, FF], F32, kind=EI); Wu = P.dram("w_up", [D, FF], F32, kind=EI); Wd = P.dram("w_down", [FF, D], F32, kind=EI)
    oT_d = P.dram("oT", [D, TC], F32, kind=EI)
    h1_d = P.dram("h1s", [D, TC], F32)
    oT = T("oT", [128, DT, TB], BF16)
    aT = T("aT", [128, FT, TB], BF16)
    if mode == "mid":
        g_mix, gk = loadvec("g_mix", DT)
        Win = P.dram("w_in", [D, NIN], F32, kind=EI)
        zT_d = P.dram("zT", [NIN, TC], F32, kind=EO)
        h2_d = P.dram("h2T", [D, TC], F32, kind=EO)
    else:
        g_fin, gfin = loadvec("g_fin", DT)
        Wglu = P.dram("w_glu", [G, G], F32, kind=EI)
        bglu, bgk = loadvec("b_glu", G // 128)
        h2_d = P.dram("h2s", [D, TC], F32)
        y_d = P.dram("yT", [D, TC], F32, kind=EO)
        zgb = aT[:, 0:G // 128, :]

    for tb in range(NBLK):
        cs = slice(tb * TB, (tb + 1) * TB)
        for kt in range(DT):
            i = cnt[0] % 2; cnt[0] += 1
            P.dma(("sync", "scalar")[i], xt[i], oT_d[kt * 128:(kt + 1) * 128, cs], writes=[f"xt{i}"])
            if mode == "last" and kt < G // 128:
                P.V("vector", "tensor_copy", zgb[:, kt, :], xt[i], reads=[f"xt{i}"], writes=["aT"])
            else:
                P.V("vector", "tensor_copy", oT[:, kt, :], xt[i], reads=[f"xt{i}"], writes=["oT"])
        if mode == "last":
            jobs = []
            for nt in range(G // 128):
                pi = nt % 2
                def pre(nt=nt, pi=pi):
                    P.dma("sync", rs[pi], oT_d[nt * 128:(nt + 1) * 128, cs], writes=[f"rs{pi}"])
                def epi(nt=nt, pi=pi):
                    P.act(ev[pi], PSA[pi][:, :TB], AF.Sigmoid, bias=bglu[:, nt:nt + 1], reads=[f"psa{pi}", bgk], writes=[f"ev{pi}"])
                    P.V("vector", "tensor_tensor", oT[:, nt, :], ev[pi], rs[pi], ALU.mult, reads=[f"ev{pi}", f"rs{pi}"], writes=["oT"])
                jobs.append(dict(W=Wglu, KT=G // 128, n0=nt * 128, nsz=128, XT=zgb, xkey="aT", pst=PSA[pi], pskey=f"psa{pi}", epi=epi, pre=pre))
            S.run(jobs)
        jobs = []
        for nt in range(DT):
            pi = nt % 2
            def pre(nt=nt, pi=pi):
                P.dma("sync", rs[pi], hT_d[nt * 128:(nt + 1) * 128, cs], writes=[f"rs{pi}"])
            def epi(nt=nt, pi=pi):
                P.V("vector", "tensor_tensor", ev[pi], PSA[pi][:, :TB], rs[pi], ALU.add, reads=[f"psa{pi}", f"rs{pi}"], writes=[f"ev{pi}"])
                P.dma("scalar", h1_d[nt * 128:(nt + 1) * 128, cs], ev[pi], reads=[f"ev{pi}"], writes=["h1s"])
            jobs.append(dict(W=Wout, KT=DT, n0=nt * 128, nsz=128, XT=oT, xkey="oT", pst=PSA[pi], pskey=f"psa{pi}", epi=epi, pre=pre))
        S.run(jobs)
        norm(h1_d, "h1s", g_ffn, gfk, tb)
        jobs = []
        for ft in range(FT):
            pi = ft % 2
            def epi_g(ft=ft, pi=pi):
                P.act(ev[pi], PSA[pi][:, :TB], AF.Silu, reads=[f"psa{pi}"], writes=[f"ev{pi}"])
            def epi(ft=ft, pi=pi):
                P.V("vector", "tensor_tensor", aT[:, ft, :], ev[pi], PSB[pi][:, :TB], ALU.mult, reads=[f"ev{pi}", f"psb{pi}"], writes=["aT"])
            jobs.append(dict(W=Wg, KT=DT, n0=ft * 128, nsz=128, XT=hn, xkey="hn", pst=PSA[pi], pskey=f"psa{pi}", epi=epi_g))
            jobs.append(dict(W=Wu, KT=DT, n0=ft * 128, nsz=128, XT=hn, xkey="hn", pst=PSB[pi], pskey=f"psb{pi}", epi=epi))
        S.run(jobs)
        dst = h2_d
        jobs = []
        for nt in range(DT):
            pi = nt % 2
            def pre(nt=nt, pi=pi):
                P.dma("sync", rs[pi], h1_d[nt * 128:(nt + 1) * 128, cs], reads=["h1s"], writes=[f"rs{pi}"])
            def epi(nt=nt, pi=pi):
                P.V("vector", "tensor_tensor", ev2[pi], PSA[pi][:, :TB], rs[pi], ALU.add, reads=[f"psa{pi}", f"rs{pi}"], writes=[f"evb{pi}"])
                P.dma("scalar", dst[nt * 128:(nt + 1) * 128, cs], ev2[pi], reads=[f"evb{pi}"], writes=["h2"], final=(mode == "mid"))
            jobs.append(dict(W=Wd, KT=FT, n0=nt * 128, nsz=128, XT=aT, xkey="aT", pst=PSA[pi], pskey=f"psa{pi}", epi=epi, pre=pre))
        S.run(jobs)
        if mode == "mid":
            norm(h2_d, "h2", g_mix, gk, tb)
            inproj(Win, NIN, zT_d, tb)
        else:
            norm(h2_d, "h2", g_fin, gfin, tb, out_dram=y_d)
    return P


EI, EO = "ExternalInput", "ExternalOutput"

def rwkv_consts():
    ident = np.eye(128, dtype=np.float32)
    blk = np.zeros((128, 128), np.float32); blk[:64, :64] = 1; blk[64:, 64:] = 1
    su = np.triu(np.ones((64, 64), np.float32), 1)
    ui = np.triu(np.ones((64, 64), np.float32), 0)
    sl = su.T.copy()
    m = np.zeros((2, 64, 4, 2, 64), np.float32)
    for h2 in range(2):
        for g in range(2):
            m[h2, :, 0, g] = su; m[h2, :, 1, g] = ui; m[h2, :, 2, g] = sl; m[h2, :, 3, g] = np.eye(64)
    ind = np.zeros((128, 2), np.float32); ind[:64, 0] = 1; ind[64:, 1] = 1
    return {"c_ident": ident, "c_blk": blk, "c_masks": m.reshape(128, 8 * 64), "c_ind": ind}

def build_rwkv(L, TS=1024, stage=99):
    P = Prog()
    CH = 64
    NSEG = L // TS; NCH = TS // CH
    zin = {n: P.dram("z" + n, [2, 128, L + 1], F32, kind=EI) for n in "rkvg"}
    zw = P.dram("zw", [96, L + 1], F32, kind=EI)
    za = P.dram("za", [96, L + 1], F32, kind=EI)
    par_d = P.dram("par", [2, 128, 9], F32, kind=EI)
    parw_d = P.dram("parw", [96, 2], F32, kind=EI)
    wup_d = P.dram("wup", [96, 256], F32, kind=EI)
    aup_d = P.dram("aup", [96, 256], F32, kind=EI)
    gup_d = P.dram("gup", [2, 128, 256], F32, kind=EI)
    lnwb_d = P.dram("lnwb", [2, 256], F32, kind=EI)
    c_ident = P.dram("c_ident", [128, 128], F32, kind=EI)
    c_blk = P.dram("c_blk", [128, 128], F32, kind=EI)
    c_masks = P.dram("c_masks", [128, 8 * 64], F32, kind=EI)
    c_ind = P.dram("c_ind", [128, 2], F32, kind=EI)
    ob = P.dram("ob", [L, 256], F32, kind=EO)

    def T(name, shape, dt=F32):
        return P.sb(name, shape, dt)
    ident = T("ident", [128, 128]); blk = T("blk", [128, 128]); masks = T("masks", [128, 4, 2, 64]); ind = T("ind", [128, 2])
    par = [T(f"par{g}", [128, 9]) for g in range(2)]
    parw = T("parw", [96, 2]); wup = T("wup", [96, 256]); aup = T("aup", [96, 256])
    gup = [T(f"gup{g}", [128, 256]) for g in range(2)]
    lnw = T("lnw", [128, 2, 64]); lnb = T("lnb", [128, 2, 64])
    ones = T("ones", [128, 64])
    P.dma("sync", ident, c_ident, writes=["ident"]); P.dma("sync", blk, c_blk, writes=["blk"])
    P.dma("sync", masks.rearrange("p a g s -> p (a g s)"), c_masks, writes=["masks"]); P.dma("sync", ind, c_ind, writes=["ind"])
    for g in range(2):
        P.dma("scalar", par[g], par_d[g], writes=[f"par{g}"])
        P.dma("scalar", gup[g], gup_d[g], writes=[f"gup{g}"])
    P.dma("scalar", parw, parw_d, writes=["parw"]); P.dma("scalar", wup, wup_d, writes=["wup"]); P.dma("scalar", aup, aup_d, writes=["aup"])
    lv = lnwb_d.rearrange("a (g h v) -> a h g v", g=2, h=2)
    for h2 in range(2):
        P.dma("gpsimd", lnw[64 * h2:64 * h2 + 64], lv[0:1, h2].partition_broadcast(64), writes=["lnw"])
        P.dma("gpsimd", lnb[64 * h2:64 * h2 + 64], lv[1:2, h2].partition_broadcast(64), writes=["lnb"])
    P.V("vector", "memset", ones, 1.0, writes=["ones"])
    msu = masks[:, 0]; mui = masks[:, 1]; msl = masks[:, 2]; mid = masks[:, 3]

    inp = {n: [T(f"in_{n}{g}", [128, TS + 1]) for g in range(2)] for n in "rkvg"}
    in_w = T("in_w", [96, TS + 1]); in_a = T("in_a", [96, TS + 1])
    xr = [T(f"xr{g}", [128, TS]) for g in range(2)]
    xk = [T(f"xk{g}", [128, TS]) for g in range(2)]
    xv = [T(f"xv{g}", [128, TS]) for g in range(2)]
    sg = [T(f"sg{g}", [128, TS]) for g in range(2)]
    lw = [T(f"lw{g}", [128, TS]) for g in range(2)]
    aa = [T(f"aa{g}", [128, TS]) for g in range(2)]
    tkk = [T(f"tkk{g}", [128, TS]) for g in range(2)]
    ttm = [T(f"ttm{g}", [128, TS]) for g in range(2)]
    tpr = [T(f"tpr{g}", [128, TS]) for g in range(2)]
    tcs = [T(f"tcs{g}", [128, TS]) for g in range(2)]
    te1 = [T(f"te1{g}", [128, TS]) for g in range(2)]
    te2 = [T(f"te2{g}", [128, TS]) for g in range(2)]
    gC = [T(f"gC{g}", [128, NCH]) for g in range(2)]
    tw = T("tw", [96, TS]); xa = T("xa", [96, TS])
    ST = [T(f"ST{g}", [128, 64]) for g in range(2)]
    for g in range(2):
        P.V("vector", "memset", ST[g], 0.0, writes=[f"ST{g}"])
    def CT(name):
        return T(name, [128, 2, 64])
    cN = CT("cN"); cNT = CT("cNT"); cAak = CT("cAak"); cBbr = CT("cBbr"); cBkr = CT("cBkr")
    cP = [CT("cP0"), CT("cP1")]; cPT = [CT("cPT0"), CT("cPT1")]; cR = [CT("cR0"), CT("cR1")]
    cV = CT("cV"); cBt = CT("cBt"); cKt = CT("cKt"); cWT = CT("cWT"); cUT = CT("cUT"); cY = CT("cY")
    cYc = CT("cYc"); cSq = CT("cSq"); cYB = T("cYB", [64, 2, 64]); cOut = [T("cOut0", [64, 2, 2, 64]), T("cOut1", [64, 2, 2, 64])]
    st1 = T("st1", [128, 2]); st2 = T("st2", [128, 2]); st3 = T("st3", [128, 2]); bo = T("bo", [128, 2])
    PS = [P.ps(f"ps{i}", [128, 512], F32) for i in range(8)]
    def slot(b, i):
        return PS[b][:, i * 128:(i + 1) * 128], f"ps{b}"
    (pAab, kAab), (pAabT, kAabT), (pAak, kAak), (pBbr, kBbr) = [slot(0, i) for i in range(4)]
    (pBkr, kBkr), (ptV, ktV), (ptB, ktB), (ptK, ktK) = [slot(1, i) for i in range(4)]
    (pP, kP), (pPT, kPT) = slot(2, 0), slot(2, 1)
    (pR, kR) = slot(3, 0)
    (pWT, kWT) = slot(4, 0)
    (pUT, kUT) = slot(5, 0)
    (pYT, kYT) = slot(6, 0)
    pM, kM = PS[6][:, 256:512], "ps6"
    pBo, kBo = PS[6][:, 128:130], "ps6"
    (pS, kS) = slot(7, 0)

    MU_R, MU_K, MU_V, MU_G, W0, A0, KK, KA, RK = range(9)
    dq = ["sync", "scalar", "gpsimd"]
    for seg in range(NSEG):
        t0 = seg * TS
        qi = 0
        for n in "rkvg":
            for g in range(2):
                P.dma(dq[qi % 3], inp[n][g], zin[n][g, :, t0:t0 + TS + 1], writes=[f"in_{n}{g}"]); qi += 1
        P.dma("sync", in_w, zw[:, t0:t0 + TS + 1], writes=["in_w"])
        P.dma("scalar", in_a, za[:, t0:t0 + TS + 1], writes=["in_a"])

        def lerp(out, okey, src, skey, mu_ap, tmp, tkey, np_=128, eng="vector", mkey=None):
            P.V(eng, "tensor_tensor", tmp[:np_], src[:np_, 0:TS], src[:np_, 1:TS + 1], ALU.subtract, reads=[skey], writes=[tkey])
            P.V("vector", "scalar_tensor_tensor", out[:np_], tmp[:np_], mu_ap, src[:np_, 1:TS + 1], ALU.mult, ALU.add,
                reads=[tkey, skey, mkey], writes=[okey])
        for g in range(2):
            lerp(xr[g], f"xr{g}", inp["r"][g], f"in_r{g}", par[g][:, MU_R:MU_R + 1], ttm[g], f"ttm{g}", mkey=f"par{g}")
            lerp(xk[g], f"xk{g}", inp["k"][g], f"in_k{g}", par[g][:, MU_K:MU_K + 1], ttm[g], f"ttm{g}", mkey=f"par{g}")
            lerp(xv[g], f"xv{g}", inp["v"][g], f"in_v{g}", par[g][:, MU_V:MU_V + 1], ttm[g], f"ttm{g}", mkey=f"par{g}")
            lerp(sg[g], f"sg{g}", inp["g"][g], f"in_g{g}", par[g][:, MU_G:MU_G + 1], ttm[g], f"ttm{g}", mkey=f"par{g}")
            P.act(sg[g], sg[g], AF.Sigmoid, reads=[f"sg{g}"], writes=[f"sg{g}"])
        if stage == 0: return P
        lerp(tw, "tw", in_w, "in_w", parw[:, 0:1], ttm[0], "ttm0", np_=96, mkey="parw")
        P.act(tw, tw, AF.Tanh, reads=["tw"], writes=["tw"])
        lerp(xa, "xa", in_a, "in_a", parw[:, 1:2], ttm[1], "ttm1", np_=96, mkey="parw")
        if stage == 1: return P
        for g in range(2):
            for hf in range(TS // 512):
                cs_ = slice(hf * 512, hf * 512 + 512)
                P.mm(PS[0][:, :], wup[:, g * 128:(g + 1) * 128], tw[:, cs_], True, True, reads=["wup", "tw"], writes=["ps0"])
                P.act(lw[g][:, cs_], PS[0][:, :], AF.Sigmoid, bias=par[g][:, W0:W0 + 1], reads=["ps0", f"par{g}"], writes=[f"lw{g}"])
                P.mm(PS[1][:, :], aup[:, g * 128:(g + 1) * 128], xa[:, cs_], True, True, reads=["aup", "xa"], writes=["ps1"])
                P.act(aa[g][:, cs_], PS[1][:, :], AF.Sigmoid, bias=par[g][:, A0:A0 + 1], reads=["ps1", f"par{g}"], writes=[f"aa{g}"])
            P.V("gpsimd", "tensor_scalar", lw[g], lw[g], -float(np.exp(-0.5)), None, ALU.mult, reads=[f"lw{g}"], writes=[f"lw{g}"])
            P.V("vector", "tensor_scalar", tkk[g], xk[g], par[g][:, KK:KK + 1], None, ALU.mult, reads=[f"xk{g}", f"par{g}"], writes=[f"tkk{g}"])
            P.V("gpsimd", "tensor_tensor", ttm[g], tkk[g], tkk[g], ALU.mult, reads=[f"tkk{g}"], writes=[f"ttm{g}"])
            for hf in range(TS // 512):
                cs_ = slice(hf * 512, hf * 512 + 512)
                P.mm(PS[2][:, :], blk, ttm[g][:, cs_], True, True, reads=["blk", f"ttm{g}"], writes=["ps2"])
                P.V("vector", "tensor_scalar", tpr[g][:, cs_], PS[2][:, :], 1e-24, None, ALU.max, reads=["ps2"], writes=[f"tpr{g}"])
            P.act(tpr[g], tpr[g], AF.Sqrt, reads=[f"tpr{g}"], writes=[f"tpr{g}"])
            P.V("vector", "reciprocal", tpr[g], tpr[g], reads=[f"tpr{g}"], writes=[f"tpr{g}"])
            P.V("vector", "tensor_tensor", tkk[g], tkk[g], tpr[g], ALU.mult, reads=[f"tkk{g}", f"tpr{g}"], writes=[f"tkk{g}"])
            P.V("vector", "tensor_scalar", ttm[g], aa[g], 1.0, par[g][:, KA:KA + 1], ALU.subtract, ALU.mult, reads=[f"aa{g}", f"par{g}"], writes=[f"ttm{g}"])
            P.V("vector", "scalar_tensor_tensor", xk[g], ttm[g], 1.0, xk[g], ALU.add, ALU.mult, reads=[f"ttm{g}", f"xk{g}"], writes=[f"xk{g}"])
            P.V("vector", "scalar_tensor_tensor", tpr[g], xr[g], par[g][:, RK:RK + 1], xk[g], ALU.mult, ALU.mult, reads=[f"xr{g}", f"xk{g}", f"par{g}"], writes=[f"tpr{g}"])
            P.V("gpsimd", "tensor_tensor", aa[g], tkk[g], aa[g], ALU.mult, reads=[f"tkk{g}", f"aa{g}"], writes=[f"aa{g}"])
            for c in range(NCH):
                cc = slice(c * CH, (c + 1) * CH)
                P.V("vector", "tensor_tensor_scan", tcs[g][:, cc], ones[:, :], lw[g][:, cc], 0.0, ALU.mult, ALU.add, reads=[f"lw{g}", "ones"], writes=[f"tcs{g}"])
            P.act(te1[g], tcs[g], AF.Exp, reads=[f"tcs{g}"], writes=[f"te1{g}"])
            P.act(te2[g], tcs[g], AF.Exp, scale=-1.0, reads=[f"tcs{g}"], writes=[f"te2{g}"])
            P.V("gpsimd", "tensor_tensor", lw[g], tcs[g], lw[g], ALU.subtract, reads=[f"tcs{g}", f"lw{g}"], writes=[f"lw{g}"])
            P.act(lw[g], lw[g], AF.Exp, reads=[f"lw{g}"], writes=[f"lw{g}"])
            e1v = te1[g].rearrange("p (c s) -> p c s", s=CH)
            P.V("vector", "tensor_copy", gC[g], e1v[:, :, CH - 1], reads=[f"te1{g}"], writes=[f"gC{g}"])
            P.V("vector", "scalar_tensor_tensor", lw[g], tkk[g], -1.0, lw[g], ALU.mult, ALU.mult, reads=[f"tkk{g}", f"lw{g}"], writes=[f"lw{g}"])
            P.V("gpsimd", "tensor_tensor", xr[g], xr[g], te1[g], ALU.mult, reads=[f"xr{g}", f"te1{g}"], writes=[f"xr{g}"])
            P.V("vector", "tensor_tensor", tcs[g].rearrange("p (c s) -> p c s", s=CH), te2[g].rearrange("p (c s) -> p c s", s=CH),
                gC[g].unsqueeze(2).to_broadcast([128, NCH, CH]), ALU.mult, reads=[f"te2{g}", f"gC{g}"], writes=[f"tcs{g}"])
            P.V("gpsimd", "tensor_tensor", tkk[g], aa[g], te2[g], ALU.mult, reads=[f"aa{g}", f"te2{g}"], writes=[f"tkk{g}"])
            P.V("vector", "tensor_tensor", te2[g], xk[g], te2[g], ALU.mult, reads=[f"xk{g}", f"te2{g}"], writes=[f"te2{g}"])
            P.V("gpsimd", "tensor_tensor", aa[g], aa[g], tcs[g], ALU.mult, reads=[f"aa{g}", f"tcs{g}"], writes=[f"aa{g}"])
            P.V("vector", "tensor_tensor", tcs[g], xk[g], tcs[g], ALU.mult, reads=[f"xk{g}", f"tcs{g}"], writes=[f"tcs{g}"])
        if stage == 2: return P
        alb, rb, beb, kb, bet, kt = lw, xr, tkk, te2, aa, tcs
        kalb, krb, kbeb, kkb, kbet, kkt = "lw", "xr", "tkk", "te2", "aa", "tcs"

        for c in range(NCH):
            cg = seg * NCH + c
            cols = slice(c * CH, (c + 1) * CH)
            def F(x, h):
                g, h2 = h // 2, h % 2
                return x[g][64 * h2:64 * h2 + 64, cols]
            def RW(h):
                return slice(64 * (h % 2), 64 * (h % 2) + 64)
            def O(ps, h):
                return ps[RW(h), (h // 2) * 64:(h // 2) * 64 + 64]
            def C(t, h):
                return t[RW(h), h // 2, :]
            for h in range(4):
                g, h2 = h // 2, h % 2
                rows = RW(h)
                idb = ident[rows, 64 * h2:64 * h2 + 64]
                P.mm(O(pAab, h), F(beb, h), F(alb, h), True, True, reads=[f"{kbeb}{g}", f"{kalb}{g}"], writes=[kAab])
                P.mm(O(pAabT, h), F(alb, h), F(beb, h), True, True, reads=[f"{kbeb}{g}", f"{kalb}{g}"], writes=[kAabT])
                P.mm(O(pAak, h), F(kb, h), F(alb, h), True, True, reads=[f"{kkb}{g}", f"{kalb}{g}"], writes=[kAak])
                P.mm(O(pBbr, h), F(beb, h), F(rb, h), True, True, reads=[f"{kbeb}{g}", f"{krb}{g}"], writes=[kBbr])
            for h in range(4):
                g, h2 = h // 2, h % 2
                rows = RW(h)
                idb = ident[rows, 64 * h2:64 * h2 + 64]
                P.mm(O(pBkr, h), F(kb, h), F(rb, h), True, True, reads=[f"{kkb}{g}", f"{krb}{g}"], writes=[kBkr])
                P.mm(O(ptV, h), F(xv, h), idb, True, True, reads=[f"xv{g}", "ident"], writes=[ktV])
                P.mm(O(ptB, h), F(bet, h), idb, True, True, reads=[f"{kbet}{g}", "ident"], writes=[ktB])
                P.mm(O(ptK, h), F(kt, h), idb, True, True, reads=[f"{kkt}{g}", "ident"], writes=[ktK])
            if stage == 3: return P
            f2 = lambda t: t.rearrange("p g s -> p (g s)")
            P.V("vector", "tensor_tensor", f2(cN), pAab, f2(msu), ALU.mult, reads=[kAab, "masks"], writes=["cN"])
            P.V("vector", "tensor_tensor", f2(cNT), pAabT, f2(msl), ALU.mult, reads=[kAabT, "masks"], writes=["cNT"])
            P.V("vector", "tensor_tensor", f2(cAak), pAak, f2(msu), ALU.mult, reads=[kAak, "masks"], writes=["cAak"])
            P.V("vector", "tensor_tensor", f2(cBbr), pBbr, f2(mui), ALU.mult, reads=[kBbr, "masks"], writes=["cBbr"])
            P.V("vector", "tensor_tensor", f2(cBkr), pBkr, f2(mui), ALU.mult, reads=[kBkr, "masks"], writes=["cBkr"])
            P.act(f2(cV), ptV, AF.Copy, reads=[ktV], writes=["cV"])
            P.act(f2(cBt), ptB, AF.Copy, reads=[ktB], writes=["cBt"])
            P.act(f2(cKt), ptK, AF.Copy, reads=[ktK], writes=["cKt"])
            P.V("gpsimd", "tensor_tensor", f2(cR[0]), f2(cN), f2(mid), ALU.add, reads=["cN", "masks"], writes=["cR0"])
            if stage == 4: return P
            Pc, PTc, Rc = (cN, "cN"), (cNT, "cNT"), (cR[0], "cR0")
            for i in range(1, 6):
                nP = (cP[i % 2], f"cP{i%2}"); nPT = (cPT[i % 2], f"cPT{i%2}"); nR = (cR[i % 2], f"cR{i%2}")
                for h in range(4):
                    if i < 5:
                        P.mm(O(pP, h), C(PTc[0], h), C(Pc[0], h), True, True, reads=[PTc[1], Pc[1]], writes=[kP])
                    P.mm(O(pPT, h), C(Pc[0], h), C(PTc[0], h), True, True, reads=[PTc[1], Pc[1]], writes=[kPT])
                if i < 5:
                    P.act(f2(nP[0]), pP, AF.Copy, reads=[kP], writes=[nP[1]])
                P.act(f2(nPT[0]), pPT, AF.Copy, reads=[kPT], writes=[nPT[1]])
                for h in range(4):
                    P.mm(O(pR, h), C(nPT[0], h), C(Rc[0], h), True, True, reads=[nPT[1], Rc[1]], writes=[kR])
                P.V("vector", "tensor_tensor", f2(nR[0]), pR, f2(Rc[0]), ALU.add, reads=[kR, Rc[1]], writes=[nR[1]])
                Pc, PTc, Rc = nP, nPT, nR
            Tm = Rc
            if stage == 5: return P
            for h in range(4):
                g = h // 2
                P.mm(O(pWT, h), F(alb, h), ST[g][RW(h), :], True, False, reads=[f"{kalb}{g}", f"ST{g}"], writes=[kWT])
                P.mm(O(pWT, h), C(cAak, h), C(cV, h), False, True, reads=["cAak", "cV"], writes=[kWT])
            P.act(f2(cWT), pWT, AF.Copy, reads=[kWT], writes=["cWT"])
            for h in range(4):
                P.mm(O(pUT, h), C(Tm[0], h), C(cWT, h), True, True, reads=[Tm[1], "cWT"], writes=[kUT])
            P.V("vector", "tensor_scalar", f2(cUT), pUT, 1.0, None, ALU.mult, reads=[kUT], writes=["cUT"])
            for h in range(4):
                g = h // 2
                P.mm(O(pYT, h), F(rb, h), ST[g][RW(h), :], True, False, reads=[f"{krb}{g}", f"ST{g}"], writes=[kYT])
                P.mm(O(pYT, h), C(cBbr, h), C(cUT, h), False, False, reads=["cBbr", "cUT"], writes=[kYT])
                P.mm(O(pYT, h), C(cBkr, h), C(cV, h), False, True, reads=["cBkr", "cV"], writes=[kYT])
            P.act(f2(cY), pYT, AF.Copy, reads=[kYT], writes=["cY"])
            for h in range(4):
                P.mm(O(pS, h), C(cBt, h), C(cUT, h), True, False, reads=["cBt", "cUT"], writes=[kS])
                P.mm(O(pS, h), C(cKt, h), C(cV, h), False, True, reads=["cKt", "cV"], writes=[kS])
            for g in range(2):
                P.V("vector", "scalar_tensor_tensor", ST[g], ST[g], gC[g][:, c:c + 1], pS[:, g * 64:(g + 1) * 64], ALU.mult, ALU.add,
                    reads=[f"ST{g}", f"gC{g}", kS], writes=[f"ST{g}"])
            if stage == 6: return P
            B3 = lambda t: t.unsqueeze(2).to_broadcast([128, 2, 64])
            P.V("vector", "tensor_reduce", st1, cY, AX.X, ALU.add, reads=["cY"], writes=["st1"])
            P.V("vector", "tensor_scalar", st1, st1, 1.0 / 64, None, ALU.mult, reads=["st1"], writes=["st1"])
            P.V("vector", "tensor_tensor", cYc, cY, B3(st1), ALU.subtract, reads=["cY", "st1"], writes=["cYc"])
            P.V("gpsimd", "tensor_tensor", cSq, cYc, cYc, ALU.mult, reads=["cYc"], writes=["cSq"])
            P.V("vector", "tensor_reduce", st2, cSq, AX.X, ALU.add, reads=["cSq"], writes=["st2"])
            P.V("vector", "tensor_scalar", st2, st2, 1.0 / 64, 64e-5, ALU.mult, ALU.add, reads=["st2"], writes=["st2"])
            P.act(st2, st2, AF.Sqrt, reads=["st2"], writes=["st2"])
            P.V("vector", "reciprocal", st3, st2, reads=["st2"], writes=["st3"])
            P.V("vector", "tensor_tensor", cYc, cYc, B3(st3), ALU.mult, reads=["cYc", "st3"], writes=["cYc"])
            P.V("gpsimd", "tensor_tensor", cYc, cYc, lnw, ALU.mult, reads=["cYc", "lnw"], writes=["cYc"])
            P.V("gpsimd", "tensor_tensor", cYc, cYc, lnb, ALU.add, reads=["cYc", "lnb"], writes=["cYc"])
            for h in range(4):
                g = h // 2
                P.mm(pBo[RW(h), g:g + 1], F(tpr, h), ones[RW(h), 0:1], True, True, reads=[f"tpr{g}", "ones"], writes=[kBo])
            P.act(bo, pBo, AF.Copy, reads=[kBo], writes=["bo"])
            P.V("vector", "tensor_tensor", cSq, cV, B3(bo), ALU.mult, reads=["cV", "bo"], writes=["cSq"])
            P.V("gpsimd", "tensor_tensor", cYc, cYc, cSq, ALU.add, reads=["cYc", "cSq"], writes=["cYc"])
            P.dma("gpsimd", cYB, cYc[64:128], reads=["cYc"], writes=["cYB"])
            for g in range(2):
                P.mm(pM[:64, :], sg[g][:, cols], gup[g], g == 0, g == 1, reads=[f"sg{g}", f"gup{g}"], writes=[kM])
            co = cOut[cg % 2]; cok = f"cOut{cg%2}"
            pMv = pM[:64, :].rearrange("p (g h v) -> p g h v", g=2, h=2)
            P.V("vector", "tensor_tensor", co[:, :, 0, :], cYc[0:64], pMv[:, :, 0, :], ALU.mult, reads=["cYc", kM], writes=[cok])
            P.V("vector", "tensor_tensor", co[:, :, 1, :], cYB, pMv[:, :, 1, :], ALU.mult, reads=["cYB", kM], writes=[cok])
            P.dma("sync" if cg % 2 == 0 else "scalar", ob[cg * CH:(cg + 1) * CH, :], co.rearrange("p g h v -> p (g h v)"), reads=[cok], final=True)
    return P


EI, EO = "ExternalInput", "ExternalOutput"
NEG = -1.0e30

def dsa_consts():
    RA = np.zeros((128, 128), np.float32)
    for d in range(16):
        RA[d, d + 16] = -1.0; RA[d + 16, d] = 1.0
    RI = np.zeros((128, 128), np.float32)
    for b in (0, 64):
        for d in range(8):
            RI[b + d, b + d + 8] = -1.0; RI[b + d + 8, b + d] = 1.0
    return {"c_RAT": np.ascontiguousarray(RA.T), "c_RIT": np.ascontiguousarray(RI.T), "c_ident": np.eye(128, dtype=np.float32),
            "c_iota": np.tile(np.arange(512, dtype=np.float32)[None, :], (128, 1))}

def build_dsa(L, stage=99, idx_dt=None):
    IDT = F32
    P = Prog()
    J = L // 1024
    qT_d = P.dram("qT", [J, 16, 128, 128], F32, kind=EI)
    cosq_d = P.dram("cosq", [J, 32, 128], F32, kind=EI); sinq_d = P.dram("sinq", [J, 32, 128], F32, kind=EI)
    qiT_d = P.dram("qiT", [J, 16, 128, 128], F32, kind=EI)
    cosqi_d = P.dram("cosqi", [J, 16, 128], F32, kind=EI); sinqi_d = P.dram("sinqi", [J, 16, 128], F32, kind=EI)
    kT_d = P.dram("kT", [4, 128, L], F32, kind=EI)
    cosk_d = P.dram("cosk", [32, L], F32, kind=EI); sink_d = P.dram("sink", [32, L], F32, kind=EI)
    kiT_d = P.dram("kiT", [64, L], F32, kind=EI)
    coski_d = P.dram("coski", [16, L], F32, kind=EI); sinki_d = P.dram("sinki", [16, L], F32, kind=EI)
    v_d = P.dram("v", [L, 512], F32, kind=EI)
    wi_d = P.dram("wi", [J, 128, 32], F32, kind=EI)
    qpos_d = P.dram("qpos", [J, 128, 1], F32, kind=EI)
    RAT_d = P.dram("c_RAT", [128, 128], F32, kind=EI); RIT_d = P.dram("c_RIT", [128, 128], F32, kind=EI)
    ident_d = P.dram("c_ident", [128, 128], F32, kind=EI); iota_d = P.dram("c_iota", [128, 512], F32, kind=EI)
    oa_d = P.dram("oa", [J, 128, 2048], F32, kind=EO)
    kscr = P.dram("kscr", [4, 128, L], BF16)
    vscr = P.dram("vscr", [L, 512], BF16)

    T = P.sb
    RAT = T("RAT", [128, 128]); RIT = T("RIT", [128, 128]); identf = T("identf", [128, 128]); identb = T("identb", [128, 128], BF16)
    iota = T("iota", [128, 512])
    P.dma("sync", RAT, RAT_d, writes=["RAT"]); P.dma("sync", RIT, RIT_d, writes=["RIT"])
    P.dma("scalar", identf, ident_d, writes=["identf"]); P.dma("scalar", iota, iota_d, writes=["iota"])
    P.V("vector", "tensor_copy", identb, identf, reads=["identf"], writes=["identb"])
    PS = [P.ps(f"ps{i}", [128, 512], F32) for i in range(6)]
    PSB = [P.ps(f"psb{i}", [128, 1024], BF16) for i in range(2)]

    xin = [T(f"xin{i}", [128, 512]) for i in range(2)]
    tc_ = [T(f"tcos{i}", [128, 512]) for i in range(2)]
    tsn = [T(f"tsin{i}", [128, 512]) for i in range(2)]
    rt = T("rt", [128, 512]); ru = T("ru", [128, 512])
    xo = [T(f"xo{i}", [128, 512], BF16) for i in range(2)]
    cnt = [0]

    def rotary(src_ap, Tn, RT, rtkey, ranges, cos_ap, sin_ap, out_ap, okey, out_reads=(), tab_rows=None):
        i = cnt[0] % 2; cnt[0] += 1
        if isinstance(src_ap, list):
            for (r0, nr, ap_) in src_ap:
                P.dma("sync", xin[i][r0:r0 + nr, :Tn], ap_, writes=[f"xin{i}"])
        else:
            P.dma("sync", xin[i][:, :Tn], src_ap, writes=[f"xin{i}"])
        for (r0, nr) in ranges:
            P.dma("scalar", tc_[i][r0:r0 + nr, :Tn], cos_ap, writes=[f"tcos{i}"])
            P.dma("gpsimd", tsn[i][r0:r0 + nr, :Tn], sin_ap, writes=[f"tsin{i}"])
        pk = f"ps{i}"
        P.mm(PS[i][:, :Tn], RT, xin[i][:, :Tn], True, True, reads=[rtkey, f"xin{i}"], writes=[pk])
        P.act(out_ap, xin[i][:, :Tn], AF.Copy, reads=[f"xin{i}"] + list(out_reads), writes=[okey])
        for (r0, nr) in ranges:
            rs = slice(r0, r0 + nr)
            P.V("vector", "tensor_tensor", rt[rs, :Tn], PS[i][rs, :Tn], tsn[i][rs, :Tn], ALU.mult, reads=[pk, f"tsin{i}"], writes=["rt"])
            P.V("gpsimd", "tensor_tensor", ru[rs, :Tn], xin[i][rs, :Tn], tc_[i][rs, :Tn], ALU.mult, reads=[f"xin{i}", f"tcos{i}"], writes=["ru"])
            P.V("vector", "tensor_tensor", out_ap[rs], rt[rs, :Tn], ru[rs, :Tn], ALU.add, reads=["rt", "ru"], writes=[okey])

    kiT = T("kiT", [128, L], IDT)
    xoi = [T(f"xoi{i}", [128, 512], IDT) for i in range(2)]
    for kv in range(4):
        for ch in range(L // 512):
            cs = slice(ch * 512, ch * 512 + 512)
            i = cnt[0] % 2
            rotary(kT_d[kv, :, cs], 512, RAT, "RAT", [(0, 32)], cosk_d[:, cs], sink_d[:, cs], xo[i], f"xo{i}")
            P.dma("sync", kscr[kv, :, cs], xo[i], reads=[f"xo{i}"], writes=["kscr"])
    for ch in range(L // 512):
        cs = slice(ch * 512, ch * 512 + 512)
        i = cnt[0] % 2
        rotary([(0, 64, kiT_d[:, cs]), (64, 64, kiT_d[:, cs])], 512, RIT, "RIT", [(0, 16), (64, 16)], coski_d[:, cs], sinki_d[:, cs], xoi[i], f"xoi{i}")
        P.V("vector", "tensor_copy", kiT[:, cs], xoi[i], reads=[f"xoi{i}"], writes=["kiT"])
    NKMAX = L
    acc = T("acc", [128, NKMAX]); work = T("work", [128, NKMAX]); mb = T("mb", [128, NKMAX], BF16)
    pbf = T("pbf", [128, NKMAX], BF16)
    Kb = [T(f"Kb{i}", [128, NKMAX], BF16) for i in range(1)]
    Vb = [T(f"Vb{i}", [128, NKMAX // 128, 128], BF16) for i in range(1)]
    nvt = min(4, NKMAX // 512)
    vst = work[:, 0:nvt * 512].rearrange("p (t c) -> p t c", c=512)
    vsb = pbf[:, 0:nvt * 512].rearrange("p (t c) -> p t c", c=512)
    vr = nvt * 128
    for it in range(L // vr):
        P.dma("scalar", vst, v_d[it * vr:(it + 1) * vr, :].rearrange("(t p) c -> p t c", p=128), writes=["work"])
        P.V("gpsimd", "tensor_copy", vsb, vst, reads=["work"], writes=["pbf"])
        P.dma("scalar", vscr[it * vr:(it + 1) * vr, :].rearrange("(t p) c -> p t c", p=128), vsb, reads=["pbf"], writes=["vscr"])
    if stage == 0: return P

    QT = T("QT", [128, 16, 128], BF16); QI = T("QI", [128, 16, 128], IDT)
    wi = T("wi", [128, 32]); qpos = T("qpos", [128, 1]); qoff = T("qoff", [128, 2])
    rl = [T(f"rl{i}", [128, 512]) for i in range(2)]
    m8 = T("m8", [128, 8]); thr = T("thr", [128, 1]); mx = T("mx", [128, 1]); nmx = T("nmx", [128, 1]); rsum = T("rsum", [128, 1]); rinv = T("rinv", [128, 1]); rinv2 = T("rinv2", [128, 2])
    pT = [T(f"pT{i}", [128, 4, 128], BF16) for i in range(2)]
    obh = [T(f"obh{i}", [128, 128]) for i in range(2)]
    cbias = rt
    SC = float(32 ** -0.5 * 64 ** -0.5)

    for j in range(J):
        nk = 1024 * (j + 1)
        NCHK = nk // 512
        P.dma("sync", wi, wi_d[j], writes=["wi"]); P.dma("sync", qpos, qpos_d[j], writes=["qpos"])
        P.V("vector", "tensor_scalar", wi, wi, SC, None, ALU.mult, reads=["wi"], writes=["wi"])
        for h in range(16):
            rotary(qT_d[j, h], 128, RAT, "RAT", [(0, 32)], cosq_d[j], sinq_d[j], QT[:, h, :], "QT")
        for pr in range(16):
            rotary(qiT_d[j, pr], 128, RIT, "RIT", [(0, 16), (64, 16)], cosqi_d[j], sinqi_d[j], QI[:, pr, :], "QI")
        for hh in range(32):
            pr, h2 = hh // 2, hh % 2
            rows = slice(64 * h2, 64 * h2 + 64)
            for ck in range(NCHK):
                cs = slice(ck * 512, ck * 512 + 512)
                pi = 2 + (hh * NCHK + ck) % 2
                pk = f"ps{pi}"
                P.mm(PS[pi], QI[rows, pr, :], kiT[rows, cs], True, True, reads=["QI", "kiT"], writes=[pk])
                ri = (hh * NCHK + ck) % 2
                P.act(rl[ri], PS[pi], AF.Relu, reads=[pk], writes=[f"rl{ri}"])
                if hh == 0:
                    P.V("vector", "tensor_scalar", acc[:, cs], rl[ri], wi[:, 0:1], None, ALU.mult, reads=[f"rl{ri}", "wi"], writes=["acc"])
                else:
                    P.V("vector", "scalar_tensor_tensor", acc[:, cs], rl[ri], wi[:, hh:hh + 1], acc[:, cs], ALU.mult, ALU.add,
                        reads=[f"rl{ri}", "wi", "acc"], writes=["acc"])
        for t in range(2):
            off = nk - 1024 + t * 512
            P.V("vector", "tensor_scalar", qoff[:, t:t + 1], qpos, float(-off), None, ALU.add, reads=["qpos"], writes=["qoff"])
            P.V("vector", "tensor_scalar", cbias, iota, qoff[:, t:t + 1], NEG, ALU.is_gt, ALU.mult, reads=["iota", "qoff"], writes=["rt"])
            P.V("vector", "tensor_tensor", acc[:, off:off + 512], acc[:, off:off + 512], cbias, ALU.add, reads=["acc", "rt"], writes=["acc"])
        for r in range(32):
            src = acc if r == 0 else work
            sk = "acc" if r == 0 else "work"
            P.V("vector", "max", m8, src[:, :nk], reads=[sk], writes=["m8"])
            if r < 31:
                P.V("vector", "match_replace", work[:, :nk], m8, src[:, :nk], NEG, reads=["m8", sk], writes=["work"])
        P.V("vector", "tensor_scalar", thr, m8[:, 7:8], -1.0e29, None, ALU.max, reads=["m8"], writes=["thr"])
        P.V("vector", "tensor_scalar", mb[:, :nk], acc[:, :nk], thr, NEG, ALU.is_lt, ALU.mult, reads=["acc", "thr"], writes=["mb"])
        if stage == 1:
            P.dma("sync", oa_d[j, :, 0:1024], acc[:, nk - 1024:nk], reads=["acc"], final=True)
            return P
        wk = [work, acc]; wkk = ["work", "acc"]
        def A1(h):
            kv = h // 4
            if h % 4 == 0:
                P.dma("sync", Kb[0][:, :nk], kscr[kv, :, :nk], reads=["kscr"], writes=["Kb0"])
            w_ = wk[h % 2]; wkey = wkk[h % 2]
            for ck in range(NCHK):
                cs = slice(ck * 512, ck * 512 + 512)
                pi = 2 + ck % 2
                pk = f"ps{pi}"
                P.mm(PS[pi], QT[:, h, :], Kb[0][:, cs], True, True, reads=["QT", "Kb0"], writes=[pk])
                P.V("vector", "scalar_tensor_tensor", w_[:, cs], PS[pi], float(128 ** -0.5), mb[:, cs], ALU.mult, ALU.add,
                    reads=[pk, "mb"], writes=[wkey])
            P.V("vector", "reduce_max", mx, w_[:, :nk], AX.X, reads=[wkey], writes=["mx"])
            P.V("vector", "tensor_scalar", nmx, mx, -1.0, None, ALU.mult, reads=["mx"], writes=["nmx"])
        def A2(h):
            w_ = wk[h % 2]; wkey = wkk[h % 2]
            P.act(pbf[:, :nk], w_[:, :nk], AF.Exp, bias=nmx, accum_out=rsum, reads=[wkey, "nmx"], writes=["pbf", "rsum"])
            P.V("vector", "reciprocal", rinv, rsum, reads=["rsum"], writes=["rinv"])
        def B(h):
            kv = h // 4
            if h % 4 == 0:
                P.dma("scalar", Vb[0][:, :nk // 128, :], vscr[0:nk, kv * 128:(kv + 1) * 128].rearrange("(t p) c -> p t c", p=128),
                      reads=["vscr"], writes=["Vb0"])
            P.V("vector", "tensor_copy", rinv2[:, h % 2:h % 2 + 1], rinv, reads=["rinv"], writes=[f"rinv2_{h % 2}"])
            NT4 = nk // 512
            for t4 in range(NT4):
                bq = t4 % 2
                for u in range(4):
                    kt = t4 * 4 + u
                    P.tr(PSB[bq][:, u * 128:(u + 1) * 128], pbf[:, kt * 128:(kt + 1) * 128], identb, reads=["pbf", "identb"], writes=[f"psb{bq}"])
                if t4 % 2 == 0:
                    P.act(pT[bq].rearrange("p u q -> p (u q)"), PSB[bq][:, 0:512], AF.Copy, reads=[f"psb{bq}"], writes=[f"pT{bq}"])
                else:
                    P.V("vector", "tensor_scalar", pT[bq].rearrange("p u q -> p (u q)"), PSB[bq][:, 0:512], 1.0, None, ALU.mult, reads=[f"psb{bq}"], writes=[f"pT{bq}"])
                for u in range(4):
                    kt = t4 * 4 + u
                    P.mm(PS[4 + h % 2][:, 0:128], pT[bq][:, u, :], Vb[0][:, kt, :], kt == 0, kt == nk // 128 - 1,
                         reads=[f"pT{bq}", "Vb0"], writes=[f"ps{4 + h % 2}"])
            P.V("vector", "tensor_scalar", obh[h % 2], PS[4 + h % 2][:, 0:128], rinv2[:, h % 2:h % 2 + 1], None, ALU.mult,
                reads=[f"ps{4 + h % 2}", f"rinv2_{h % 2}"], writes=[f"obh{h % 2}"])
            P.dma("gpsimd", oa_d[j, :, h * 128:(h + 1) * 128], obh[h % 2], reads=[f"obh{h % 2}"], final=True)
        A1(0); A2(0)
        for h in range(16):
            if h + 1 < 16:
                A1(h + 1)
            B(h)
            if h + 1 < 16:
                A2(h + 1)
    return P


EI, EO = "ExternalInput", "ExternalOutput"
PI = float(np.pi)

def build_s5(L, C=512):
    P = Prog()
    NCK = L // C
    uT_d = P.dram("uT", [2, 128, L], F32, kind=EI)
    Bre_d = P.dram("Bre", [8, 128, 128], F32, kind=EI); Bim_d = P.dram("Bim", [8, 128, 128], F32, kind=EI)
    Cre_d = P.dram("Cre", [8, 128, 128], F32, kind=EI); Cim_d = P.dram("Cim", [8, 128, 128], F32, kind=EI)
    lam_d = P.dram("lam", [128, 8, 3], F32, kind=EI)
    dsk_d = P.dram("dsk", [128, 2], F32, kind=EI)
    iota_d = P.dram("c_iota", [128, C + 1], F32, kind=EI)
    zg_d = P.dram("zgT", [2, 128, L], F32, kind=EO)
    T = P.sb
    Bre = T("Bre", [128, 8, 128]); Bim = T("Bim", [128, 8, 128]); Cre = T("Cre", [128, 8, 128]); Cim = T("Cim", [128, 8, 128])
    lam = T("lam", [128, 8, 3]); dsk = T("dsk", [128, 2]); iota = T("iota", [128, C + 1])
    for r in range(8):
        P.dma("sync", Bre[:, r, :], Bre_d[r], writes=["Bre"]); P.dma("scalar", Bim[:, r, :], Bim_d[r], writes=["Bim"])
        P.dma("sync", Cre[:, r, :], Cre_d[r], writes=["Cre"]); P.dma("scalar", Cim[:, r, :], Cim_d[r], writes=["Cim"])
    P.dma("sync", lam, lam_d, writes=["lam"]); P.dma("sync", dsk, dsk_d, writes=["dsk"]); P.dma("sync", iota, iota_d, writes=["iota"])
    P.V("vector", "tensor_scalar", Cim.rearrange("p r c -> p (r c)"), Cim.rearrange("p r c -> p (r c)"), -1.0, None, ALU.mult, reads=["Cim"], writes=["Cim"])

    W = C + 1
    ki = T("ki", [128, W], I32); t1 = T("t1", [128, W]); t2 = T("t2", [128, W]); t3 = T("t3", [128, W])

    def sin_of(out, okey, ang, akey, w, shift):
        P.V("vector", "tensor_scalar", t1[:, :w], ang, shift, 1.0 / (2 * PI), ALU.add, ALU.mult, reads=[akey], writes=["t1"])
        P.V("vector", "tensor_copy", ki[:, :w], t1[:, :w], reads=["t1"], writes=["ki"])
        P.V("vector", "tensor_copy", t2[:, :w], ki[:, :w], reads=["ki"], writes=["t2"])
        P.V("vector", "tensor_scalar", t1[:, :w], ang, shift, None, ALU.add, reads=[akey], writes=["t1"])
        P.V("vector", "scalar_tensor_tensor", t1[:, :w], t2[:, :w], -2 * PI, t1[:, :w], ALU.mult, ALU.add, reads=["t2", "t1"], writes=["t1"])
        P.V("vector", "tensor_scalar", t2[:, :w], t1[:, :w], PI, -2 * PI, ALU.is_gt, ALU.mult, reads=["t1"], writes=["t2"])
        P.V("vector", "tensor_scalar", t3[:, :w], t1[:, :w], -PI, 2 * PI, ALU.is_lt, ALU.mult, reads=["t1"], writes=["t3"])
        P.V("vector", "tensor_tensor", t1[:, :w], t1[:, :w], t2[:, :w], ALU.add, reads=["t1", "t2"], writes=["t1"])
        P.V("vector", "tensor_tensor", t1[:, :w], t1[:, :w], t3[:, :w], ALU.add, reads=["t1", "t3"], writes=["t1"])
        P.act(out, t1[:, :w], AF.Sin, reads=["t1"], writes=[okey])

    NP = 16
    pp = T("pp", [128, 8, NP])
    LR, LI, ST, TH, MAG, CT, SN, AR, AI, DEN, CR, CI, COSC, SINC, TMP, TMP2 = range(16)
    cosT = T("cosT", [128, 8, W]); sinT = T("sinT", [128, 8, W]); Er = T("Er", [128, 8, C]); Ei = T("Ei", [128, 8, C])
    rmag = T("rmag", [128, 8, C]); ang = T("ang", [128, W])
    def pc(r, i):
        return pp[:, r, i:i + 1]
    K = ["pp"]
    for r in range(8):
        P.V("vector", "tensor_scalar", pc(r, LR), lam[:, r, 0:1], -1e-4, None, ALU.min, reads=["lam"], writes=K)
        P.V("vector", "tensor_copy", pc(r, LI), lam[:, r, 1:2], reads=["lam"], writes=K)
        P.act(pc(r, ST), lam[:, r, 2:3], AF.Exp, reads=["lam"], writes=K)
        P.V("vector", "tensor_tensor", pc(r, TH), pc(r, LI), pc(r, ST), ALU.mult, reads=K, writes=K)
        P.V("vector", "tensor_tensor", pc(r, TMP), pc(r, LR), pc(r, ST), ALU.mult, reads=K, writes=K)
        P.act(pc(r, MAG), pc(r, TMP), AF.Exp, reads=K, writes=K)
        sin_of(pc(r, SN), "pp", pc(r, TH), "pp", 1, 0.0)
        sin_of(pc(r, CT), "pp", pc(r, TH), "pp", 1, PI / 2)
        P.V("vector", "tensor_tensor", pc(r, AR), pc(r, MAG), pc(r, CT), ALU.mult, reads=K, writes=K)
        P.V("vector", "tensor_tensor", pc(r, AI), pc(r, MAG), pc(r, SN), ALU.mult, reads=K, writes=K)
        P.V("vector", "tensor_tensor", pc(r, DEN), pc(r, LR), pc(r, LR), ALU.mult, reads=K, writes=K)
        P.V("vector", "scalar_tensor_tensor", pc(r, DEN), pc(r, LI), pc(r, LI), pc(r, DEN), ALU.mult, ALU.add, reads=K, writes=K)
        P.V("vector", "reciprocal", pc(r, DEN), pc(r, DEN), reads=K, writes=K)
        P.V("vector", "tensor_scalar", pc(r, TMP), pc(r, AR), -1.0, None, ALU.add, reads=K, writes=K)
        P.V("vector", "tensor_tensor", pc(r, TMP2), pc(r, LI), pc(r, AI), ALU.mult, reads=K, writes=K)
        P.V("vector", "scalar_tensor_tensor", pc(r, CR), pc(r, TMP), pc(r, LR), pc(r, TMP2), ALU.mult, ALU.add, reads=K, writes=K)
        P.V("vector", "tensor_tensor", pc(r, CR), pc(r, CR), pc(r, DEN), ALU.mult, reads=K, writes=K)
        P.V("vector", "tensor_tensor", pc(r, TMP2), pc(r, LI), pc(r, TMP), ALU.mult, reads=K, writes=K)
        P.V("vector", "scalar_tensor_tensor", pc(r, CI), pc(r, AI), pc(r, LR), pc(r, TMP2), ALU.mult, ALU.subtract, reads=K, writes=K)
        P.V("vector", "tensor_tensor", pc(r, CI), pc(r, CI), pc(r, DEN), ALU.mult, reads=K, writes=K)
        P.V("vector", "tensor_scalar", ang, iota, pc(r, TH), None, ALU.mult, reads=["iota"] + K, writes=["ang"])
        sin_of(sinT[:, r, :], "sinT", ang, "ang", W, 0.0)
        sin_of(cosT[:, r, :], "cosT", ang, "ang", W, PI / 2)
        P.V("vector", "tensor_scalar", t1[:, :C], sinT[:, r, :C], pc(r, CI), None, ALU.mult, reads=["sinT"] + K, writes=["t1"])
        P.V("vector", "scalar_tensor_tensor", Er[:, r, :], cosT[:, r, :C], pc(r, CR), t1[:, :C], ALU.mult, ALU.add, reads=["cosT", "t1"] + K, writes=["Er"])
        P.V("vector", "tensor_scalar", t1[:, :C], sinT[:, r, :C], pc(r, CR), None, ALU.mult, reads=["sinT"] + K, writes=["t1"])
        P.V("vector", "scalar_tensor_tensor", Ei[:, r, :], cosT[:, r, :C], pc(r, CI), t1[:, :C], ALU.mult, ALU.subtract, reads=["cosT", "t1"] + K, writes=["Ei"])
        P.V("vector", "tensor_scalar", rmag[:, r, :], iota[:, :C], 0.0, pc(r, MAG), ALU.mult, ALU.add, reads=["iota"] + K, writes=["rmag"])
    wst = T("wst", [128, 8, 2]);
    P.V("vector", "memset", wst, 0.0, writes=["wst"])
    PSb = [P.ps(f"psb{i}", [128, 512], F32) for i in range(4)]
    PSy = [P.ps(f"psy{i}", [128, 512], F32) for i in range(2)]
    ub = [T(f"ub{i}", [128, C]) for i in range(2)]
    vr = T("vr", [128, C]); vi = T("vi", [128, C]); m1 = T("m1", [128, C]); m2 = T("m2", [128, C])
    wr = T("wr", [128, C]); wi_ = T("wi_", [128, C])
    xr = [T(f"xr{i}", [128, C]) for i in range(2)]; xi = [T(f"xi{i}", [128, C]) for i in range(2)]
    d1 = T("d1", [128, C]); d2 = T("d2", [128, C]); cw = T("cw", [128, 4])
    yb = T("yb", [128, C]); g1 = T("g1", [128, C]); g2 = T("g2", [128, C]); zo = [T(f"zo{i}", [128, C]) for i in range(2)]
    it = 0
    for cb in range(2):
        for ck in range(NCK):
            cs = slice(ck * C, (ck + 1) * C)
            ui = (cb * NCK + ck) % 2
            P.dma("sync", ub[ui], uT_d[cb, :, cs], writes=[f"ub{ui}"])
            yi = (cb * NCK + ck) % 2
            for rr in range(4):
                r = cb * 4 + rr
                pbr = PSb[2 * (it % 2)]; pbi = PSb[2 * (it % 2) + 1]; pbk = f"psb{2 * (it % 2)}"; pbk2 = f"psb{2 * (it % 2) + 1}"
                P.mm(pbr[:, 0:C], Bre[:, r, :], ub[ui], True, True, reads=["Bre", f"ub{ui}"], writes=[pbk])
                P.mm(pbi[:, 0:C], Bim[:, r, :], ub[ui], True, True, reads=["Bim", f"ub{ui}"], writes=[pbk2])
                bur = pbr[:, 0:C]; bui = pbi[:, 0:C]
                P.V("vector", "tensor_tensor", m1, bur, Er[:, r, :], ALU.mult, reads=[pbk, "Er"], writes=["m1"])
                P.V("vector", "tensor_tensor", m2, bui, Ei[:, r, :], ALU.mult, reads=[pbk2, "Ei"], writes=["m2"])
                P.V("gpsimd", "tensor_tensor", vr, m1, m2, ALU.subtract, reads=["m1", "m2"], writes=["vr"])
                P.V("vector", "tensor_tensor", m1, bui, Er[:, r, :], ALU.mult, reads=[pbk2, "Er"], writes=["m1"])
                P.V("vector", "tensor_tensor", m2, bur, Ei[:, r, :], ALU.mult, reads=[pbk, "Ei"], writes=["m2"])
                P.V("gpsimd", "tensor_tensor", vi, m1, m2, ALU.add, reads=["m1", "m2"], writes=["vi"])
                P.V("vector", "tensor_tensor_scan", wr, rmag[:, r, :], vr, wst[:, r, 0:1], ALU.mult, ALU.add, reads=["rmag", "vr", "wst"], writes=["wr"])
                P.V("vector", "tensor_tensor_scan", wi_, rmag[:, r, :], vi, wst[:, r, 1:2], ALU.mult, ALU.add, reads=["rmag", "vi", "wst"], writes=["wi_"])
                P.V("vector", "tensor_tensor", cw[:, 0:1], wr[:, C - 1:C], cosT[:, r, C:C + 1], ALU.mult, reads=["wr", "cosT"], writes=["cw"])
                P.V("vector", "tensor_tensor", cw[:, 1:2], wi_[:, C - 1:C], sinT[:, r, C:C + 1], ALU.mult, reads=["wi_", "sinT"], writes=["cw"])
                P.V("vector", "tensor_tensor", cw[:, 2:3], wr[:, C - 1:C], sinT[:, r, C:C + 1], ALU.mult, reads=["wr", "sinT"], writes=["cw"])
                P.V("vector", "tensor_tensor", cw[:, 3:4], wi_[:, C - 1:C], cosT[:, r, C:C + 1], ALU.mult, reads=["wi_", "cosT"], writes=["cw"])
                P.V("vector", "tensor_tensor", wst[:, r, 0:1], cw[:, 0:1], cw[:, 1:2], ALU.subtract, reads=["cw"], writes=["wst"])
                P.V("vector", "tensor_tensor", wst[:, r, 1:2], cw[:, 2:3], cw[:, 3:4], ALU.add, reads=["cw"], writes=["wst"])
                xi_ = it % 2
                P.V("gpsimd", "tensor_tensor", d1, wr, cosT[:, r, :C], ALU.mult, reads=["wr", "cosT"], writes=["d1"])
                P.V("gpsimd", "tensor_tensor", d2, wi_, sinT[:, r, :C], ALU.mult, reads=["wi_", "sinT"], writes=["d2"])
                P.V("gpsimd", "tensor_tensor", xr[xi_], d1, d2, ALU.subtract, reads=["d1", "d2"], writes=[f"xr{xi_}"])
                P.V("gpsimd", "tensor_tensor", d1, wr, sinT[:, r, :C], ALU.mult, reads=["wr", "sinT"], writes=["d1"])
                P.V("vector", "tensor_tensor", d2, wi_, cosT[:, r, :C], ALU.mult, reads=["wi_", "cosT"], writes=["d2"])
                P.V("gpsimd", "tensor_tensor", xi[xi_], d1, d2, ALU.add, reads=["d1", "d2"], writes=[f"xi{xi_}"])
                P.mm(PSy[yi][:, 0:C], Cre[:, r, :], xr[xi_], rr == 0, False, reads=["Cre", f"xr{xi_}"], writes=[f"psy{yi}"])
                P.mm(PSy[yi][:, 0:C], Cim[:, r, :], xi[xi_], False, rr == 3, reads=["Cim", f"xi{xi_}"], writes=[f"psy{yi}"])
                it += 1
            P.V("vector", "scalar_tensor_tensor", yb, ub[ui], dsk[:, cb:cb + 1], PSy[yi][:, 0:C], ALU.mult, ALU.add, reads=[f"ub{ui}", "dsk", f"psy{yi}"], writes=["yb"])
            P.V("gpsimd", "tensor_tensor", g1, yb, yb, ALU.mult, reads=["yb"], writes=["g1"])
            P.V("vector", "tensor_scalar", g1, g1, 0.044715, 1.0, ALU.mult, ALU.add, reads=["g1"], writes=["g1"])
            P.V("gpsimd", "tensor_tensor", g1, g1, yb, ALU.mult, reads=["g1", "yb"], writes=["g1"])
            P.act(g2, g1, AF.Tanh, scale=float(np.sqrt(2.0 / np.pi)), reads=["g1"], writes=["g2"])
            P.V("vector", "tensor_scalar", g2, g2, 1.0, 0.5, ALU.add, ALU.mult, reads=["g2"], writes=["g2"])
            zi = (cb * NCK + ck) % 2
            P.V("gpsimd", "tensor_tensor", zo[zi], g2, yb, ALU.mult, reads=["g2", "yb"], writes=[f"zo{zi}"])
            P.dma("scalar", zg_d[cb, :, cs], zo[zi], reads=[f"zo{zi}"], final=True)
    return P


EI, EO = "ExternalInput", "ExternalOutput"

def ret_consts(head):
    log_g = np.log(1.0 - 2.0 ** (-5.0 - np.float32(head))).astype(np.float32)
    pos = np.arange(128, dtype=np.float32)
    diff = pos[None, :] - pos[:, None]
    intraT = np.where(diff >= 0, np.exp(np.maximum(diff, 0.0) * log_g), 0.0).astype(np.float32)
    xi = np.exp((pos + 1.0) * log_g).astype(np.float32)
    zeta = np.exp((127.0 - pos) * log_g).astype(np.float32)
    gch = np.exp(128.0 * log_g).astype(np.float32)
    return {"c_intraT": intraT, "c_xi": np.tile(xi[None, :], (128, 1)), "c_zg": np.stack([zeta, np.full(128, gch, np.float32)], 1).astype(np.float32),
            "c_ident": np.eye(128, dtype=np.float32)}

def build_ret(L):
    P = Prog()
    NC = L // 128
    qT_d = P.dram("qT", [2, 128, L], F32, kind=EI); kT_d = P.dram("kT", [2, 128, L], F32, kind=EI)
    cos_d = P.dram("cosr", [128, L], F32, kind=EI); sin_d = P.dram("sinr", [128, L], F32, kind=EI)
    v_d = P.dram("v", [L, 256], F32, kind=EI); g_d = P.dram("gate", [L, 256], F32, kind=EI)
    intraT_d = P.dram("c_intraT", [128, 128], F32, kind=EI); xi_d = P.dram("c_xi", [128, 128], F32, kind=EI)
    zg_d = P.dram("c_zg", [128, 2], F32, kind=EI); ident_d = P.dram("c_ident", [128, 128], F32, kind=EI)
    od_d = P.dram("od", [L, 256], F32, kind=EO)
    T = P.sb
    intraT = T("intraT", [128, 128]); xib = T("xib", [128, 128]); zg = T("zg", [128, 2]); identf = T("identf", [128, 128]); identb = T("identb", [128, 128], BF16)
    P.dma("sync", intraT, intraT_d, writes=["intraT"]); P.dma("sync", xib, xi_d, writes=["xib"]); P.dma("sync", zg, zg_d, writes=["zg"])
    P.dma("sync", identf, ident_d, writes=["identf"])
    P.V("vector", "tensor_copy", identb, identf, reads=["identf"], writes=["identb"])
    Sf = T("Sf", [128, 2, 256]); Sb = T("Sb", [128, 2, 256], BF16)
    P.V("vector", "memset", Sf, 0.0, writes=["Sf"]); P.V("vector", "memset", Sb, 0.0, writes=["Sb"])
    NB = 2
    qin = [T(f"qin{i}", [128, 2, 128]) for i in range(NB)]; kin = [T(f"kin{i}", [128, 2, 128]) for i in range(NB)]
    cs_ = [T(f"cs{i}", [128, 128]) for i in range(NB)]; sn_ = [T(f"sn{i}", [128, 128]) for i in range(NB)]
    vin = [T(f"vin{i}", [128, 256]) for i in range(NB)]; gin = [T(f"gin{i}", [128, 256]) for i in range(NB)]
    a1 = T("a1", [128, 128]); a2 = T("a2", [128, 128]); a3 = T("a3", [128, 128]); a4 = T("a4", [128, 128])
    QT = T("QT", [128, 2, 128], BF16); KT = T("KT", [128, 2, 128], BF16); QX = T("QX", [128, 2, 128], BF16); qf = T("qf", [128, 2, 128])
    Vb = T("Vb", [128, 256], BF16); attT = T("attT", [128, 128], BF16); Kz = T("Kz", [128, 256], BF16)
    osb = T("osb", [128, 256]); oc = T("oc", [128, 256]); sq = T("sq", [128, 256]); sgt = T("sgt", [128, 256])
    outb = [T(f"outb{i}", [128, 256]) for i in range(2)]
    s1 = T("s1", [128, 1]); s2 = T("s2", [128, 1]); nm = T("nm", [128, 1]); rstd = T("rstd", [128, 1])
    pA = P.ps("psA", [128, 512], F32); pO = P.ps("psO", [128, 512], F32); pS = [P.ps(f"psS{i}", [128, 512], F32) for i in range(2)]
    pT = P.ps("psT", [128, 1024], BF16)
    for c in range(NC):
        i = c % NB
        cs = slice(c * 128, (c + 1) * 128)
        P.dma("sync", qin[i], qT_d[:, :, cs].rearrange("a p t -> p a t"), writes=[f"qin{i}"])
        P.dma("scalar", kin[i], kT_d[:, :, cs].rearrange("a p t -> p a t"), writes=[f"kin{i}"])
        P.dma("gpsimd", cs_[i], cos_d[:, cs], writes=[f"cs{i}"]); P.dma("gpsimd", sn_[i], sin_d[:, cs], writes=[f"sn{i}"])
        P.dma("sync", vin[i], v_d[cs, :], writes=[f"vin{i}"]); P.dma("scalar", gin[i], g_d[cs, :], writes=[f"gin{i}"])
        def rot(xin, xkey, out_f32, okey, scale):
            P.V("vector", "tensor_tensor", a1, xin[:, 0, :], cs_[i], ALU.mult, reads=[xkey, f"cs{i}"], writes=["a1"])
            P.V("gpsimd", "tensor_tensor", a2, xin[:, 1, :], sn_[i], ALU.mult, reads=[xkey, f"sn{i}"], writes=["a2"])
            P.V("vector", "tensor_tensor", a3, xin[:, 0, :], sn_[i], ALU.mult, reads=[xkey, f"sn{i}"], writes=["a3"])
            P.V("gpsimd", "tensor_tensor", a4, xin[:, 1, :], cs_[i], ALU.mult, reads=[xkey, f"cs{i}"], writes=["a4"])
            if scale == 1.0:
                P.V("vector", "tensor_tensor", out_f32[:, 0, :], a1, a2, ALU.subtract, reads=["a1", "a2"], writes=[okey])
                P.V("gpsimd", "tensor_tensor", out_f32[:, 1, :], a3, a4, ALU.add, reads=["a3", "a4"], writes=[okey])
            else:
                P.V("vector", "scalar_tensor_tensor", out_f32[:, 0, :], a1, scale, a2, ALU.mult, ALU.subtract, reads=["a1", "a2"], writes=[okey])
        rot(qin[i], f"qin{i}", qf, "qf", 1.0)
        P.act(QT.rearrange("p a t -> p (a t)"), qf.rearrange("p a t -> p (a t)"), AF.Copy, reads=["qf"], writes=["QT"])
        P.V("vector", "tensor_tensor", QX, qf, xib.unsqueeze(1).to_broadcast([128, 2, 128]), ALU.mult, reads=["qf", "xib"], writes=["QX"])
        P.V("vector", "tensor_tensor", a1, kin[i][:, 0, :], cs_[i], ALU.mult, reads=[f"kin{i}", f"cs{i}"], writes=["a1"])
        P.V("gpsimd", "tensor_tensor", a2, kin[i][:, 1, :], sn_[i], ALU.mult, reads=[f"kin{i}", f"sn{i}"], writes=["a2"])
        P.V("vector", "tensor_tensor", a3, kin[i][:, 0, :], sn_[i], ALU.mult, reads=[f"kin{i}", f"sn{i}"], writes=["a3"])
        P.V("gpsimd", "tensor_tensor", a4, kin[i][:, 1, :], cs_[i], ALU.mult, reads=[f"kin{i}", f"cs{i}"], writes=["a4"])
        P.V("vector", "tensor_tensor", a1, a1, a2, ALU.subtract, reads=["a1", "a2"], writes=["a1"])
        P.V("gpsimd", "tensor_tensor", a3, a3, a4, ALU.add, reads=["a3", "a4"], writes=["a3"])
        P.act(KT[:, 0, :], a1, AF.Copy, scale=1.0 / 16, reads=["a1"], writes=["KT"])
        P.act(KT[:, 1, :], a3, AF.Copy, scale=1.0 / 16, reads=["a3"], writes=["KT"])
        P.V("gpsimd", "tensor_copy", Vb, vin[i], reads=[f"vin{i}"], writes=["Vb"])
        for dt in range(2):
            P.mm(pA[:, 0:128], KT[:, dt, :], QT[:, dt, :], dt == 0, dt == 1, reads=["KT", "QT"], writes=["psA"])
        P.V("vector", "tensor_tensor", attT, pA[:, 0:128], intraT, ALU.mult, reads=["psA", "intraT"], writes=["attT"])
        P.mm(pO[:, 0:256], attT, Vb, True, False, reads=["attT", "Vb"], writes=["psO"])
        for dt in range(2):
            P.mm(pO[:, 0:256], QX[:, dt, :], Sb[:, dt, :], False, dt == 1, reads=["QX", "Sb"], writes=["psO"])
        for dt in range(2):
            P.tr(pT[:, dt * 128:(dt + 1) * 128], KT[:, dt, :], identb, reads=["KT", "identb"], writes=["psT"])
        P.V("vector", "tensor_scalar", Kz, pT[:, 0:256], zg[:, 0:1], None, ALU.mult, reads=["psT", "zg"], writes=["Kz"])
        for dt in range(2):
            P.mm(pS[dt][:, 0:256], Kz[:, dt * 128:(dt + 1) * 128], Vb, True, True, reads=["Kz", "Vb"], writes=[f"psS{dt}"])
            P.V("vector", "scalar_tensor_tensor", Sf[:, dt, :], Sf[:, dt, :], zg[:, 1:2], pS[dt][:, 0:256], ALU.mult, ALU.add,
                reads=["Sf", "zg", f"psS{dt}", "psO"], writes=["Sf"])
        P.act(Sb.rearrange("p a t -> p (a t)"), Sf.rearrange("p a t -> p (a t)"), AF.Copy, reads=["Sf"], writes=["Sb"])
        P.act(osb, pO[:, 0:256], AF.Copy, accum_out=s1, reads=["psO"], writes=["osb", "s1"])
        P.V("vector", "tensor_scalar", nm, s1, -1.0 / 256, None, ALU.mult, reads=["s1"], writes=["nm"])
        P.V("vector", "tensor_scalar", oc, osb, nm, None, ALU.add, reads=["osb", "nm"], writes=["oc"])
        P.act(sq, oc, AF.Square, accum_out=s2, reads=["oc"], writes=["sq", "s2"])
        P.V("vector", "tensor_scalar", s2, s2, 1.0 / 256, 1e-5, ALU.mult, ALU.add, reads=["s2"], writes=["s2"])
        P.act(s2, s2, AF.Sqrt, reads=["s2"], writes=["s2"])
        P.V("vector", "reciprocal", rstd, s2, reads=["s2"], writes=["rstd"])
        P.act(sgt, gin[i], AF.Silu, reads=[f"gin{i}"], writes=["sgt"])
        ob = outb[c % 2]
        P.V("vector", "scalar_tensor_tensor", ob, oc, rstd, sgt, ALU.mult, ALU.mult, reads=["oc", "rstd", "sgt"], writes=[f"outb{c%2}"])
        P.dma("sync", od_d[cs, :], ob, reads=[f"outb{c%2}"], final=True)
    return P


D_MODEL = 4096; SEQ = 8192; D_FF = 11008
ROPE_THETA = 500000.0

def _vec(v):
    return np.ascontiguousarray(np.asarray(v, np.float32).reshape(-1, 128).T)

def _launch(P, ins):
    nc = P.build()
    res = run_bass_kernel_spmd(nc, ins, core_ids=list(range(len(ins))))
    return res.results

def _rot_tables(L, inv):
    ang = np.arange(L, dtype=np.float32)[:, None] * inv[None, :].astype(np.float32)
    c = np.cos(ang).astype(np.float32); s = np.sin(ang).astype(np.float32)
    return np.ascontiguousarray(np.concatenate([c, c], 1).T), np.ascontiguousarray(np.concatenate([s, s], 1).T)

def _inv_freq(rot):
    return (np.float32(ROPE_THETA) ** (-np.arange(0, rot, 2, dtype=np.float32) / np.float32(rot))).astype(np.float32)

def _dsa_blocks(c, L):
    return [8 * j + (c if j % 2 == 0 else 7 - c) for j in range(L // 1024)]

def _dsa_inputs(q0, k0, v0, qi0, ki0, wi0, c, L, tabs):
    (ca, sa), (ci, si) = tabs
    d = {}
    tok = [np.arange(b * 128, b * 128 + 128) for b in _dsa_blocks(c, L)]
    d["qT"] = np.ascontiguousarray(np.stack([q0[t].transpose(1, 2, 0) for t in tok]))
    d["cosq"] = np.ascontiguousarray(np.stack([ca[:, t] for t in tok])); d["sinq"] = np.ascontiguousarray(np.stack([sa[:, t] for t in tok]))
    d["qiT"] = np.ascontiguousarray(np.stack([qi0[t].transpose(1, 2, 0).reshape(16, 128, 128) for t in tok]))
    d["cosqi"] = np.ascontiguousarray(np.stack([ci[:, t] for t in tok])); d["sinqi"] = np.ascontiguousarray(np.stack([si[:, t] for t in tok]))
    d["wi"] = np.ascontiguousarray(np.stack([wi0[t] for t in tok]))
    d["qpos"] = np.ascontiguousarray(np.stack([t.astype(np.float32)[:, None] for t in tok]))
    return d

def _rwkv_inputs(zrT, prm, core, L):
    Dh = 2048
    def sec(off, n):
        a = zrT[off:off + n]
        return np.concatenate([np.zeros((n, 1), np.float32), a], axis=1)
    ch = slice(core * 256, core * 256 + 256)
    d = {}
    for i, nme in enumerate("rkv"):
        d["z" + nme] = np.ascontiguousarray(sec(i * Dh + core * 256, 256).reshape(2, 128, L + 1))
    d["zg"] = None
    mu = prm["e_mu"]
    cols = [mu[0:Dh][ch], mu[Dh:2 * Dh][ch], mu[2 * Dh:3 * Dh][ch], mu[3 * Dh + 192:3 * Dh + 448],
            prm["e_w0"][ch], prm["e_a0"][ch], prm["e_k_k"][ch], prm["e_k_a"][ch], prm["e_r_k"].reshape(-1)[ch]]
    d["par"] = np.ascontiguousarray(np.stack(cols, axis=1).reshape(2, 128, 9).astype(np.float32))
    d["wup"] = np.ascontiguousarray(prm["e_w_up"][:, ch]); d["aup"] = np.ascontiguousarray(prm["e_a_up"][:, ch])
    d["gup"] = np.ascontiguousarray(prm["e_g_up"][:, ch].reshape(2, 128, 256))
    d["lnwb"] = np.ascontiguousarray(np.stack([prm["e_ln_w"][ch], prm["e_ln_b"][ch]], axis=0))
    return d

def _s5_inputs(prm, c, C=512):
    d = {}
    Bre = np.zeros((8, 128, 128), np.float32); Bim = np.zeros_like(Bre); Cre = np.zeros_like(Bre); Cim = np.zeros_like(Bre)
    lam = np.zeros((128, 8, 3), np.float32)
    for r in range(8):
        for s in range(2):
            g = c * 16 + r * 2 + s
            gl = (r % 4) * 2 + s
            Bre[r, 16 * gl:16 * gl + 16, 64 * s:64 * s + 64] = prm["o_b_re"][g].T
            Bim[r, 16 * gl:16 * gl + 16, 64 * s:64 * s + 64] = prm["o_b_im"][g].T
            Cre[r, 64 * s:64 * s + 64, 16 * gl:16 * gl + 16] = prm["o_c_re"][g].T
            Cim[r, 64 * s:64 * s + 64, 16 * gl:16 * gl + 16] = prm["o_c_im"][g].T
            lam[64 * s:64 * s + 64, r, 0] = prm["o_lam_re"][g]; lam[64 * s:64 * s + 64, r, 1] = prm["o_lam_im"][g]
            lam[64 * s:64 * s + 64, r, 2] = prm["o_log_step"][g]
    d["Bre"] = Bre; d["Bim"] = Bim; d["Cre"] = Cre; d["Cim"] = Cim; d["lam"] = lam
    d["dsk"] = np.ascontiguousarray(prm["o_d_skip"][c * 256:(c + 1) * 256].reshape(2, 128).T)
    d["c_iota"] = np.tile(np.arange(C + 1, dtype=np.float32)[None, :], (128, 1))
    return d

def kernel(**inp):
    A = {k: np.asarray(v, np.float32) for k, v in inp.items()}
    L = SEQ; NCORE = 8; TC = L // NCORE
    x = A["x"][0]
    xT = np.ascontiguousarray(x.T)
    tsl = [slice(c * TC, (c + 1) * TC) for c in range(NCORE)]
    P = build_dense(dict(D=D_MODEL, FF=D_FF, TC=TC, TB=512, mode="in0", NIN=11808, kchunk=8))
    w_in0 = np.ascontiguousarray(A["e_w_in"][0]); gm0 = _vec(A["norm_mix"][0])
    r = _launch(P, [{"hT": np.ascontiguousarray(xT[:, tsl[c]]), "g_mix": gm0, "w_in": w_in0} for c in range(NCORE)])
    z0T = np.concatenate([r[c]["zT"] for c in range(NCORE)], axis=1)
    del r
    q0 = np.ascontiguousarray(z0T[0:2048].T).reshape(L, 16, 128)
    qi0 = np.ascontiguousarray(z0T[3072:5120].T).reshape(L, 32, 64)
    wi0 = np.ascontiguousarray(z0T[5184:5216].T)
    tabs = (_rot_tables(L, _inv_freq(32)), _rot_tables(L, _inv_freq(16)))
    common = {"kT": np.ascontiguousarray(z0T[2048:2560].reshape(4, 128, L)), "cosk": tabs[0][0], "sink": tabs[0][1],
              "kiT": np.ascontiguousarray(z0T[5120:5184]), "coski": tabs[1][0], "sinki": tabs[1][1],
              "v": np.ascontiguousarray(z0T[2560:3072].T)}
    common.update(dsa_consts())
    P = build_dsa(L)
    ins = []
    for c in range(NCORE):
        d = _dsa_inputs(q0, None, None, qi0, None, wi0, c, L, tabs); d.update(common); ins.append(d)
    r = _launch(P, ins)
    o_a = np.zeros((L, 2048), np.float32)
    for c in range(NCORE):
        for j, b in enumerate(_dsa_blocks(c, L)):
            o_a[b * 128:(b + 1) * 128] = r[c]["oa"][j]
    del r, ins, q0, qi0
    prm = {k: A[k][0] for k in ["e_mu", "e_w0", "e_w_up", "e_a0", "e_a_up", "e_g_up", "e_k_k", "e_k_a", "e_r_k", "e_ln_w", "e_ln_b"]}
    zrT = z0T[5216:11808]
    def pad(a):
        return np.concatenate([np.zeros((a.shape[0], 1), np.float32), a], axis=1)
    zw = np.ascontiguousarray(pad(zrT[6144:6240])); za = np.ascontiguousarray(pad(zrT[6240:6336]))
    zg = np.ascontiguousarray(pad(zrT[6336:6592]).reshape(2, 128, L + 1))
    mu = prm["e_mu"]
    parw = np.ascontiguousarray(np.stack([mu[6144:6240], mu[6240:6336]], axis=1))
    cst = rwkv_consts()
    ins = []
    for c in range(NCORE):
        d = _rwkv_inputs(zrT, prm, c, L); d["zg"] = zg; d["zw"] = zw; d["za"] = za; d["parw"] = parw; d.update(cst); ins.append(d)
    P = build_rwkv(L)
    r = _launch(P, ins)
    o_b = np.concatenate([r[c]["ob"] for c in range(NCORE)], axis=1)
    del r, ins, z0T
    oT = np.ascontiguousarray(np.concatenate([o_a, o_b], axis=1).T)
    P = build_dense(dict(D=D_MODEL, FF=D_FF, TC=TC, TB=512, mode="mid", NIN=10240, kchunk=8))
    wts = {"g_ffn": _vec(A["norm_ffn"][0]), "w_out": np.ascontiguousarray(A["e_w_out"][0]), "w_gate": np.ascontiguousarray(A["ffn_gate"][0]),
           "w_up": np.ascontiguousarray(A["ffn_up"][0]), "w_down": np.ascontiguousarray(A["ffn_down"][0]),
           "g_mix": _vec(A["norm_mix"][1]), "w_in": np.ascontiguousarray(A["o_w_in"][0])}
    ins = []
    for c in range(NCORE):
        d = {"hT": np.ascontiguousarray(xT[:, tsl[c]]), "oT": np.ascontiguousarray(oT[:, tsl[c]])}; d.update(wts); ins.append(d)
    r = _launch(P, ins)
    h2T = np.concatenate([r[c]["h2T"] for c in range(NCORE)], axis=1)
    z1T = np.concatenate([r[c]["zT"] for c in range(NCORE)], axis=1)
    del r, ins, wts, oT
    prm = {k: A[k][0] for k in ["o_lam_re", "o_lam_im", "o_log_step", "o_b_re", "o_b_im", "o_c_re", "o_c_im", "o_d_skip"]}
    ins = []
    for c in range(NCORE):
        d = _s5_inputs(prm, c); d["uT"] = np.ascontiguousarray(z1T[c * 256:(c + 1) * 256].reshape(2, 128, L)); ins.append(d)
    P = build_s5(L)
    r = _launch(P, ins)
    zgT = np.concatenate([r[c]["zgT"].reshape(256, L) for c in range(NCORE)], axis=0)
    del r, ins
    inv = (1.0 / (np.float32(10000.0) ** np.linspace(0.0, 1.0, 128, dtype=np.float32))).astype(np.float32)
    ang = np.arange(L, dtype=np.float32)[:, None] * inv[None, :]
    cosr = np.ascontiguousarray(np.cos(ang).astype(np.float32).T); sinr = np.ascontiguousarray(np.sin(ang).astype(np.float32).T)
    ins = []
    for c in range(NCORE):
        ch = slice(c * 256, (c + 1) * 256)
        d = {"qT": np.ascontiguousarray(z1T[2048:4096][ch].reshape(2, 128, L)), "kT": np.ascontiguousarray(z1T[4096:6144][ch].reshape(2, 128, L)),
             "v": np.ascontiguousarray(z1T[6144:8192][ch].T), "gate": np.ascontiguousarray(z1T[8192:10240][ch].T), "cosr": cosr, "sinr": sinr}
        d.update(ret_consts(c)); ins.append(d)
    P = build_ret(L)
    r = _launch(P, ins)
    odT = np.concatenate([np.ascontiguousarray(r[c]["od"].T) for c in range(NCORE)], axis=0)
    del r, ins, z1T
    oT = np.ascontiguousarray(np.concatenate([zgT, odT], axis=0))
    P = build_dense(dict(D=D_MODEL, FF=D_FF, TC=TC, TB=512, mode="last", G=2048, kchunk=8))
    wts = {"g_ffn": _vec(A["norm_ffn"][1]), "w_out": np.ascontiguousarray(A["o_w_out"][0]), "w_gate": np.ascontiguousarray(A["ffn_gate"][1]),
           "w_up": np.ascontiguousarray(A["ffn_up"][1]), "w_down": np.ascontiguousarray(A["ffn_down"][1]),
           "g_fin": _vec(A["final_norm"]), "w_glu": np.ascontiguousarray(A["o_w_glu"][0]), "b_glu": _vec(A["o_b_glu"][0])}
    ins = []
    for c in range(NCORE):
        d = {"hT": np.ascontiguousarray(h2T[:, tsl[c]]), "oT": np.ascontiguousarray(oT[:, tsl[c]])}; d.update(wts); ins.append(d)
    r = _launch(P, ins)
    yT = np.concatenate([r[c]["yT"] for c in range(NCORE)], axis=1)
    return np.ascontiguousarray(yT.T).reshape(1, L, D_MODEL).astype(np.float32)
```

```python
import numpy as np
import concourse.bass as bass
import concourse.mybir as mybir
from concourse.bass_utils import run_bass_kernel_spmd

F32 = mybir.dt.float32
BF16 = mybir.dt.bfloat16
I32 = mybir.dt.int32
U32 = mybir.dt.uint32
AF = mybir.ActivationFunctionType
ALU = mybir.AluOpType
AX = mybir.AxisListType


class Prog:
    ENG = ("sync", "scalar", "vector", "gpsimd", "tensor")
    NDMA = 6

    def __init__(self, name="k"):
        self.nc = bass.Bass("TRN2", target_bir_lowering=False)
        self.ops = {e: [] for e in self.ENG}
        self.cnt = {}
        self.lastw = {}
        self.readers = {}
        self.waited = {e: {} for e in self.ENG}
        self.dma_rr = {e: 0 for e in self.ENG}
        self.dma_out = {}
        self.n_inst = 0
        self.tail_waits = []

    def dram(self, name, shape, dt=F32, kind="Internal"):
        return self.nc.dram_tensor(name, list(shape), dt, kind=kind).ap()

    def sb(self, name, shape, dt=F32):
        return self.nc.alloc_sbuf_tensor("sb_" + name, list(shape), dt).ap()

    def ps(self, name, shape, dt=F32):
        return self.nc.alloc_psum_tensor("pp_" + name, list(shape), dt).ap()

    def _need(self, eng, dep, waits):
        if dep is None:
            return
        sk, val = dep
        if self.waited[eng].get(sk, 0) >= val:
            return
        waits[sk] = max(waits.get(sk, 0), val)

    def op(self, eng, fn, reads=(), writes=(), dma=False, pe_acc=False, final=False):
        waits = {}
        reads = [k for k in reads if k is not None]
        writes = list(writes)
        if eng != "tensor":
            for k in reads:
                if isinstance(k, str) and "ps" in k and k not in writes:
                    writes.append(k)
        for k in reads:
            self._need(eng, self.lastw.get(k), waits)
        for k in writes:
            lw = self.lastw.get(k)
            if not (pe_acc and lw is not None and lw[0] == "tensor" and eng == "tensor"):
                self._need(eng, lw, waits)
            for rd in self.readers.get(k, ()):
                self._need(eng, rd, waits)
        if dma:
            slot = self.dma_rr[eng]
            self.dma_rr[eng] = (slot + 1) % self.NDMA
            sk = ("dma", eng, slot)
            prev = self.dma_out.get(sk)
            self._need(eng, prev, waits)
            inc = 16
        else:
            sk = eng
            inc = 1
        val = self.cnt.get(sk, 0) + inc
        self.cnt[sk] = val
        if dma:
            self.dma_out[sk] = (sk, val)
        for s, v in waits.items():
            self.waited[eng][s] = max(self.waited[eng].get(s, 0), v)
        self.ops[eng].append((fn, sorted(waits.items(), key=str), sk, inc))
        for k in writes:
            self.lastw[k] = (sk, val)
            self.readers[k] = []
        for k in reads:
            if k not in writes:
                self.readers.setdefault(k, []).append((sk, val))
        if final:
            self.tail_waits.append((sk, val))
        self.n_inst += 1
        return (sk, val)

    def dma(self, eng, out, in_, reads=(), writes=(), final=False, **kw):
        return self.op(eng, lambda e: e.dma_start(out=out, in_=in_, **kw), reads, writes, dma=True, final=final)

    def mm(self, out, lhsT, rhs, start, stop, reads=(), writes=()):
        return self.op("tensor", lambda e: e.matmul(out, lhsT, rhs, start=start, stop=stop),
                       reads, writes, pe_acc=True)

    def tr(self, out, in_, ident, reads=(), writes=()):
        return self.op("tensor", lambda e: e.transpose(out, in_, ident), reads, writes, pe_acc=True)

    def act(self, out, in_, func, reads=(), writes=(), eng="scalar", **kw):
        return self.op(eng, lambda e: e.activation(out, in_, func, **kw), reads, writes)

    def V(self, eng, meth, *args, reads=(), writes=(), **kw):
        return self.op(eng, lambda e: getattr(e, meth)(*args, **kw), reads, writes)

    def build(self):
        nc = self.nc
        semkeys = list(self.cnt.keys())
        sems = {}
        import contextlib
        with contextlib.ExitStack() as st:
            for i, sk in enumerate(semkeys):
                sems[sk] = st.enter_context(nc.semaphore("s%d" % i))
            block = st.enter_context(nc.Block())

            def emit(engname):
                def body(e):
                    for fn, waits, sk, inc in self.ops[engname]:
                        for s, v in waits:
                            e.wait_ge(sems[s], v)
                        fn(e).then_inc(sems[sk], inc)
                    if engname == "sync":
                        for s, v in self.tail_waits:
                            e.wait_ge(sems[s], v)
                return body
            block.sync(emit("sync"))
            block.scalar(emit("scalar"))
            block.vector(emit("vector"))
            block.gpsimd(emit("gpsimd"))
            block.tensor(emit("tensor"))
        return nc


def run(prog, in_maps, trace=False):
    nc = prog.build()
    res = run_bass_kernel_spmd(nc, in_maps, core_ids=list(range(len(in_maps))), trace=trace)
    return res


def dense(P, tag, XT, KT, T, W, N, epi, kchunk=32, cast_engs=("gpsimd", "vector"), nstage=2, npsum=2, xkey=None, ps_tiles=None, n_off=0):
    Wv = W.rearrange("(kt p) n -> p kt n", p=128)
    nk = (KT + kchunk - 1) // kchunk
    stg = [P.sb(f"{tag}_stg{i}", [128, kchunk, 128], F32) for i in range(nstage)]
    wbf = [P.sb(f"{tag}_wbf{i}", [128, kchunk, 128], BF16) for i in range(nstage)]
    if ps_tiles is None:
        ps_tiles = [P.ps(f"{tag}_ps{i}", [128, 512], F32) for i in range(npsum)]
    NT = (N + 127) // 128
    it = 0
    dq = ("sync", "scalar")
    for nt in range(NT):
        n0 = nt * 128
        nsz = min(128, N - n0)
        pi = nt % len(ps_tiles)
        pst = ps_tiles[pi]
        pskey = f"{tag}_ps{pi}"
        for kc in range(nk):
            k0 = kc * kchunk
            ksz = min(kchunk, KT - k0)
            si = it % nstage
            P.dma(dq[it % 2], stg[si][:, :ksz, :nsz], Wv[:, k0:k0 + ksz, n_off + n0:n_off + n0 + nsz],
                  writes=[f"{tag}_stg{si}"])
            ce = cast_engs[it % len(cast_engs)]
            P.V(ce, "tensor_copy", wbf[si][:, :ksz, :nsz], stg[si][:, :ksz, :nsz],
                reads=[f"{tag}_stg{si}"], writes=[f"{tag}_wbf{si}"])
            for kk in range(ksz):
                P.mm(pst[:nsz, :T], wbf[si][:, kk, :nsz], XT[:, k0 + kk, :T],
                     start=(kc == 0 and kk == 0), stop=(kc == nk - 1 and kk == ksz - 1),
                     reads=[f"{tag}_wbf{si}"] + ([xkey] if xkey else []), writes=[pskey])
            it += 1
        epi(nt, nsz, pst[:nsz, :T], pskey)


EI, EO = "ExternalInput", "ExternalOutput"

class Streamer:
    NBUF = 4
    AHEAD = 3
    def __init__(self, P, kchunk, T):
        self.P = P; self.kc = kchunk; self.T = T
        self.stg = [P.sb(f"w_stg{i}", [128, kchunk, 128], F32) for i in range(self.NBUF)]
        self.wbf = [P.sb(f"w_bf{i}", [128, kchunk, 128], BF16) for i in range(self.NBUF)]
        self.it = 0

    def run(self, jobs):
        P = self.P; T = self.T
        Q = []
        for jb in jobs:
            nk = (jb["KT"] + self.kc - 1) // self.kc
            for kc in range(nk):
                Q.append((jb, kc, nk))
        base = self.it
        def load(q):
            jb, kc, nk = Q[q]
            Wv = jb["W"].rearrange("(kt p) n -> p kt n", p=128)
            k0 = kc * self.kc; ksz = min(self.kc, jb["KT"] - k0); nsz = jb["nsz"]; n0 = jb["n0"]
            si = (base + q) % self.NBUF
            if kc == 0 and jb.get("pre") is not None:
                jb["pre"]()
            P.dma("sync", self.stg[si][:, :ksz, :nsz], Wv[:, k0:k0 + ksz, n0:n0 + nsz], writes=[f"w_stg{si}"])
            ce = ("gpsimd", "vector")[(base + q) % 2]
            P.V(ce, "tensor_copy", self.wbf[si][:, :ksz, :nsz], self.stg[si][:, :ksz, :nsz], reads=[f"w_stg{si}"], writes=[f"w_bf{si}"])
        def mm(q):
            jb, kc, nk = Q[q]
            k0 = kc * self.kc; ksz = min(self.kc, jb["KT"] - k0); nsz = jb["nsz"]
            si = (base + q) % self.NBUF
            for kk in range(ksz):
                P.mm(jb["pst"][:nsz, :T], self.wbf[si][:, kk, :nsz], jb["XT"][:, k0 + kk, :T], kc == 0 and kk == 0, kc == nk - 1 and kk == ksz - 1,
                     reads=[f"w_bf{si}", jb["xkey"]], writes=[jb["pskey"]])
            if kc == nk - 1 and jb.get("epi") is not None:
                jb["epi"]()
        n = len(Q)
        for q in range(n + self.AHEAD):
            p = q - self.AHEAD
            if p >= 0:
                mm(p)
            if q < n:
                load(q)
        self.it += n

def build_dense(cfg):
    P = Prog()
    D, FF, TC, TB = cfg["D"], cfg["FF"], cfg["TC"], cfg.get("TB", 512)
    mode = cfg["mode"]; NIN = cfg.get("NIN", 0); G = cfg.get("G", 0)
    DT, FT = D // 128, (FF + 127) // 128
    assert FF % 128 == 0
    NBLK = TC // TB
    T = P.sb
    hT_d = P.dram("hT", [D, TC], F32, kind=EI)
    ones = T("ones", [128, 128]); P.V("vector", "memset", ones, 1.0, writes=["ones"])
    S = Streamer(P, cfg.get("kchunk", 8), TB)
    PSA = [P.ps(f"psa{i}", [128, 512], F32) for i in range(2)]
    PSB = [P.ps(f"psb{i}", [128, 512], F32) for i in range(2)]
    PSS = P.ps("pss", [128, 512], F32)
    hn = T("hn", [128, DT, TB], BF16)
    rstd = T("rstd", [128, TB]); xt = [T(f"xt{i}", [128, TB]) for i in range(2)]; xsq = [T(f"xsq{i}", [128, TB]) for i in range(2)]
    ev = [T(f"ev{i}", [128, TB]) for i in range(2)]; ev2 = [T(f"evb{i}", [128, TB]) for i in range(2)]
    rs = [T(f"rs{i}", [128, TB]) for i in range(2)]
    cnt = [0]

    def norm(src_d, skey, gam, gkey, tb, out_dram=None):
        cs = slice(tb * TB, (tb + 1) * TB)
        for kt in range(DT):
            i = cnt[0] % 2; cnt[0] += 1
            P.dma("sync", xt[i], src_d[kt * 128:(kt + 1) * 128, cs], reads=[skey], writes=[f"xt{i}"])
            P.V("gpsimd", "tensor_tensor", xsq[i], xt[i], xt[i], ALU.mult, reads=[f"xt{i}"], writes=[f"xsq{i}"])
            P.mm(PSS[:, :TB], ones, xsq[i], kt == 0, kt == DT - 1, reads=["ones", f"xsq{i}"], writes=["pss"])
        P.act(rstd, PSS[:, :TB], AF.Sqrt, scale=1.0 / D, bias=epsb, reads=["pss", "epsb"], writes=["rstd"])
        P.V("vector", "reciprocal", rstd, rstd, reads=["rstd"], writes=["rstd"])
        for kt in range(DT):
            i = cnt[0] % 2; cnt[0] += 1
            P.dma("sync", xt[i], src_d[kt * 128:(kt + 1) * 128, cs], reads=[skey], writes=[f"xt{i}"])
            if out_dram is None:
                P.V("vector", "scalar_tensor_tensor", hn[:, kt, :], xt[i], gam[:, kt:kt + 1], rstd, ALU.mult, ALU.mult,
                    reads=[f"xt{i}", gkey, "rstd"], writes=["hn"])
            else:
                P.V("vector", "scalar_tensor_tensor", ev[i], xt[i], gam[:, kt:kt + 1], rstd, ALU.mult, ALU.mult,
                    reads=[f"xt{i}", gkey, "rstd"], writes=[f"ev{i}"])
                P.dma("scalar", out_dram[kt * 128:(kt + 1) * 128, cs], ev[i], reads=[f"ev{i}"], final=True)

    epsb = T("epsb", [128, 1]); P.V("vector", "memset", epsb, 1e-6, writes=["epsb"])
    def loadvec(name, n):
        d = P.dram(name, [128, n], F32, kind=EI); t = T("s_" + name, [128, n]); P.dma("sync", t, d, writes=["s_" + name]); return t, "s_" + name

    def inproj(Wd, N, zT_d, tb):
        cs = slice(tb * TB, (tb + 1) * TB)
        jobs = []
        for nt in range((N + 127) // 128):
            n0 = nt * 128; nsz = min(128, N - n0); pi = nt % 2
            def epi(n0=n0, nsz=nsz, pi=pi):
                P.act(ev[pi][:nsz], PSA[pi][:nsz, :TB], AF.Copy, reads=[f"psa{pi}"], writes=[f"ev{pi}"])
                P.dma("scalar", zT_d[n0:n0 + nsz, cs], ev[pi][:nsz], reads=[f"ev{pi}"], final=True)
            jobs.append(dict(W=Wd, KT=DT, n0=n0, nsz=nsz, XT=hn, xkey="hn", pst=PSA[pi], pskey=f"psa{pi}", epi=epi))
        S.run(jobs)

    if mode == "in0":
        g_mix, gk = loadvec("g_mix", DT)
        Win = P.dram("w_in", [D, NIN], F32, kind=EI)
        zT_d = P.dram("zT", [NIN, TC], F32, kind=EO)
        for tb in range(NBLK):
            norm(hT_d, None, g_mix, gk, tb)
            inproj(Win, NIN, zT_d, tb)
        return P

    g_ffn, gfk = loadvec("g_ffn", DT)
    Wout = P.dram("w_out", [D, D], F32, kind=EI)
    Wg = P.dram("w_gate", [D, FF], F32, kind=EI); Wu = P.dram("w_up", [D, FF], F32, kind=EI); Wd = P.dram("w_down", [FF, D], F32, kind=EI)
    oT_d = P.dram("oT", [D, TC], F32, kind=EI)
    h1_d = P.dram("h1s", [D, TC], F32)
    oT = T("oT", [128, DT, TB], BF16)
    aT = T("aT", [128, FT, TB], BF16)
    if mode == "mid":
        g_mix, gk = loadvec("g_mix", DT)
        Win = P.dram("w_in", [D, NIN], F32, kind=EI)
        zT_d = P.dram("zT", [NIN, TC], F32, kind=EO)
        h2_d = P.dram("h2T", [D, TC], F32, kind=EO)
    else:
        g_fin, gfin = loadvec("g_fin", DT)
        Wglu = P.dram("w_glu", [G, G], F32, kind=EI)
        bglu, bgk = loadvec("b_glu", G // 128)
        h2_d = P.dram("h2s", [D, TC], F32)
        y_d = P.dram("yT", [D, TC], F32, kind=EO)
        zgb = aT[:, 0:G // 128, :]

    for tb in range(NBLK):
        cs = slice(tb * TB, (tb + 1) * TB)
        for kt in range(DT):
            i = cnt[0] % 2; cnt[0] += 1
            P.dma(("sync", "scalar")[i], xt[i], oT_d[kt * 128:(kt + 1) * 128, cs], writes=[f"xt{i}"])
            if mode == "last" and kt < G // 128:
                P.V("vector", "tensor_copy", zgb[:, kt, :], xt[i], reads=[f"xt{i}"], writes=["aT"])
            else:
                P.V("vector", "tensor_copy", oT[:, kt, :], xt[i], reads=[f"xt{i}"], writes=["oT"])
        if mode == "last":
            jobs = []
            for nt in range(G // 128):
                pi = nt % 2
                def pre(nt=nt, pi=pi):
                    P.dma("sync", rs[pi], oT_d[nt * 128:(nt + 1) * 128, cs], writes=[f"rs{pi}"])
                def epi(nt=nt, pi=pi):
                    P.act(ev[pi], PSA[pi][:, :TB], AF.Sigmoid, bias=bglu[:, nt:nt + 1], reads=[f"psa{pi}", bgk], writes=[f"ev{pi}"])
                    P.V("vector", "tensor_tensor", oT[:, nt, :], ev[pi], rs[pi], ALU.mult, reads=[f"ev{pi}", f"rs{pi}"], writes=["oT"])
                jobs.append(dict(W=Wglu, KT=G // 128, n0=nt * 128, nsz=128, XT=zgb, xkey="aT", pst=PSA[pi], pskey=f"psa{pi}", epi=epi, pre=pre))
            S.run(jobs)
        jobs = []
        for nt in range(DT):
            pi = nt % 2
            def pre(nt=nt, pi=pi):
                P.dma("sync", rs[pi], hT_d[nt * 128:(nt + 1) * 128, cs], writes=[f"rs{pi}"])
            def epi(nt=nt, pi=pi):
                P.V("vector", "tensor_tensor", ev[pi], PSA[pi][:, :TB], rs[pi], ALU.add, reads=[f"psa{pi}", f"rs{pi}"], writes=[f"ev{pi}"])
                P.dma("scalar", h1_d[nt * 128:(nt + 1) * 128, cs], ev[pi], reads=[f"ev{pi}"], writes=["h1s"])
            jobs.append(dict(W=Wout, KT=DT, n0=nt * 128, nsz=128, XT=oT, xkey="oT", pst=PSA[pi], pskey=f"psa{pi}", epi=epi, pre=pre))
        S.run(jobs)
        norm(h1_d, "h1s", g_ffn, gfk, tb)
        jobs = []
        for ft in range(FT):
            pi = ft % 2
            def epi_g(ft=ft, pi=pi):
                P.act(ev[pi], PSA[pi][:, :TB], AF.Silu, reads=[f"psa{pi}"], writes=[f"ev{pi}"])
            def epi(ft=ft, pi=pi):
                P.V("vector", "tensor_tensor", aT[:, ft, :], ev[pi], PSB[pi][:, :TB], ALU.mult, reads=[f"ev{pi}", f"psb{pi}"], writes=["aT"])
            jobs.append(dict(W=Wg, KT=DT, n0=ft * 128, nsz=128, XT=hn, xkey="hn", pst=PSA[pi], pskey=f"psa{pi}", epi=epi_g))
            jobs.append(dict(W=Wu, KT=DT, n0=ft * 128, nsz=128, XT=hn, xkey="hn", pst=PSB[pi], pskey=f"psb{pi}", epi=epi))
        S.run(jobs)
        dst = h2_d
        jobs = []
        for nt in range(DT):
            pi = nt % 2
            def pre(nt=nt, pi=pi):
                P.dma("sync", rs[pi], h1_d[nt * 128:(nt + 1) * 128, cs], reads=["h1s"], writes=[f"rs{pi}"])
            def epi(nt=nt, pi=pi):
                P.V("vector", "tensor_tensor", ev2[pi], PSA[pi][:, :TB], rs[pi], ALU.add, reads=[f"psa{pi}", f"rs{pi}"], writes=[f"evb{pi}"])
                P.dma("scalar", dst[nt * 128:(nt + 1) * 128, cs], ev2[pi], reads=[f"evb{pi}"], writes=["h2"], final=(mode == "mid"))
            jobs.append(dict(W=Wd, KT=FT, n0=nt * 128, nsz=128, XT=aT, xkey="aT", pst=PSA[pi], pskey=f"psa{pi}", epi=epi, pre=pre))
        S.run(jobs)
        if mode == "mid":
            norm(h2_d, "h2", g_mix, gk, tb)
            inproj(Win, NIN, zT_d, tb)
        else:
            norm(h2_d, "h2", g_fin, gfin, tb, out_dram=y_d)
    return P


EI, EO = "ExternalInput", "ExternalOutput"

def rwkv_consts():
    ident = np.eye(128, dtype=np.float32)
    blk = np.zeros((128, 128), np.float32); blk[:64, :64] = 1; blk[64:, 64:] = 1
    su = np.triu(np.ones((64, 64), np.float32), 1)
    ui = np.triu(np.ones((64, 64), np.float32), 0)
    sl = su.T.copy()
    m = np.zeros((2, 64, 4, 4, 64), np.float32)
    for h2 in range(2):
        for g in range(4):
            m[h2, :, 0, g] = su; m[h2, :, 1, g] = ui; m[h2, :, 2, g] = sl; m[h2, :, 3, g] = np.eye(64)
    ind = np.zeros((128, 2), np.float32); ind[:64, 0] = 1; ind[64:, 1] = 1
    return {"c_ident": ident, "c_blk": blk, "c_masks": m.reshape(128, 16 * 64), "c_ind": ind}

def build_rwkv(L, TS=1024, stage=99):
    P = Prog()
    CH = 64
    NSEG = L // TS; NCH = TS // CH
    zin = {n: P.dram("z" + n, [2, 128, L + 1], F32, kind=EI) for n in "rkvg"}
    zw = P.dram("zw", [96, L + 1], F32, kind=EI)
    za = P.dram("za", [96, L + 1], F32, kind=EI)
    par_d = P.dram("par", [2, 128, 9], F32, kind=EI)
    parw_d = P.dram("parw", [96, 2], F32, kind=EI)
    wup_d = P.dram("wup", [96, 256], F32, kind=EI)
    aup_d = P.dram("aup", [96, 256], F32, kind=EI)
    gup_d = P.dram("gup", [2, 128, 256], F32, kind=EI)
    lnwb_d = P.dram("lnwb", [2, 256], F32, kind=EI)
    c_ident = P.dram("c_ident", [128, 128], F32, kind=EI)
    c_blk = P.dram("c_blk", [128, 128], F32, kind=EI)
    c_masks = P.dram("c_masks", [128, 16 * 64], F32, kind=EI)
    c_ind = P.dram("c_ind", [128, 2], F32, kind=EI)
    ob = P.dram("ob", [L, 256], F32, kind=EO)

    def T(name, shape, dt=F32):
        return P.sb(name, shape, dt)
    ident = T("ident", [128, 128]); blk = T("blk", [128, 128]); masks = T("masks", [128, 4, 4, 64]); ind = T("ind", [128, 2])
    par = [T(f"par{g}", [128, 9]) for g in range(2)]
    parw = T("parw", [96, 2]); wup = T("wup", [96, 256]); aup = T("aup", [96, 256])
    gup = [T(f"gup{g}", [128, 256]) for g in range(2)]
    lnw = T("lnw", [128, 2, 64]); lnb = T("lnb", [128, 2, 64])
    ones = T("ones", [128, 64])
    P.dma("sync", ident, c_ident, writes=["ident"]); P.dma("sync", blk, c_blk, writes=["blk"])
    P.dma("sync", masks.rearrange("p a g s -> p (a g s)"), c_masks, writes=["masks"]); P.dma("sync", ind, c_ind, writes=["ind"])
    for g in range(2):
        P.dma("scalar", par[g], par_d[g], writes=[f"par{g}"])
        P.dma("scalar", gup[g], gup_d[g], writes=[f"gup{g}"])
    P.dma("scalar", parw, parw_d, writes=["parw"]); P.dma("scalar", wup, wup_d, writes=["wup"]); P.dma("scalar", aup, aup_d, writes=["aup"])
    lv = lnwb_d.rearrange("a (g h v) -> a h g v", g=2, h=2)
    for h2 in range(2):
        P.dma("gpsimd", lnw[64 * h2:64 * h2 + 64], lv[0:1, h2].partition_broadcast(64), writes=["lnw"])
        P.dma("gpsimd", lnb[64 * h2:64 * h2 + 64], lv[1:2, h2].partition_broadcast(64), writes=["lnb"])
    P.V("vector", "memset", ones, 1.0, writes=["ones"])
    msu = masks[:, 0]; mui = masks[:, 1]; msl = masks[:, 2]; mid = masks[:, 3]

    inp = {n: [T(f"in_{n}{g}", [128, TS + 1]) for g in range(2)] for n in "rkvg"}
    in_w = T("in_w", [96, TS + 1]); in_a = T("in_a", [96, TS + 1])
    xr = [T(f"xr{g}", [128, TS]) for g in range(2)]
    xk = [T(f"xk{g}", [128, TS]) for g in range(2)]
    xv = [T(f"xv{g}", [128, TS]) for g in range(2)]
    sg = [T(f"sg{g}", [128, TS]) for g in range(2)]
    lw = [T(f"lw{g}", [128, TS]) for g in range(2)]
    aa = [T(f"aa{g}", [128, TS]) for g in range(2)]
    tkk = [T(f"tkk{g}", [128, TS]) for g in range(2)]
    ttm = [T(f"ttm{g}", [128, TS]) for g in range(2)]
    tpr = [T(f"tpr{g}", [128, TS]) for g in range(2)]
    tcs = [T(f"tcs{g}", [128, TS]) for g in range(2)]
    te1 = [T(f"te1{g}", [128, TS]) for g in range(2)]
    te2 = [T(f"te2{g}", [128, TS]) for g in range(2)]
    gC = [T(f"gC{g}", [128, NCH]) for g in range(2)]
    tw = T("tw", [96, TS]); xa = T("xa", [96, TS])
    ST = [T(f"ST{g}", [128, 64]) for g in range(2)]
    for g in range(2):
        P.V("vector", "memset", ST[g], 0.0, writes=[f"ST{g}"])
    def CT(name):
        return T(name, [128, 4, 64])
    cN = CT("cN"); cNT = CT("cNT"); cAak = CT("cAak"); cBbr = CT("cBbr"); cBkr = CT("cBkr")
    cP = [CT("cP0"), CT("cP1")]; cPT = [CT("cPT0"), CT("cPT1")]; cR = [CT("cR0"), CT("cR1")]
    cV = CT("cV"); cBt = CT("cBt"); cKt = CT("cKt"); cWT = T("cWT", [128, 2, 64]); cUT = T("cUT", [128, 2, 64]); cY = T("cY", [128, 2, 64])
    cYc = T("cYc", [128, 2, 64]); cSq = T("cSq", [128, 2, 64]); cYB = T("cYB", [64, 2, 64]); cOut = [T("cOut0", [64, 2, 2, 64]), T("cOut1", [64, 2, 2, 64])]
    st1 = T("st1", [128, 2]); st2 = T("st2", [128, 2]); st3 = T("st3", [128, 2]); bo = T("bo", [128, 2])
    PS = [P.ps(f"ps{i}", [128, 512], F32) for i in range(8)]
    def slot(b, i):
        return PS[b][:, i * 256:(i + 1) * 256], f"ps{b}"
    (pAab, kAab), (pAabT, kAabT) = slot(0, 0), slot(0, 1)
    (pAak, kAak), (pBbr, kBbr) = slot(1, 0), slot(1, 1)
    (pBkr, kBkr), (ptV, ktV) = slot(2, 0), slot(2, 1)
    (ptB, ktB), (ptK, ktK) = slot(3, 0), slot(3, 1)
    (pP, kP), (pPT, kPT) = slot(4, 0), slot(4, 1)
    (pR, kR) = slot(5, 0)
    pWT, kWT = PS[6][:, 0:128], "ps6"
    pUT, kUT = PS[6][:, 128:256], "ps6"
    pS, kS = PS[6][:, 256:384], "ps6"
    pYT, kYT = PS[7][:, 0:128], "ps7"
    pBo, kBo = PS[7][:, 128:130], "ps7"
    pM, kM = PS[7][:, 256:512], "ps7"

    MU_R, MU_K, MU_V, MU_G, W0, A0, KK, KA, RK = range(9)
    dq = ["sync", "scalar", "gpsimd"]
    for seg in range(NSEG):
        t0 = seg * TS
        qi = 0
        for n in "rkvg":
            for g in range(2):
                P.dma(dq[qi % 3], inp[n][g], zin[n][g, :, t0:t0 + TS + 1], writes=[f"in_{n}{g}"]); qi += 1
        P.dma("sync", in_w, zw[:, t0:t0 + TS + 1], writes=["in_w"])
        P.dma("scalar", in_a, za[:, t0:t0 + TS + 1], writes=["in_a"])

        def lerp(out, okey, src, skey, mu_ap, tmp, tkey, np_=128, eng="vector", mkey=None):
            P.V(eng, "tensor_tensor", tmp[:np_], src[:np_, 0:TS], src[:np_, 1:TS + 1], ALU.subtract, reads=[skey], writes=[tkey])
            P.V("vector", "scalar_tensor_tensor", out[:np_], tmp[:np_], mu_ap, src[:np_, 1:TS + 1], ALU.mult, ALU.add,
                reads=[tkey, skey, mkey], writes=[okey])
        for g in range(2):
            lerp(xr[g], f"xr{g}", inp["r"][g], f"in_r{g}", par[g][:, MU_R:MU_R + 1], ttm[g], f"ttm{g}", mkey=f"par{g}")
            lerp(xk[g], f"xk{g}", inp["k"][g], f"in_k{g}", par[g][:, MU_K:MU_K + 1], ttm[g], f"ttm{g}", mkey=f"par{g}")
            lerp(xv[g], f"xv{g}", inp["v"][g], f"in_v{g}", par[g][:, MU_V:MU_V + 1], ttm[g], f"ttm{g}", mkey=f"par{g}")
            lerp(sg[g], f"sg{g}", inp["g"][g], f"in_g{g}", par[g][:, MU_G:MU_G + 1], ttm[g], f"ttm{g}", mkey=f"par{g}")
            P.act(sg[g], sg[g], AF.Sigmoid, reads=[f"sg{g}"], writes=[f"sg{g}"])
        if stage == 0: return P
        lerp(tw, "tw", in_w, "in_w", parw[:, 0:1], ttm[0], "ttm0", np_=96, mkey="parw")
        P.act(tw, tw, AF.Tanh, reads=["tw"], writes=["tw"])
        lerp(xa, "xa", in_a, "in_a", parw[:, 1:2], ttm[1], "ttm1", np_=96, mkey="parw")
        if stage == 1: return P
        for g in range(2):
            for hf in range(TS // 512):
                cs_ = slice(hf * 512, hf * 512 + 512)
                P.mm(PS[0][:, :], wup[:, g * 128:(g + 1) * 128], tw[:, cs_], True, True, reads=["wup", "tw"], writes=["ps0"])
                P.act(lw[g][:, cs_], PS[0][:, :], AF.Sigmoid, bias=par[g][:, W0:W0 + 1], reads=["ps0", f"par{g}"], writes=[f"lw{g}"])
                P.mm(PS[1][:, :], aup[:, g * 128:(g + 1) * 128], xa[:, cs_], True, True, reads=["aup", "xa"], writes=["ps1"])
                P.act(aa[g][:, cs_], PS[1][:, :], AF.Sigmoid, bias=par[g][:, A0:A0 + 1], reads=["ps1", f"par{g}"], writes=[f"aa{g}"])
            P.V("gpsimd", "tensor_scalar", lw[g], lw[g], -float(np.exp(-0.5)), None, ALU.mult, reads=[f"lw{g}"], writes=[f"lw{g}"])
            P.V("vector", "tensor_scalar", tkk[g], xk[g], par[g][:, KK:KK + 1], None, ALU.mult, reads=[f"xk{g}", f"par{g}"], writes=[f"tkk{g}"])
            P.V("gpsimd", "tensor_tensor", ttm[g], tkk[g], tkk[g], ALU.mult, reads=[f"tkk{g}"], writes=[f"ttm{g}"])
            for hf in range(TS // 512):
                cs_ = slice(hf * 512, hf * 512 + 512)
                P.mm(PS[2][:, :], blk, ttm[g][:, cs_], True, True, reads=["blk", f"ttm{g}"], writes=["ps2"])
                P.V("vector", "tensor_scalar", tpr[g][:, cs_], PS[2][:, :], 1e-24, None, ALU.max, reads=["ps2"], writes=[f"tpr{g}"])
            P.act(tpr[g], tpr[g], AF.Sqrt, reads=[f"tpr{g}"], writes=[f"tpr{g}"])
            P.V("vector", "reciprocal", tpr[g], tpr[g], reads=[f"tpr{g}"], writes=[f"tpr{g}"])
            P.V("vector", "tensor_tensor", tkk[g], tkk[g], tpr[g], ALU.mult, reads=[f"tkk{g}", f"tpr{g}"], writes=[f"tkk{g}"])
            P.V("vector", "tensor_scalar", ttm[g], aa[g], 1.0, par[g][:, KA:KA + 1], ALU.subtract, ALU.mult, reads=[f"aa{g}", f"par{g}"], writes=[f"ttm{g}"])
            P.V("vector", "scalar_tensor_tensor", xk[g], ttm[g], 1.0, xk[g], ALU.add, ALU.mult, reads=[f"ttm{g}", f"xk{g}"], writes=[f"xk{g}"])
            P.V("vector", "scalar_tensor_tensor", tpr[g], xr[g], par[g][:, RK:RK + 1], xk[g], ALU.mult, ALU.mult, reads=[f"xr{g}", f"xk{g}", f"par{g}"], writes=[f"tpr{g}"])
            P.V("gpsimd", "tensor_tensor", aa[g], tkk[g], aa[g], ALU.mult, reads=[f"tkk{g}", f"aa{g}"], writes=[f"aa{g}"])
            for c in range(NCH):
                cc = slice(c * CH, (c + 1) * CH)
                P.V("vector", "tensor_tensor_scan", tcs[g][:, cc], ones[:, :], lw[g][:, cc], 0.0, ALU.mult, ALU.add, reads=[f"lw{g}", "ones"], writes=[f"tcs{g}"])
            P.act(te1[g], tcs[g], AF.Exp, reads=[f"tcs{g}"], writes=[f"te1{g}"])
            P.act(te2[g], tcs[g], AF.Exp, scale=-1.0, reads=[f"tcs{g}"], writes=[f"te2{g}"])
            P.V("gpsimd", "tensor_tensor", lw[g], tcs[g], lw[g], ALU.subtract, reads=[f"tcs{g}", f"lw{g}"], writes=[f"lw{g}"])
            P.act(lw[g], lw[g], AF.Exp, reads=[f"lw{g}"], writes=[f"lw{g}"])
            e1v = te1[g].rearrange("p (c s) -> p c s", s=CH)
            P.V("vector", "tensor_copy", gC[g], e1v[:, :, CH - 1], reads=[f"te1{g}"], writes=[f"gC{g}"])
            P.V("vector", "scalar_tensor_tensor", lw[g], tkk[g], -1.0, lw[g], ALU.mult, ALU.mult, reads=[f"tkk{g}", f"lw{g}"], writes=[f"lw{g}"])
            P.V("gpsimd", "tensor_tensor", xr[g], xr[g], te1[g], ALU.mult, reads=[f"xr{g}", f"te1{g}"], writes=[f"xr{g}"])
            P.V("vector", "tensor_tensor", tcs[g].rearrange("p (c s) -> p c s", s=CH), te2[g].rearrange("p (c s) -> p c s", s=CH),
                gC[g].unsqueeze(2).to_broadcast([128, NCH, CH]), ALU.mult, reads=[f"te2{g}", f"gC{g}"], writes=[f"tcs{g}"])
            P.V("gpsimd", "tensor_tensor", tkk[g], aa[g], te2[g], ALU.mult, reads=[f"aa{g}", f"te2{g}"], writes=[f"tkk{g}"])
            P.V("vector", "tensor_tensor", te2[g], xk[g], te2[g], ALU.mult, reads=[f"xk{g}", f"te2{g}"], writes=[f"te2{g}"])
            P.V("gpsimd", "tensor_tensor", aa[g], aa[g], tcs[g], ALU.mult, reads=[f"aa{g}", f"tcs{g}"], writes=[f"aa{g}"])
            P.V("vector", "tensor_tensor", tcs[g], xk[g], tcs[g], ALU.mult, reads=[f"xk{g}", f"tcs{g}"], writes=[f"tcs{g}"])
        if stage == 2: return P
        alb, rb, beb, kb, bet, kt = lw, xr, tkk, te2, aa, tcs
        kalb, krb, kbeb, kkb, kbet, kkt = "lw", "xr", "tkk", "te2", "aa", "tcs"

        for c in range(NCH):
            cg = seg * NCH + c
            cols = slice(c * CH, (c + 1) * CH)
            cc = c % 2
            def Fc(x, h, c_):
                g, h2 = h // 2, h % 2
                return x[g][64 * h2:64 * h2 + 64, c_ * CH:(c_ + 1) * CH]
            def F(x, h):
                return Fc(x, h, c)
            def RW(h):
                return slice(64 * (h % 2), 64 * (h % 2) + 64)
            def O(ps, h, cc_=None):
                if cc_ is None:
                    return ps[RW(h), (h // 2) * 64:(h // 2) * 64 + 64]
                b = cc_ * 2 + h // 2
                return ps[RW(h), b * 64:b * 64 + 64]
            def C(t, h, cc_=None):
                if cc_ is None:
                    return t[RW(h), h // 2, :]
                return t[RW(h), cc_ * 2 + h // 2, :]
            f2 = lambda t: t.rearrange("p g s -> p (g s)")
            if cc == 0:
                for c_ in (c, c + 1):
                    q_ = c_ - c
                    for h in range(4):
                        g, h2 = h // 2, h % 2
                        P.mm(O(pAab, h, q_), Fc(beb, h, c_), Fc(alb, h, c_), True, True, reads=[f"{kbeb}{g}", f"{kalb}{g}"], writes=[kAab])
                        P.mm(O(pAabT, h, q_), Fc(alb, h, c_), Fc(beb, h, c_), True, True, reads=[f"{kbeb}{g}", f"{kalb}{g}"], writes=[kAabT])
                for c_ in (c, c + 1):
                    q_ = c_ - c
                    for h in range(4):
                        g, h2 = h // 2, h % 2
                        P.mm(O(pAak, h, q_), Fc(kb, h, c_), Fc(alb, h, c_), True, True, reads=[f"{kkb}{g}", f"{kalb}{g}"], writes=[kAak])
                        P.mm(O(pBbr, h, q_), Fc(beb, h, c_), Fc(rb, h, c_), True, True, reads=[f"{kbeb}{g}", f"{krb}{g}"], writes=[kBbr])
                for c_ in (c, c + 1):
                    q_ = c_ - c
                    for h in range(4):
                        g, h2 = h // 2, h % 2
                        idb = ident[RW(h), 64 * h2:64 * h2 + 64]
                        P.mm(O(pBkr, h, q_), Fc(kb, h, c_), Fc(rb, h, c_), True, True, reads=[f"{kkb}{g}", f"{krb}{g}"], writes=[kBkr])
                        P.mm(O(ptV, h, q_), Fc(xv, h, c_), idb, True, True, reads=[f"xv{g}", "ident"], writes=[ktV])
                for c_ in (c, c + 1):
                    q_ = c_ - c
                    for h in range(4):
                        g, h2 = h // 2, h % 2
                        idb = ident[RW(h), 64 * h2:64 * h2 + 64]
                        P.mm(O(ptB, h, q_), Fc(bet, h, c_), idb, True, True, reads=[f"{kbet}{g}", "ident"], writes=[ktB])
                        P.mm(O(ptK, h, q_), Fc(kt, h, c_), idb, True, True, reads=[f"{kkt}{g}", "ident"], writes=[ktK])
                P.V("vector", "tensor_tensor", f2(cN), pAab, f2(msu), ALU.mult, reads=[kAab, "masks"], writes=["cN"])
                P.V("vector", "tensor_tensor", f2(cNT), pAabT, f2(msl), ALU.mult, reads=[kAabT, "masks"], writes=["cNT"])
                P.V("vector", "tensor_tensor", f2(cAak), pAak, f2(msu), ALU.mult, reads=[kAak, "masks"], writes=["cAak"])
                P.V("vector", "tensor_tensor", f2(cBbr), pBbr, f2(mui), ALU.mult, reads=[kBbr, "masks"], writes=["cBbr"])
                P.V("vector", "tensor_tensor", f2(cBkr), pBkr, f2(mui), ALU.mult, reads=[kBkr, "masks"], writes=["cBkr"])
                P.act(f2(cV), ptV, AF.Copy, reads=[ktV], writes=["cV"])
                P.act(f2(cBt), ptB, AF.Copy, reads=[ktB], writes=["cBt"])
                P.act(f2(cKt), ptK, AF.Copy, reads=[ktK], writes=["cKt"])
                P.V("gpsimd", "tensor_tensor", f2(cR[0]), f2(cN), f2(mid), ALU.add, reads=["cN", "masks"], writes=["cR0"])
                Pc, PTc, Rc = (cN, "cN"), (cNT, "cNT"), (cR[0], "cR0")
                for i in range(1, 6):
                    nP = (cP[i % 2], f"cP{i%2}"); nPT = (cPT[i % 2], f"cPT{i%2}"); nR = (cR[i % 2], f"cR{i%2}")
                    for q_ in range(2):
                        for h in range(4):
                            if i < 5:
                                P.mm(O(pP, h, q_), C(PTc[0], h, q_), C(Pc[0], h, q_), True, True, reads=[PTc[1], Pc[1]], writes=[kP])
                            P.mm(O(pPT, h, q_), C(Pc[0], h, q_), C(PTc[0], h, q_), True, True, reads=[PTc[1], Pc[1]], writes=[kPT])
                    if i < 5:
                        P.act(f2(nP[0]), pP, AF.Copy, reads=[kP], writes=[nP[1]])
                    P.act(f2(nPT[0]), pPT, AF.Copy, reads=[kPT], writes=[nPT[1]])
                    for q_ in range(2):
                        for h in range(4):
                            P.mm(O(pR, h, q_), C(nPT[0], h, q_), C(Rc[0], h, q_), True, True, reads=[nPT[1], Rc[1]], writes=[kR])
                    P.V("vector", "tensor_tensor", f2(nR[0]), pR, f2(Rc[0]), ALU.add, reads=[kR, Rc[1]], writes=[nR[1]])
                    Pc, PTc, Rc = nP, nPT, nR
                Tm = Rc
            if stage == 5: return P
            for h in range(4):
                g = h // 2
                P.mm(O(pWT, h), F(alb, h), ST[g][RW(h), :], True, False, reads=[f"{kalb}{g}", f"ST{g}"], writes=[kWT])
                P.mm(O(pWT, h), C(cAak, h, cc), C(cV, h, cc), False, True, reads=["cAak", "cV"], writes=[kWT])
            P.act(f2(cWT), pWT, AF.Copy, reads=[kWT], writes=["cWT"])
            for h in range(4):
                P.mm(O(pUT, h), C(Tm[0], h, cc), C(cWT, h), True, True, reads=[Tm[1], "cWT"], writes=[kUT])
            P.V("vector", "tensor_scalar", f2(cUT), pUT, 1.0, None, ALU.mult, reads=[kUT], writes=["cUT"])
            for h in range(4):
                g = h // 2
                P.mm(O(pYT, h), F(rb, h), ST[g][RW(h), :], True, False, reads=[f"{krb}{g}", f"ST{g}"], writes=[kYT])
                P.mm(O(pYT, h), C(cBbr, h, cc), C(cUT, h), False, False, reads=["cBbr", "cUT"], writes=[kYT])
                P.mm(O(pYT, h), C(cBkr, h, cc), C(cV, h, cc), False, True, reads=["cBkr", "cV"], writes=[kYT])
            P.act(f2(cY), pYT, AF.Copy, reads=[kYT], writes=["cY"])
            for h in range(4):
                P.mm(O(pS, h), C(cBt, h, cc), C(cUT, h), True, False, reads=["cBt", "cUT"], writes=[kS])
                P.mm(O(pS, h), C(cKt, h, cc), C(cV, h, cc), False, True, reads=["cKt", "cV"], writes=[kS])
            for g in range(2):
                P.V("vector", "scalar_tensor_tensor", ST[g], ST[g], gC[g][:, c:c + 1], pS[:, g * 64:(g + 1) * 64], ALU.mult, ALU.add,
                    reads=[f"ST{g}", f"gC{g}", kS], writes=[f"ST{g}"])
            if stage == 6: return P
            B3 = lambda t: t.unsqueeze(2).to_broadcast([128, 2, 64])
            P.V("vector", "tensor_reduce", st1, cY, AX.X, ALU.add, reads=["cY"], writes=["st1"])
            P.V("vector", "tensor_scalar", st1, st1, 1.0 / 64, None, ALU.mult, reads=["st1"], writes=["st1"])
            P.V("vector", "tensor_tensor", cYc, cY, B3(st1), ALU.subtract, reads=["cY", "st1"], writes=["cYc"])
            P.V("gpsimd", "tensor_tensor", cSq, cYc, cYc, ALU.mult, reads=["cYc"], writes=["cSq"])
            P.V("vector", "tensor_reduce", st2, cSq, AX.X, ALU.add, reads=["cSq"], writes=["st2"])
            P.V("vector", "tensor_scalar", st2, st2, 1.0 / 64, 64e-5, ALU.mult, ALU.add, reads=["st2"], writes=["st2"])
            P.act(st2, st2, AF.Sqrt, reads=["st2"], writes=["st2"])
            P.V("vector", "reciprocal", st3, st2, reads=["st2"], writes=["st3"])
            P.V("vector", "tensor_tensor", cYc, cYc, B3(st3), ALU.mult, reads=["cYc", "st3"], writes=["cYc"])
            P.V("gpsimd", "tensor_tensor", cYc, cYc, lnw, ALU.mult, reads=["cYc", "lnw"], writes=["cYc"])
            P.V("gpsimd", "tensor_tensor", cYc, cYc, lnb, ALU.add, reads=["cYc", "lnb"], writes=["cYc"])
            for h in range(4):
                g = h // 2
                P.mm(pBo[RW(h), g:g + 1], F(tpr, h), ones[RW(h), 0:1], True, True, reads=[f"tpr{g}", "ones"], writes=[kBo])
            P.act(bo, pBo, AF.Copy, reads=[kBo], writes=["bo"])
            P.V("vector", "tensor_tensor", cSq, cV[:, 2 * cc:2 * cc + 2, :], B3(bo), ALU.mult, reads=["cV", "bo"], writes=["cSq"])
            P.V("gpsimd", "tensor_tensor", cYc, cYc, cSq, ALU.add, reads=["cYc", "cSq"], writes=["cYc"])
            P.dma("gpsimd", cYB, cYc[64:128], reads=["cYc"], writes=["cYB"])
            for g in range(2):
                P.mm(pM[:64, :], sg[g][:, cols], gup[g], g == 0, g == 1, reads=[f"sg{g}", f"gup{g}"], writes=[kM])
            co = cOut[cg % 2]; cok = f"cOut{cg%2}"
            pMv = pM[:64, :].rearrange("p (g h v) -> p g h v", g=2, h=2)
            P.V("vector", "tensor_tensor", co[:, :, 0, :], cYc[0:64], pMv[:, :, 0, :], ALU.mult, reads=["cYc", kM], writes=[cok])
            P.V("vector", "tensor_tensor", co[:, :, 1, :], cYB, pMv[:, :, 1, :], ALU.mult, reads=["cYB", kM], writes=[cok])
            P.dma("sync" if cg % 2 == 0 else "scalar", ob[cg * CH:(cg + 1) * CH, :], co.rearrange("p g h v -> p (g h v)"), reads=[cok], final=True)
    return P


EI, EO = "ExternalInput", "ExternalOutput"
NEG = -1.0e30

def dsa_consts():
    RA = np.zeros((128, 128), np.float32)
    for d in range(16):
        RA[d, d + 16] = -1.0; RA[d + 16, d] = 1.0
    RI = np.zeros((128, 128), np.float32)
    for b in (0, 64):
        for d in range(8):
            RI[b + d, b + d + 8] = -1.0; RI[b + d + 8, b + d] = 1.0
    return {"c_RAT": np.ascontiguousarray(RA.T), "c_RIT": np.ascontiguousarray(RI.T), "c_ident": np.eye(128, dtype=np.float32),
            "c_iota": np.tile(np.arange(512, dtype=np.float32)[None, :], (128, 1))}

def build_dsa(L, stage=99, idx_dt=None):
    IDT = F32
    P = Prog()
    J = L // 1024
    qT_d = P.dram("qT", [J, 16, 128, 128], F32, kind=EI)
    cosq_d = P.dram("cosq", [J, 32, 128], F32, kind=EI); sinq_d = P.dram("sinq", [J, 32, 128], F32, kind=EI)
    qiT_d = P.dram("qiT", [J, 16, 128, 128], F32, kind=EI)
    cosqi_d = P.dram("cosqi", [J, 16, 128], F32, kind=EI); sinqi_d = P.dram("sinqi", [J, 16, 128], F32, kind=EI)
    kT_d = P.dram("kT", [4, 128, L], F32, kind=EI)
    cosk_d = P.dram("cosk", [32, L], F32, kind=EI); sink_d = P.dram("sink", [32, L], F32, kind=EI)
    kiT_d = P.dram("kiT", [64, L], F32, kind=EI)
    coski_d = P.dram("coski", [16, L], F32, kind=EI); sinki_d = P.dram("sinki", [16, L], F32, kind=EI)
    v_d = P.dram("v", [L, 512], F32, kind=EI)
    wi_d = P.dram("wi", [J, 128, 32], F32, kind=EI)
    qpos_d = P.dram("qpos", [J, 128, 1], F32, kind=EI)
    RAT_d = P.dram("c_RAT", [128, 128], F32, kind=EI); RIT_d = P.dram("c_RIT", [128, 128], F32, kind=EI)
    ident_d = P.dram("c_ident", [128, 128], F32, kind=EI); iota_d = P.dram("c_iota", [128, 512], F32, kind=EI)
    oa_d = P.dram("oa", [J, 128, 2048], F32, kind=EO)
    kscr = P.dram("kscr", [4, 128, L], BF16)
    vscr = P.dram("vscr", [L, 512], BF16)

    T = P.sb
    RAT = T("RAT", [128, 128]); RIT = T("RIT", [128, 128]); identf = T("identf", [128, 128]); identb = T("identb", [128, 128], BF16)
    iota = T("iota", [128, 512])
    P.dma("sync", RAT, RAT_d, writes=["RAT"]); P.dma("sync", RIT, RIT_d, writes=["RIT"])
    P.dma("scalar", identf, ident_d, writes=["identf"]); P.dma("scalar", iota, iota_d, writes=["iota"])
    P.V("vector", "tensor_copy", identb, identf, reads=["identf"], writes=["identb"])
    PS = [P.ps(f"ps{i}", [128, 512], F32) for i in range(6)]
    PSB = [P.ps(f"psb{i}", [128, 1024], BF16) for i in range(2)]

    xin = [T(f"xin{i}", [128, 512]) for i in range(2)]
    tc_ = [T(f"tcos{i}", [128, 512]) for i in range(2)]
    tsn = [T(f"tsin{i}", [128, 512]) for i in range(2)]
    rt = T("rt", [128, 512]); ru = T("ru", [128, 512])
    xo = [T(f"xo{i}", [128, 512], BF16) for i in range(2)]
    cnt = [0]

    def rotary(src_ap, Tn, RT, rtkey, ranges, cos_ap, sin_ap, out_ap, okey, out_reads=(), tab_rows=None):
        i = cnt[0] % 2; cnt[0] += 1
        if isinstance(src_ap, list):
            for (r0, nr, ap_) in src_ap:
                P.dma("sync", xin[i][r0:r0 + nr, :Tn], ap_, writes=[f"xin{i}"])
        else:
            P.dma("sync", xin[i][:, :Tn], src_ap, writes=[f"xin{i}"])
        for (r0, nr) in ranges:
            P.dma("scalar", tc_[i][r0:r0 + nr, :Tn], cos_ap, writes=[f"tcos{i}"])
            P.dma("gpsimd", tsn[i][r0:r0 + nr, :Tn], sin_ap, writes=[f"tsin{i}"])
        pk = f"ps{i}"
        P.mm(PS[i][:, :Tn], RT, xin[i][:, :Tn], True, True, reads=[rtkey, f"xin{i}"], writes=[pk])
        P.act(out_ap, xin[i][:, :Tn], AF.Copy, reads=[f"xin{i}"] + list(out_reads), writes=[okey])
        for (r0, nr) in ranges:
            rs = slice(r0, r0 + nr)
            P.V("vector", "tensor_tensor", rt[rs, :Tn], PS[i][rs, :Tn], tsn[i][rs, :Tn], ALU.mult, reads=[pk, f"tsin{i}"], writes=["rt"])
            P.V("gpsimd", "tensor_tensor", ru[rs, :Tn], xin[i][rs, :Tn], tc_[i][rs, :Tn], ALU.mult, reads=[f"xin{i}", f"tcos{i}"], writes=["ru"])
            P.V("vector", "tensor_tensor", out_ap[rs], rt[rs, :Tn], ru[rs, :Tn], ALU.add, reads=["rt", "ru"], writes=[okey])

    kiT = T("kiT", [128, L], IDT)
    xoi = [T(f"xoi{i}", [128, 512], IDT) for i in range(2)]
    for kv in range(4):
        for ch in range(L // 512):
            cs = slice(ch * 512, ch * 512 + 512)
            i = cnt[0] % 2
            rotary(kT_d[kv, :, cs], 512, RAT, "RAT", [(0, 32)], cosk_d[:, cs], sink_d[:, cs], xo[i], f"xo{i}")
            P.dma("sync", kscr[kv, :, cs], xo[i], reads=[f"xo{i}"], writes=["kscr"])
    for ch in range(L // 512):
        cs = slice(ch * 512, ch * 512 + 512)
        i = cnt[0] % 2
        rotary([(0, 64, kiT_d[:, cs]), (64, 64, kiT_d[:, cs])], 512, RIT, "RIT", [(0, 16), (64, 16)], coski_d[:, cs], sinki_d[:, cs], xoi[i], f"xoi{i}")
        P.V("vector", "tensor_copy", kiT[:, cs], xoi[i], reads=[f"xoi{i}"], writes=["kiT"])
    NKMAX = L
    acc = T("acc", [128, NKMAX]); work = T("work", [128, NKMAX]); mb = T("mb", [128, NKMAX], BF16)
    pbf = T("pbf", [128, NKMAX], BF16)
    Kb = [T(f"Kb{i}", [128, NKMAX], BF16) for i in range(1)]
    Vb = [T(f"Vb{i}", [128, NKMAX // 128, 128], BF16) for i in range(1)]
    nvt = min(4, NKMAX // 512)
    vst = work[:, 0:nvt * 512].rearrange("p (t c) -> p t c", c=512)
    vsb = pbf[:, 0:nvt * 512].rearrange("p (t c) -> p t c", c=512)
    vr = nvt * 128
    for it in range(L // vr):
        P.dma("scalar", vst, v_d[it * vr:(it + 1) * vr, :].rearrange("(t p) c -> p t c", p=128), writes=["work"])
        P.V("gpsimd", "tensor_copy", vsb, vst, reads=["work"], writes=["pbf"])
        P.dma("scalar", vscr[it * vr:(it + 1) * vr, :].rearrange("(t p) c -> p t c", p=128), vsb, reads=["pbf"], writes=["vscr"])
    if stage == 0: return P

    QT = T("QT", [128, 16, 128], BF16); QI = T("QI", [128, 16, 128], IDT)
    wi = T("wi", [128, 32]); qpos = T("qpos", [128, 1]); qoff = T("qoff", [128, 2])
    rl = [T(f"rl{i}", [128, 512]) for i in range(2)]
    m8 = T("m8", [128, 8]); thr = T("thr", [128, 1]); mx = T("mx", [128, 1]); nmx = T("nmx", [128, 1]); rsum = T("rsum", [128, 1]); rinv = T("rinv", [128, 1]); rinv2 = T("rinv2", [128, 2])
    pT = [T(f"pT{i}", [128, 4, 128], BF16) for i in range(2)]
    obh = [T(f"obh{i}", [128, 128]) for i in range(2)]
    cbias = rt
    SC = float(32 ** -0.5 * 64 ** -0.5)

    for j in range(J):
        nk = 1024 * (j + 1)
        NCHK = nk // 512
        P.dma("sync", wi, wi_d[j], writes=["wi"]); P.dma("sync", qpos, qpos_d[j], writes=["qpos"])
        P.V("vector", "tensor_scalar", wi, wi, SC, None, ALU.mult, reads=["wi"], writes=["wi"])
        for h in range(16):
            rotary(qT_d[j, h], 128, RAT, "RAT", [(0, 32)], cosq_d[j], sinq_d[j], QT[:, h, :], "QT")
        for pr in range(16):
            rotary(qiT_d[j, pr], 128, RIT, "RIT", [(0, 16), (64, 16)], cosqi_d[j], sinqi_d[j], QI[:, pr, :], "QI")
        for hh in range(32):
            pr, h2 = hh // 2, hh % 2
            rows = slice(64 * h2, 64 * h2 + 64)
            for ck in range(NCHK):
                cs = slice(ck * 512, ck * 512 + 512)
                pi = 2 + (hh * NCHK + ck) % 2
                pk = f"ps{pi}"
                P.mm(PS[pi], QI[rows, pr, :], kiT[rows, cs], True, True, reads=["QI", "kiT"], writes=[pk])
                ri = (hh * NCHK + ck) % 2
                P.act(rl[ri], PS[pi], AF.Relu, reads=[pk], writes=[f"rl{ri}"])
                if hh == 0:
                    P.V("vector", "tensor_scalar", acc[:, cs], rl[ri], wi[:, 0:1], None, ALU.mult, reads=[f"rl{ri}", "wi"], writes=["acc"])
                else:
                    P.V("vector", "scalar_tensor_tensor", acc[:, cs], rl[ri], wi[:, hh:hh + 1], acc[:, cs], ALU.mult, ALU.add,
                        reads=[f"rl{ri}", "wi", "acc"], writes=["acc"])
        for t in range(2):
            off = nk - 1024 + t * 512
            P.V("vector", "tensor_scalar", qoff[:, t:t + 1], qpos, float(-off), None, ALU.add, reads=["qpos"], writes=["qoff"])
            P.V("vector", "tensor_scalar", cbias, iota, qoff[:, t:t + 1], NEG, ALU.is_gt, ALU.mult, reads=["iota", "qoff"], writes=["rt"])
            P.V("vector", "tensor_tensor", acc[:, off:off + 512], acc[:, off:off + 512], cbias, ALU.add, reads=["acc", "rt"], writes=["acc"])
        for r in range(32):
            src = acc if r == 0 else work
            sk = "acc" if r == 0 else "work"
            P.V("vector", "max", m8, src[:, :nk], reads=[sk], writes=["m8"])
            if r < 31:
                P.V("vector", "match_replace", work[:, :nk], m8, src[:, :nk], NEG, reads=["m8", sk], writes=["work"])
        P.V("vector", "tensor_scalar", thr, m8[:, 7:8], -1.0e29, None, ALU.max, reads=["m8"], writes=["thr"])
        P.V("vector", "tensor_scalar", mb[:, :nk], acc[:, :nk], thr, NEG, ALU.is_lt, ALU.mult, reads=["acc", "thr"], writes=["mb"])
        if stage == 1:
            P.dma("sync", oa_d[j, :, 0:1024], acc[:, nk - 1024:nk], reads=["acc"], final=True)
            return P
        wk = [work, acc]; wkk = ["work", "acc"]
        def A1(h):
            kv = h // 4
            if h % 4 == 0:
                P.dma("sync", Kb[0][:, :nk], kscr[kv, :, :nk], reads=["kscr"], writes=["Kb0"])
            w_ = wk[h % 2]; wkey = wkk[h % 2]
            for ck in range(NCHK):
                cs = slice(ck * 512, ck * 512 + 512)
                pi = 2 + ck % 2
                pk = f"ps{pi}"
                P.mm(PS[pi], QT[:, h, :], Kb[0][:, cs], True, True, reads=["QT", "Kb0"], writes=[pk])
                P.V("vector", "scalar_tensor_tensor", w_[:, cs], PS[pi], float(128 ** -0.5), mb[:, cs], ALU.mult, ALU.add,
                    reads=[pk, "mb"], writes=[wkey])
            P.V("vector", "reduce_max", mx, w_[:, :nk], AX.X, reads=[wkey], writes=["mx"])
            P.V("vector", "tensor_scalar", nmx, mx, -1.0, None, ALU.mult, reads=["mx"], writes=["nmx"])
        def A2(h):
            w_ = wk[h % 2]; wkey = wkk[h % 2]
            P.act(pbf[:, :nk], w_[:, :nk], AF.Exp, bias=nmx, accum_out=rsum, reads=[wkey, "nmx"], writes=["pbf", "rsum"])
            P.V("vector", "reciprocal", rinv, rsum, reads=["rsum"], writes=["rinv"])
        def B(h):
            kv = h // 4
            if h % 4 == 0:
                P.dma("scalar", Vb[0][:, :nk // 128, :], vscr[0:nk, kv * 128:(kv + 1) * 128].rearrange("(t p) c -> p t c", p=128),
                      reads=["vscr"], writes=["Vb0"])
            P.V("vector", "tensor_copy", rinv2[:, h % 2:h % 2 + 1], rinv, reads=["rinv"], writes=[f"rinv2_{h % 2}"])
            NT4 = nk // 512
            for t4 in range(NT4):
                bq = t4 % 2
                for u in range(4):
                    kt = t4 * 4 + u
                    P.tr(PSB[bq][:, u * 128:(u + 1) * 128], pbf[:, kt * 128:(kt + 1) * 128], identb, reads=["pbf", "identb"], writes=[f"psb{bq}"])
                if t4 % 2 == 0:
                    P.act(pT[bq].rearrange("p u q -> p (u q)"), PSB[bq][:, 0:512], AF.Copy, reads=[f"psb{bq}"], writes=[f"pT{bq}"])
                else:
                    P.V("vector", "tensor_scalar", pT[bq].rearrange("p u q -> p (u q)"), PSB[bq][:, 0:512], 1.0, None, ALU.mult, reads=[f"psb{bq}"], writes=[f"pT{bq}"])
                for u in range(4):
                    kt = t4 * 4 + u
                    P.mm(PS[4 + h % 2][:, 0:128], pT[bq][:, u, :], Vb[0][:, kt, :], kt == 0, kt == nk // 128 - 1,
                         reads=[f"pT{bq}", "Vb0"], writes=[f"ps{4 + h % 2}"])
            P.V("vector", "tensor_scalar", obh[h % 2], PS[4 + h % 2][:, 0:128], rinv2[:, h % 2:h % 2 + 1], None, ALU.mult,
                reads=[f"ps{4 + h % 2}", f"rinv2_{h % 2}"], writes=[f"obh{h % 2}"])
            P.dma("gpsimd", oa_d[j, :, h * 128:(h + 1) * 128], obh[h % 2], reads=[f"obh{h % 2}"], final=True)
        A1(0); A2(0)
        for h in range(16):
            if h + 1 < 16:
                A1(h + 1)
            B(h)
            if h + 1 < 16:
                A2(h + 1)
    return P


EI, EO = "ExternalInput", "ExternalOutput"
PI = float(np.pi)

def build_s5(L, C=512):
    P = Prog()
    NCK = L // C
    uT_d = P.dram("uT", [2, 128, L], F32, kind=EI)
    Bre_d = P.dram("Bre", [8, 128, 128], F32, kind=EI); Bim_d = P.dram("Bim", [8, 128, 128], F32, kind=EI)
    Cre_d = P.dram("Cre", [8, 128, 128], F32, kind=EI); Cim_d = P.dram("Cim", [8, 128, 128], F32, kind=EI)
    lam_d = P.dram("lam", [128, 8, 3], F32, kind=EI)
    dsk_d = P.dram("dsk", [128, 2], F32, kind=EI)
    iota_d = P.dram("c_iota", [128, C + 1], F32, kind=EI)
    zg_d = P.dram("zgT", [2, 128, L], F32, kind=EO)
    T = P.sb
    Bre = T("Bre", [128, 8, 128]); Bim = T("Bim", [128, 8, 128]); Cre = T("Cre", [128, 8, 128]); Cim = T("Cim", [128, 8, 128])
    lam = T("lam", [128, 8, 3]); dsk = T("dsk", [128, 2]); iota = T("iota", [128, C + 1])
    for r in range(8):
        P.dma("sync", Bre[:, r, :], Bre_d[r], writes=["Bre"]); P.dma("scalar", Bim[:, r, :], Bim_d[r], writes=["Bim"])
        P.dma("sync", Cre[:, r, :], Cre_d[r], writes=["Cre"]); P.dma("scalar", Cim[:, r, :], Cim_d[r], writes=["Cim"])
    P.dma("sync", lam, lam_d, writes=["lam"]); P.dma("sync", dsk, dsk_d, writes=["dsk"]); P.dma("sync", iota, iota_d, writes=["iota"])
    P.V("vector", "tensor_scalar", Cim.rearrange("p r c -> p (r c)"), Cim.rearrange("p r c -> p (r c)"), -1.0, None, ALU.mult, reads=["Cim"], writes=["Cim"])

    W = C + 1
    ki = T("ki", [128, W], I32); t1 = T("t1", [128, W]); t2 = T("t2", [128, W]); t3 = T("t3", [128, W])

    def sin_of(out, okey, ang, akey, w, shift):
        P.V("vector", "tensor_scalar", t1[:, :w], ang, shift, 1.0 / (2 * PI), ALU.add, ALU.mult, reads=[akey], writes=["t1"])
        P.V("vector", "tensor_copy", ki[:, :w], t1[:, :w], reads=["t1"], writes=["ki"])
        P.V("vector", "tensor_copy", t2[:, :w], ki[:, :w], reads=["ki"], writes=["t2"])
        P.V("vector", "tensor_scalar", t1[:, :w], ang, shift, None, ALU.add, reads=[akey], writes=["t1"])
        P.V("vector", "scalar_tensor_tensor", t1[:, :w], t2[:, :w], -2 * PI, t1[:, :w], ALU.mult, ALU.add, reads=["t2", "t1"], writes=["t1"])
        P.V("vector", "tensor_scalar", t2[:, :w], t1[:, :w], PI, -2 * PI, ALU.is_gt, ALU.mult, reads=["t1"], writes=["t2"])
        P.V("vector", "tensor_scalar", t3[:, :w], t1[:, :w], -PI, 2 * PI, ALU.is_lt, ALU.mult, reads=["t1"], writes=["t3"])
        P.V("vector", "tensor_tensor", t1[:, :w], t1[:, :w], t2[:, :w], ALU.add, reads=["t1", "t2"], writes=["t1"])
        P.V("vector", "tensor_tensor", t1[:, :w], t1[:, :w], t3[:, :w], ALU.add, reads=["t1", "t3"], writes=["t1"])
        P.act(out, t1[:, :w], AF.Sin, reads=["t1"], writes=[okey])

    NP = 16
    pp = T("pp", [128, 8, NP])
    LR, LI, ST, TH, MAG, CT, SN, AR, AI, DEN, CR, CI, COSC, SINC, TMP, TMP2 = range(16)
    cosT = T("cosT", [128, 8, W]); sinT = T("sinT", [128, 8, W]); Er = T("Er", [128, 8, C]); Ei = T("Ei", [128, 8, C])
    rmag = T("rmag", [128, 8, C]); ang = T("ang", [128, W])
    def pc(r, i):
        return pp[:, r, i:i + 1]
    K = ["pp"]
    for r in range(8):
        P.V("vector", "tensor_scalar", pc(r, LR), lam[:, r, 0:1], -1e-4, None, ALU.min, reads=["lam"], writes=K)
        P.V("vector", "tensor_copy", pc(r, LI), lam[:, r, 1:2], reads=["lam"], writes=K)
        P.act(pc(r, ST), lam[:, r, 2:3], AF.Exp, reads=["lam"], writes=K)
        P.V("vector", "tensor_tensor", pc(r, TH), pc(r, LI), pc(r, ST), ALU.mult, reads=K, writes=K)
        P.V("vector", "tensor_tensor", pc(r, TMP), pc(r, LR), pc(r, ST), ALU.mult, reads=K, writes=K)
        P.act(pc(r, MAG), pc(r, TMP), AF.Exp, reads=K, writes=K)
        sin_of(pc(r, SN), "pp", pc(r, TH), "pp", 1, 0.0)
        sin_of(pc(r, CT), "pp", pc(r, TH), "pp", 1, PI / 2)
        P.V("vector", "tensor_tensor", pc(r, AR), pc(r, MAG), pc(r, CT), ALU.mult, reads=K, writes=K)
        P.V("vector", "tensor_tensor", pc(r, AI), pc(r, MAG), pc(r, SN), ALU.mult, reads=K, writes=K)
        P.V("vector", "tensor_tensor", pc(r, DEN), pc(r, LR), pc(r, LR), ALU.mult, reads=K, writes=K)
        P.V("vector", "scalar_tensor_tensor", pc(r, DEN), pc(r, LI), pc(r, LI), pc(r, DEN), ALU.mult, ALU.add, reads=K, writes=K)
        P.V("vector", "reciprocal", pc(r, DEN), pc(r, DEN), reads=K, writes=K)
        P.V("vector", "tensor_scalar", pc(r, TMP), pc(r, AR), -1.0, None, ALU.add, reads=K, writes=K)
        P.V("vector", "tensor_tensor", pc(r, TMP2), pc(r, LI), pc(r, AI), ALU.mult, reads=K, writes=K)
        P.V("vector", "scalar_tensor_tensor", pc(r, CR), pc(r, TMP), pc(r, LR), pc(r, TMP2), ALU.mult, ALU.add, reads=K, writes=K)
        P.V("vector", "tensor_tensor", pc(r, CR), pc(r, CR), pc(r, DEN), ALU.mult, reads=K, writes=K)
        P.V("vector", "tensor_tensor", pc(r, TMP2), pc(r, LI), pc(r, TMP), ALU.mult, reads=K, writes=K)
        P.V("vector", "scalar_tensor_tensor", pc(r, CI), pc(r, AI), pc(r, LR), pc(r, TMP2), ALU.mult, ALU.subtract, reads=K, writes=K)
        P.V("vector", "tensor_tensor", pc(r, CI), pc(r, CI), pc(r, DEN), ALU.mult, reads=K, writes=K)
        P.V("vector", "tensor_scalar", ang, iota, pc(r, TH), None, ALU.mult, reads=["iota"] + K, writes=["ang"])
        sin_of(sinT[:, r, :], "sinT", ang, "ang", W, 0.0)
        sin_of(cosT[:, r, :], "cosT", ang, "ang", W, PI / 2)
        P.V("vector", "tensor_scalar", t1[:, :C], sinT[:, r, :C], pc(r, CI), None, ALU.mult, reads=["sinT"] + K, writes=["t1"])
        P.V("vector", "scalar_tensor_tensor", Er[:, r, :], cosT[:, r, :C], pc(r, CR), t1[:, :C], ALU.mult, ALU.add, reads=["cosT", "t1"] + K, writes=["Er"])
        P.V("vector", "tensor_scalar", t1[:, :C], sinT[:, r, :C], pc(r, CR), None, ALU.mult, reads=["sinT"] + K, writes=["t1"])
        P.V("vector", "scalar_tensor_tensor", Ei[:, r, :], cosT[:, r, :C], pc(r, CI), t1[:, :C], ALU.mult, ALU.subtract, reads=["cosT", "t1"] + K, writes=["Ei"])
        P.V("vector", "tensor_scalar", rmag[:, r, :], iota[:, :C], 0.0, pc(r, MAG), ALU.mult, ALU.add, reads=["iota"] + K, writes=["rmag"])
    wst = T("wst", [128, 8, 2]);
    P.V("vector", "memset", wst, 0.0, writes=["wst"])
    PSb = [P.ps(f"psb{i}", [128, 512], F32) for i in range(4)]
    PSy = [P.ps(f"psy{i}", [128, 512], F32) for i in range(2)]
    ub = [T(f"ub{i}", [128, C]) for i in range(2)]
    vr = T("vr", [128, C]); vi = T("vi", [128, C]); m1 = T("m1", [128, C]); m2 = T("m2", [128, C])
    wr = T("wr", [128, C]); wi_ = T("wi_", [128, C])
    xr = [T(f"xr{i}", [128, C]) for i in range(2)]; xi = [T(f"xi{i}", [128, C]) for i in range(2)]
    d1 = T("d1", [128, C]); d2 = T("d2", [128, C]); cw = T("cw", [128, 4])
    yb = T("yb", [128, C]); g1 = T("g1", [128, C]); g2 = T("g2", [128, C]); zo = [T(f"zo{i}", [128, C]) for i in range(2)]
    it = 0
    for cb in range(2):
        for ck in range(NCK):
            cs = slice(ck * C, (ck + 1) * C)
            ui = (cb * NCK + ck) % 2
            P.dma("sync", ub[ui], uT_d[cb, :, cs], writes=[f"ub{ui}"])
            yi = (cb * NCK + ck) % 2
            for rr in range(4):
                r = cb * 4 + rr
                pbr = PSb[2 * (it % 2)]; pbi = PSb[2 * (it % 2) + 1]; pbk = f"psb{2 * (it % 2)}"; pbk2 = f"psb{2 * (it % 2) + 1}"
                P.mm(pbr[:, 0:C], Bre[:, r, :], ub[ui], True, True, reads=["Bre", f"ub{ui}"], writes=[pbk])
                P.mm(pbi[:, 0:C], Bim[:, r, :], ub[ui], True, True, reads=["Bim", f"ub{ui}"], writes=[pbk2])
                bur = pbr[:, 0:C]; bui = pbi[:, 0:C]
                P.V("vector", "tensor_tensor", m1, bur, Er[:, r, :], ALU.mult, reads=[pbk, "Er"], writes=["m1"])
                P.V("vector", "tensor_tensor", m2, bui, Ei[:, r, :], ALU.mult, reads=[pbk2, "Ei"], writes=["m2"])
                P.V("gpsimd", "tensor_tensor", vr, m1, m2, ALU.subtract, reads=["m1", "m2"], writes=["vr"])
                P.V("vector", "tensor_tensor", m1, bui, Er[:, r, :], ALU.mult, reads=[pbk2, "Er"], writes=["m1"])
                P.V("vector", "tensor_tensor", m2, bur, Ei[:, r, :], ALU.mult, reads=[pbk, "Ei"], writes=["m2"])
                P.V("gpsimd", "tensor_tensor", vi, m1, m2, ALU.add, reads=["m1", "m2"], writes=["vi"])
                P.V("vector", "tensor_tensor_scan", wr, rmag[:, r, :], vr, wst[:, r, 0:1], ALU.mult, ALU.add, reads=["rmag", "vr", "wst"], writes=["wr"])
                P.V("vector", "tensor_tensor_scan", wi_, rmag[:, r, :], vi, wst[:, r, 1:2], ALU.mult, ALU.add, reads=["rmag", "vi", "wst"], writes=["wi_"])
                P.V("vector", "tensor_tensor", cw[:, 0:1], wr[:, C - 1:C], cosT[:, r, C:C + 1], ALU.mult, reads=["wr", "cosT"], writes=["cw"])
                P.V("vector", "tensor_tensor", cw[:, 1:2], wi_[:, C - 1:C], sinT[:, r, C:C + 1], ALU.mult, reads=["wi_", "sinT"], writes=["cw"])
                P.V("vector", "tensor_tensor", cw[:, 2:3], wr[:, C - 1:C], sinT[:, r, C:C + 1], ALU.mult, reads=["wr", "sinT"], writes=["cw"])
                P.V("vector", "tensor_tensor", cw[:, 3:4], wi_[:, C - 1:C], cosT[:, r, C:C + 1], ALU.mult, reads=["wi_", "cosT"], writes=["cw"])
                P.V("vector", "tensor_tensor", wst[:, r, 0:1], cw[:, 0:1], cw[:, 1:2], ALU.subtract, reads=["cw"], writes=["wst"])
                P.V("vector", "tensor_tensor", wst[:, r, 1:2], cw[:, 2:3], cw[:, 3:4], ALU.add, reads=["cw"], writes=["wst"])
                xi_ = it % 2
                P.V("gpsimd", "tensor_tensor", d1, wr, cosT[:, r, :C], ALU.mult, reads=["wr", "cosT"], writes=["d1"])
                P.V("gpsimd", "tensor_tensor", d2, wi_, sinT[:, r, :C], ALU.mult, reads=["wi_", "sinT"], writes=["d2"])
                P.V("gpsimd", "tensor_tensor", xr[xi_], d1, d2, ALU.subtract, reads=["d1", "d2"], writes=[f"xr{xi_}"])
                P.V("gpsimd", "tensor_tensor", d1, wr, sinT[:, r, :C], ALU.mult, reads=["wr", "sinT"], writes=["d1"])
                P.V("vector", "tensor_tensor", d2, wi_, cosT[:, r, :C], ALU.mult, reads=["wi_", "cosT"], writes=["d2"])
                P.V("gpsimd", "tensor_tensor", xi[xi_], d1, d2, ALU.add, reads=["d1", "d2"], writes=[f"xi{xi_}"])
                P.mm(PSy[yi][:, 0:C], Cre[:, r, :], xr[xi_], rr == 0, False, reads=["Cre", f"xr{xi_}"], writes=[f"psy{yi}"])
                P.mm(PSy[yi][:, 0:C], Cim[:, r, :], xi[xi_], False, rr == 3, reads=["Cim", f"xi{xi_}"], writes=[f"psy{yi}"])
                it += 1
            P.V("vector", "scalar_tensor_tensor", yb, ub[ui], dsk[:, cb:cb + 1], PSy[yi][:, 0:C], ALU.mult, ALU.add, reads=[f"ub{ui}", "dsk", f"psy{yi}"], writes=["yb"])
            P.V("gpsimd", "tensor_tensor", g1, yb, yb, ALU.mult, reads=["yb"], writes=["g1"])
            P.V("vector", "tensor_scalar", g1, g1, 0.044715, 1.0, ALU.mult, ALU.add, reads=["g1"], writes=["g1"])
            P.V("gpsimd", "tensor_tensor", g1, g1, yb, ALU.mult, reads=["g1", "yb"], writes=["g1"])
            P.act(g2, g1, AF.Tanh, scale=float(np.sqrt(2.0 / np.pi)), reads=["g1"], writes=["g2"])
            P.V("vector", "tensor_scalar", g2, g2, 1.0, 0.5, ALU.add, ALU.mult, reads=["g2"], writes=["g2"])
            zi = (cb * NCK + ck) % 2
            P.V("gpsimd", "tensor_tensor", zo[zi], g2, yb, ALU.mult, reads=["g2", "yb"], writes=[f"zo{zi}"])
            P.dma("scalar", zg_d[cb, :, cs], zo[zi], reads=[f"zo{zi}"], final=True)
    return P


EI, EO = "ExternalInput", "ExternalOutput"

def ret_consts(head):
    log_g = np.log(1.0 - 2.0 ** (-5.0 - np.float32(head))).astype(np.float32)
    pos = np.arange(128, dtype=np.float32)
    diff = pos[None, :] - pos[:, None]
    intraT = np.where(diff >= 0, np.exp(np.maximum(diff, 0.0) * log_g), 0.0).astype(np.float32)
    xi = np.exp((pos + 1.0) * log_g).astype(np.float32)
    zeta = np.exp((127.0 - pos) * log_g).astype(np.float32)
    gch = np.exp(128.0 * log_g).astype(np.float32)
    return {"c_intraT": intraT, "c_xi": np.tile(xi[None, :], (128, 1)), "c_zg": np.stack([zeta, np.full(128, gch, np.float32)], 1).astype(np.float32),
            "c_ident": np.eye(128, dtype=np.float32)}

def build_ret(L):
    P = Prog()
    NC = L // 128
    qT_d = P.dram("qT", [2, 128, L], F32, kind=EI); kT_d = P.dram("kT", [2, 128, L], F32, kind=EI)
    cos_d = P.dram("cosr", [128, L], F32, kind=EI); sin_d = P.dram("sinr", [128, L], F32, kind=EI)
    v_d = P.dram("v", [L, 256], F32, kind=EI); g_d = P.dram("gate", [L, 256], F32, kind=EI)
    intraT_d = P.dram("c_intraT", [128, 128], F32, kind=EI); xi_d = P.dram("c_xi", [128, 128], F32, kind=EI)
    zg_d = P.dram("c_zg", [128, 2], F32, kind=EI); ident_d = P.dram("c_ident", [128, 128], F32, kind=EI)
    od_d = P.dram("od", [L, 256], F32, kind=EO)
    T = P.sb
    intraT = T("intraT", [128, 128]); xib = T("xib", [128, 128]); zg = T("zg", [128, 2]); identf = T("identf", [128, 128]); identb = T("identb", [128, 128], BF16)
    P.dma("sync", intraT, intraT_d, writes=["intraT"]); P.dma("sync", xib, xi_d, writes=["xib"]); P.dma("sync", zg, zg_d, writes=["zg"])
    P.dma("sync", identf, ident_d, writes=["identf"])
    P.V("vector", "tensor_copy", identb, identf, reads=["identf"], writes=["identb"])
    Sf = T("Sf", [128, 2, 256]); Sb = T("Sb", [128, 2, 256], BF16)
    P.V("vector", "memset", Sf, 0.0, writes=["Sf"]); P.V("vector", "memset", Sb, 0.0, writes=["Sb"])
    NB = 2
    qin = [T(f"qin{i}", [128, 2, 128]) for i in range(NB)]; kin = [T(f"kin{i}", [128, 2, 128]) for i in range(NB)]
    cs_ = [T(f"cs{i}", [128, 128]) for i in range(NB)]; sn_ = [T(f"sn{i}", [128, 128]) for i in range(NB)]
    vin = [T(f"vin{i}", [128, 256]) for i in range(NB)]; gin = [T(f"gin{i}", [128, 256]) for i in range(NB)]
    a1 = T("a1", [128, 128]); a2 = T("a2", [128, 128]); a3 = T("a3", [128, 128]); a4 = T("a4", [128, 128])
    QT = T("QT", [128, 2, 128], BF16); KT = T("KT", [128, 2, 128], BF16); QX = T("QX", [128, 2, 128], BF16); qf = T("qf", [128, 2, 128])
    Vb = T("Vb", [128, 256], BF16); attT = T("attT", [128, 128], BF16); Kz = T("Kz", [128, 256], BF16)
    osb = T("osb", [128, 256]); oc = T("oc", [128, 256]); sq = T("sq", [128, 256]); sgt = T("sgt", [128, 256])
    outb = [T(f"outb{i}", [128, 256]) for i in range(2)]
    s1 = T("s1", [128, 1]); s2 = T("s2", [128, 1]); nm = T("nm", [128, 1]); rstd = T("rstd", [128, 1])
    pA = P.ps("psA", [128, 512], F32); pO = P.ps("psO", [128, 512], F32); pS = [P.ps(f"psS{i}", [128, 512], F32) for i in range(2)]
    pT = P.ps("psT", [128, 1024], BF16)
    for c in range(NC):
        i = c % NB
        cs = slice(c * 128, (c + 1) * 128)
        P.dma("sync", qin[i], qT_d[:, :, cs].rearrange("a p t -> p a t"), writes=[f"qin{i}"])
        P.dma("scalar", kin[i], kT_d[:, :, cs].rearrange("a p t -> p a t"), writes=[f"kin{i}"])
        P.dma("gpsimd", cs_[i], cos_d[:, cs], writes=[f"cs{i}"]); P.dma("gpsimd", sn_[i], sin_d[:, cs], writes=[f"sn{i}"])
        P.dma("sync", vin[i], v_d[cs, :], writes=[f"vin{i}"]); P.dma("scalar", gin[i], g_d[cs, :], writes=[f"gin{i}"])
        def rot(xin, xkey, out_f32, okey, scale):
            P.V("vector", "tensor_tensor", a1, xin[:, 0, :], cs_[i], ALU.mult, reads=[xkey, f"cs{i}"], writes=["a1"])
            P.V("gpsimd", "tensor_tensor", a2, xin[:, 1, :], sn_[i], ALU.mult, reads=[xkey, f"sn{i}"], writes=["a2"])
            P.V("vector", "tensor_tensor", a3, xin[:, 0, :], sn_[i], ALU.mult, reads=[xkey, f"sn{i}"], writes=["a3"])
            P.V("gpsimd", "tensor_tensor", a4, xin[:, 1, :], cs_[i], ALU.mult, reads=[xkey, f"cs{i}"], writes=["a4"])
            if scale == 1.0:
                P.V("vector", "tensor_tensor", out_f32[:, 0, :], a1, a2, ALU.subtract, reads=["a1", "a2"], writes=[okey])
                P.V("gpsimd", "tensor_tensor", out_f32[:, 1, :], a3, a4, ALU.add, reads=["a3", "a4"], writes=[okey])
            else:
                P.V("vector", "scalar_tensor_tensor", out_f32[:, 0, :], a1, scale, a2, ALU.mult, ALU.subtract, reads=["a1", "a2"], writes=[okey])
        rot(qin[i], f"qin{i}", qf, "qf", 1.0)
        P.act(QT.rearrange("p a t -> p (a t)"), qf.rearrange("p a t -> p (a t)"), AF.Copy, reads=["qf"], writes=["QT"])
        P.V("vector", "tensor_tensor", QX, qf, xib.unsqueeze(1).to_broadcast([128, 2, 128]), ALU.mult, reads=["qf", "xib"], writes=["QX"])
        P.V("vector", "tensor_tensor", a1, kin[i][:, 0, :], cs_[i], ALU.mult, reads=[f"kin{i}", f"cs{i}"], writes=["a1"])
        P.V("gpsimd", "tensor_tensor", a2, kin[i][:, 1, :], sn_[i], ALU.mult, reads=[f"kin{i}", f"sn{i}"], writes=["a2"])
        P.V("vector", "tensor_tensor", a3, kin[i][:, 0, :], sn_[i], ALU.mult, reads=[f"kin{i}", f"sn{i}"], writes=["a3"])
        P.V("gpsimd", "tensor_tensor", a4, kin[i][:, 1, :], cs_[i], ALU.mult, reads=[f"kin{i}", f"cs{i}"], writes=["a4"])
        P.V("vector", "tensor_tensor", a1, a1, a2, ALU.subtract, reads=["a1", "a2"], writes=["a1"])
        P.V("gpsimd", "tensor_tensor", a3, a3, a4, ALU.add, reads=["a3", "a4"], writes=["a3"])
        P.act(KT[:, 0, :], a1, AF.Copy, scale=1.0 / 16, reads=["a1"], writes=["KT"])
        P.act(KT[:, 1, :], a3, AF.Copy, scale=1.0 / 16, reads=["a3"], writes=["KT"])
        P.V("gpsimd", "tensor_copy", Vb, vin[i], reads=[f"vin{i}"], writes=["Vb"])
        for dt in range(2):
            P.mm(pA[:, 0:128], KT[:, dt, :], QT[:, dt, :], dt == 0, dt == 1, reads=["KT", "QT"], writes=["psA"])
        P.V("vector", "tensor_tensor", attT, pA[:, 0:128], intraT, ALU.mult, reads=["psA", "intraT"], writes=["attT"])
        P.mm(pO[:, 0:256], attT, Vb, True, False, reads=["attT", "Vb"], writes=["psO"])
        for dt in range(2):
            P.mm(pO[:, 0:256], QX[:, dt, :], Sb[:, dt, :], False, dt == 1, reads=["QX", "Sb"], writes=["psO"])
        for dt in range(2):
            P.tr(pT[:, dt * 128:(dt + 1) * 128], KT[:, dt, :], identb, reads=["KT", "identb"], writes=["psT"])
        P.V("vector", "tensor_scalar", Kz, pT[:, 0:256], zg[:, 0:1], None, ALU.mult, reads=["psT", "zg"], writes=["Kz"])
        for dt in range(2):
            P.mm(pS[dt][:, 0:256], Kz[:, dt * 128:(dt + 1) * 128], Vb, True, True, reads=["Kz", "Vb"], writes=[f"psS{dt}"])
            P.V("vector", "scalar_tensor_tensor", Sf[:, dt, :], Sf[:, dt, :], zg[:, 1:2], pS[dt][:, 0:256], ALU.mult, ALU.add,
                reads=["Sf", "zg", f"psS{dt}", "psO"], writes=["Sf"])
        P.act(Sb.rearrange("p a t -> p (a t)"), Sf.rearrange("p a t -> p (a t)"), AF.Copy, reads=["Sf"], writes=["Sb"])
        P.act(osb, pO[:, 0:256], AF.Copy, accum_out=s1, reads=["psO"], writes=["osb", "s1"])
        P.V("vector", "tensor_scalar", nm, s1, -1.0 / 256, None, ALU.mult, reads=["s1"], writes=["nm"])
        P.V("vector", "tensor_scalar", oc, osb, nm, None, ALU.add, reads=["osb", "nm"], writes=["oc"])
        P.act(sq, oc, AF.Square, accum_out=s2, reads=["oc"], writes=["sq", "s2"])
        P.V("vector", "tensor_scalar", s2, s2, 1.0 / 256, 1e-5, ALU.mult, ALU.add, reads=["s2"], writes=["s2"])
        P.act(s2, s2, AF.Sqrt, reads=["s2"], writes=["s2"])
        P.V("vector", "reciprocal", rstd, s2, reads=["s2"], writes=["rstd"])
        P.act(sgt, gin[i], AF.Silu, reads=[f"gin{i}"], writes=["sgt"])
        ob = outb[c % 2]
        P.V("vector", "scalar_tensor_tensor", ob, oc, rstd, sgt, ALU.mult, ALU.mult, reads=["oc", "rstd", "sgt"], writes=[f"outb{c%2}"])
        P.dma("sync", od_d[cs, :], ob, reads=[f"outb{c%2}"], final=True)
    return P


D_MODEL = 4096; SEQ = 8192; D_FF = 11008
ROPE_THETA = 500000.0

def _vec(v):
    return np.ascontiguousarray(np.asarray(v, np.float32).reshape(-1, 128).T)

def _launch(P, ins):
    nc = P.build()
    res = run_bass_kernel_spmd(nc, ins, core_ids=list(range(len(ins))))
    return res.results

def _rot_tables(L, inv):
    ang = np.arange(L, dtype=np.float32)[:, None] * inv[None, :].astype(np.float32)
    c = np.cos(ang).astype(np.float32); s = np.sin(ang).astype(np.float32)
    return np.ascontiguousarray(np.concatenate([c, c], 1).T), np.ascontiguousarray(np.concatenate([s, s], 1).T)

def _inv_freq(rot):
    return (np.float32(ROPE_THETA) ** (-np.arange(0, rot, 2, dtype=np.float32) / np.float32(rot))).astype(np.float32)

def _dsa_blocks(c, L):
    return [8 * j + (c if j % 2 == 0 else 7 - c) for j in range(L // 1024)]

def _dsa_inputs(q0, k0, v0, qi0, ki0, wi0, c, L, tabs):
    (ca, sa), (ci, si) = tabs
    d = {}
    tok = [np.arange(b * 128, b * 128 + 128) for b in _dsa_blocks(c, L)]
    d["qT"] = np.ascontiguousarray(np.stack([q0[t].transpose(1, 2, 0) for t in tok]))
    d["cosq"] = np.ascontiguousarray(np.stack([ca[:, t] for t in tok])); d["sinq"] = np.ascontiguousarray(np.stack([sa[:, t] for t in tok]))
    d["qiT"] = np.ascontiguousarray(np.stack([qi0[t].transpose(1, 2, 0).reshape(16, 128, 128) for t in tok]))
    d["cosqi"] = np.ascontiguousarray(np.stack([ci[:, t] for t in tok])); d["sinqi"] = np.ascontiguousarray(np.stack([si[:, t] for t in tok]))
    d["wi"] = np.ascontiguousarray(np.stack([wi0[t] for t in tok]))
    d["qpos"] = np.ascontiguousarray(np.stack([t.astype(np.float32)[:, None] for t in tok]))
    return d

def _rwkv_inputs(zrT, prm, core, L):
    Dh = 2048
    def sec(off, n):
        a = zrT[off:off + n]
        return np.concatenate([np.zeros((n, 1), np.float32), a], axis=1)
    ch = slice(core * 256, core * 256 + 256)
    d = {}
    for i, nme in enumerate("rkv"):
        d["z" + nme] = np.ascontiguousarray(sec(i * Dh + core * 256, 256).reshape(2, 128, L + 1))
    d["zg"] = None
    mu = prm["e_mu"]
    cols = [mu[0:Dh][ch], mu[Dh:2 * Dh][ch], mu[2 * Dh:3 * Dh][ch], mu[3 * Dh + 192:3 * Dh + 448],
            prm["e_w0"][ch], prm["e_a0"][ch], prm["e_k_k"][ch], prm["e_k_a"][ch], prm["e_r_k"].reshape(-1)[ch]]
    d["par"] = np.ascontiguousarray(np.stack(cols, axis=1).reshape(2, 128, 9).astype(np.float32))
    d["wup"] = np.ascontiguousarray(prm["e_w_up"][:, ch]); d["aup"] = np.ascontiguousarray(prm["e_a_up"][:, ch])
    d["gup"] = np.ascontiguousarray(prm["e_g_up"][:, ch].reshape(2, 128, 256))
    d["lnwb"] = np.ascontiguousarray(np.stack([prm["e_ln_w"][ch], prm["e_ln_b"][ch]], axis=0))
    return d

def _s5_inputs(prm, c, C=512):
    d = {}
    Bre = np.zeros((8, 128, 128), np.float32); Bim = np.zeros_like(Bre); Cre = np.zeros_like(Bre); Cim = np.zeros_like(Bre)
    lam = np.zeros((128, 8, 3), np.float32)
    for r in range(8):
        for s in range(2):
            g = c * 16 + r * 2 + s
            gl = (r % 4) * 2 + s
            Bre[r, 16 * gl:16 * gl + 16, 64 * s:64 * s + 64] = prm["o_b_re"][g].T
            Bim[r, 16 * gl:16 * gl + 16, 64 * s:64 * s + 64] = prm["o_b_im"][g].T
            Cre[r, 64 * s:64 * s + 64, 16 * gl:16 * gl + 16] = prm["o_c_re"][g].T
            Cim[r, 64 * s:64 * s + 64, 16 * gl:16 * gl + 16] = prm["o_c_im"][g].T
            lam[64 * s:64 * s + 64, r, 0] = prm["o_lam_re"][g]; lam[64 * s:64 * s + 64, r, 1] = prm["o_lam_im"][g]
            lam[64 * s:64 * s + 64, r, 2] = prm["o_log_step"][g]
    d["Bre"] = Bre; d["Bim"] = Bim; d["Cre"] = Cre; d["Cim"] = Cim; d["lam"] = lam
    d["dsk"] = np.ascontiguousarray(prm["o_d_skip"][c * 256:(c + 1) * 256].reshape(2, 128).T)
    d["c_iota"] = np.tile(np.arange(C + 1, dtype=np.float32)[None, :], (128, 1))
    return d

def kernel(**inp):
    A = {k: np.asarray(v, np.float32) for k, v in inp.items()}
    L = SEQ; NCORE = 8; TC = L // NCORE
    x = A["x"][0]
    xT = np.ascontiguousarray(x.T)
    tsl = [slice(c * TC, (c + 1) * TC) for c in range(NCORE)]
    P = build_dense(dict(D=D_MODEL, FF=D_FF, TC=TC, TB=512, mode="in0", NIN=11808, kchunk=8))
    w_in0 = np.ascontiguousarray(A["e_w_in"][0]); gm0 = _vec(A["norm_mix"][0])
    r = _launch(P, [{"hT": np.ascontiguousarray(xT[:, tsl[c]]), "g_mix": gm0, "w_in": w_in0} for c in range(NCORE)])
    z0T = np.concatenate([r[c]["zT"] for c in range(NCORE)], axis=1)
    del r
    q0 = np.ascontiguousarray(z0T[0:2048].T).reshape(L, 16, 128)
    qi0 = np.ascontiguousarray(z0T[3072:5120].T).reshape(L, 32, 64)
    wi0 = np.ascontiguousarray(z0T[5184:5216].T)
    tabs = (_rot_tables(L, _inv_freq(32)), _rot_tables(L, _inv_freq(16)))
    common = {"kT": np.ascontiguousarray(z0T[2048:2560].reshape(4, 128, L)), "cosk": tabs[0][0], "sink": tabs[0][1],
              "kiT": np.ascontiguousarray(z0T[5120:5184]), "coski": tabs[1][0], "sinki": tabs[1][1],
              "v": np.ascontiguousarray(z0T[2560:3072].T)}
    common.update(dsa_consts())
    P = build_dsa(L)
    ins = []
    for c in range(NCORE):
        d = _dsa_inputs(q0, None, None, qi0, None, wi0, c, L, tabs); d.update(common); ins.append(d)
    r = _launch(P, ins)
    o_a = np.zeros((L, 2048), np.float32)
    for c in range(NCORE):
        for j, b in enumerate(_dsa_blocks(c, L)):
            o_a[b * 128:(b + 1) * 128] = r[c]["oa"][j]
    del r, ins, q0, qi0
    prm = {k: A[k][0] for k in ["e_mu", "e_w0", "e_w_up", "e_a0", "e_a_up", "e_g_up", "e_k_k", "e_k_a", "e_r_k", "e_ln_w", "e_ln_b"]}
    zrT = z0T[5216:11808]
    def pad(a):
        return np.concatenate([np.zeros((a.shape[0], 1), np.float32), a], axis=1)
    zw = np.ascontiguousarray(pad(zrT[6144:6240])); za = np.ascontiguousarray(pad(zrT[6240:6336]))
    zg = np.ascontiguousarray(pad(zrT[6336:6592]).reshape(2, 128, L + 1))
    mu = prm["e_mu"]
    parw = np.ascontiguousarray(np.stack([mu[6144:6240], mu[6240:6336]], axis=1))
    cst = rwkv_consts()
    ins = []
    for c in range(NCORE):
        d = _rwkv_inputs(zrT, prm, c, L); d["zg"] = zg; d["zw"] = zw; d["za"] = za; d["parw"] = parw; d.update(cst); ins.append(d)
    P = build_rwkv(L)
    r = _launch(P, ins)
    o_b = np.concatenate([r[c]["ob"] for c in range(NCORE)], axis=1)
    del r, ins, z0T
    oT = np.ascontiguousarray(np.concatenate([o_a, o_b], axis=1).T)
    P = build_dense(dict(D=D_MODEL, FF=D_FF, TC=TC, TB=512, mode="mid", NIN=10240, kchunk=8))
    wts = {"g_ffn": _vec(A["norm_ffn"][0]), "w_out": np.ascontiguousarray(A["e_w_out"][0]), "w_gate": np.ascontiguousarray(A["ffn_gate"][0]),
           "w_up": np.ascontiguousarray(A["ffn_up"][0]), "w_down": np.ascontiguousarray(A["ffn_down"][0]),
           "g_mix": _vec(A["norm_mix"][1]), "w_in": np.ascontiguousarray(A["o_w_in"][0])}
    ins = []
    for c in range(NCORE):
        d = {"hT": np.ascontiguousarray(xT[:, tsl[c]]), "oT": np.ascontiguousarray(oT[:, tsl[c]])}; d.update(wts); ins.append(d)
    r = _launch(P, ins)
    h2T = np.concatenate([r[c]["h2T"] for c in range(NCORE)], axis=1)
    z1T = np.concatenate([r[c]["zT"] for c in range(NCORE)], axis=1)
    del r, ins, wts, oT
    prm = {k: A[k][0] for k in ["o_lam_re", "o_lam_im", "o_log_step", "o_b_re", "o_b_im", "o_c_re", "o_c_im", "o_d_skip"]}
    ins = []
    for c in range(NCORE):
        d = _s5_inputs(prm, c); d["uT"] = np.ascontiguousarray(z1T[c * 256:(c + 1) * 256].reshape(2, 128, L)); ins.append(d)
    P = build_s5(L)
    r = _launch(P, ins)
    zgT = np.concatenate([r[c]["zgT"].reshape(256, L) for c in range(NCORE)], axis=0)
    del r, ins
    inv = (1.0 / (np.float32(10000.0) ** np.linspace(0.0, 1.0, 128, dtype=np.float32))).astype(np.float32)
    ang = np.arange(L, dtype=np.float32)[:, None] * inv[None, :]
    cosr = np.ascontiguousarray(np.cos(ang).astype(np.float32).T); sinr = np.ascontiguousarray(np.sin(ang).astype(np.float32).T)
    ins = []
    for c in range(NCORE):
        ch = slice(c * 256, (c + 1) * 256)
        d = {"qT": np.ascontiguousarray(z1T[2048:4096][ch].reshape(2, 128, L)), "kT": np.ascontiguousarray(z1T[4096:6144][ch].reshape(2, 128, L)),
             "v": np.ascontiguousarray(z1T[6144:8192][ch].T), "gate": np.ascontiguousarray(z1T[8192:10240][ch].T), "cosr": cosr, "sinr": sinr}
        d.update(ret_consts(c)); ins.append(d)
    P = build_ret(L)
    r = _launch(P, ins)
    odT = np.concatenate([np.ascontiguousarray(r[c]["od"].T) for c in range(NCORE)], axis=0)
    del r, ins, z1T
    oT = np.ascontiguousarray(np.concatenate([zgT, odT], axis=0))
    P = build_dense(dict(D=D_MODEL, FF=D_FF, TC=TC, TB=512, mode="last", G=2048, kchunk=8))
    wts = {"g_ffn": _vec(A["norm_ffn"][1]), "w_out": np.ascontiguousarray(A["o_w_out"][0]), "w_gate": np.ascontiguousarray(A["ffn_gate"][1]),
           "w_up": np.ascontiguousarray(A["ffn_up"][1]), "w_down": np.ascontiguousarray(A["ffn_down"][1]),
           "g_fin": _vec(A["final_norm"]), "w_glu": np.ascontiguousarray(A["o_w_glu"][0]), "b_glu": _vec(A["o_b_glu"][0])}
    ins = []
    for c in range(NCORE):
        d = {"hT": np.ascontiguousarray(h2T[:, tsl[c]]), "oT": np.ascontiguousarray(oT[:, tsl[c]])}; d.update(wts); ins.append(d)
    r = _launch(P, ins)
    yT = np.concatenate([r[c]["yT"] for c in range(NCORE)], axis=1)
    return np.ascontiguousarray(yT.T).reshape(1, L, D_MODEL).astype(np.float32)
```
